# Optimizing a Trainium2 kernel written in Bass

```python
import math
import jax, jax.numpy as jnp
from jax import lax
import numpy as np


D_MODEL = 1024
BATCH = 8
SEQ = 4096
DEPTH = 2

MIX_W = 512
ATTN_HEADS = 8
ATTN_HEAD_DIM = 64
IDX_HEADS = 8
IDX_DIM = 64
TOPK_MAX = 256
Q_BLOCK = 128
N_BUCKETS = 32
MAX_DISTANCE = 128
RWKV_HEADS = 8
RWKV_HEAD_DIM = 64
LORA_DECAY = 64
LORA_ICLR = 64
LORA_GATE = 128
CONV_CH = 512
CONV_W = 3
D_FF = 2816
N_BRANCH = 3
NORM_EPS = 1e-6
GN_EPS = 64e-5
NEG_INF = -1e30

ATTN_COLS = 3 * MIX_W + IDX_HEADS * IDX_DIM + IDX_DIM + IDX_HEADS
RWKV_COLS = 3 * MIX_W + LORA_DECAY + LORA_ICLR + LORA_GATE
CONV_COLS = 3 * CONV_CH
GATE_COLS = N_BRANCH * D_MODEL
IN_COLS = ATTN_COLS + RWKV_COLS + CONV_COLS + GATE_COLS

kernel_name = 'hybrid_dsa_rwkv7_shortconv_block'


def split_cols(z, sizes):
    out, start = [], 0
    for n in sizes:
        out.append(z[..., start:start + n])
        start += n
    return out


def rmsnorm(x, g):
    xf = x.astype(jnp.float32)
    y = xf * lax.rsqrt(jnp.mean(xf * xf, axis=-1, keepdims=True) + NORM_EPS)
    return (y * g.astype(jnp.float32)).astype(x.dtype)


def token_shift(z):
    return jnp.pad(z, ((0, 0), (1, 0), (0, 0)))[:, :-1]


def causal_dwconv(z, w):
    s = z.shape[1]
    zp = jnp.pad(z, ((0, 0), (CONV_W - 1, 0), (0, 0)))
    return sum(zp[:, j:j + s] * w[:, j] for j in range(CONV_W))


def t5_bucket(dist):
    n = jnp.maximum(dist, 0)
    max_exact = N_BUCKETS // 2
    nf = jnp.maximum(n, 1).astype(jnp.float32)
    large = max_exact + (jnp.log(nf / max_exact) / math.log(MAX_DISTANCE / max_exact)
                         * (N_BUCKETS - max_exact)).astype(jnp.int32)
    large = jnp.minimum(large, N_BUCKETS - 1)
    return jnp.where(n < max_exact, n, large)


def dsa_attention(z_attn, positions, rel_bias):
    b, s, _ = z_attn.shape
    q, k, v, qi, ki, wi = split_cols(z_attn, (MIX_W, MIX_W, MIX_W, IDX_HEADS * IDX_DIM, IDX_DIM, IDX_HEADS))
    q = q.reshape(b, s, ATTN_HEADS, ATTN_HEAD_DIM)
    k = k.reshape(b, s, ATTN_HEADS, ATTN_HEAD_DIM)
    v = v.reshape(b, s, ATTN_HEADS, ATTN_HEAD_DIM)
    qi = qi.reshape(b, s, IDX_HEADS, IDX_DIM)
    wi = wi * IDX_HEADS ** -0.5
    n_keep = min(TOPK_MAX, s // 4)
    nb = s // Q_BLOCK
    s_idx = jnp.arange(s, dtype=jnp.int32)
    t_blocks = s_idx.reshape(nb, Q_BLOCK)
    gather = jax.vmap(lambda a, i: a[i])

    def to_blocks(a):
        return jnp.moveaxis(a.reshape((b, nb, Q_BLOCK) + a.shape[2:]), 1, 0)

    def one_block(args):
        q_b, qi_b, wi_b, pos_b, t_b = args
        sc = jnp.einsum('bqhd,bsd->bqsh', qi_b, ki) * IDX_DIM ** -0.5
        score = jnp.einsum('bqsh,bqh->bqs', jax.nn.relu(sc), wi_b).astype(jnp.float32)
        score = jnp.where(s_idx[None, None, :] <= t_b[None, :, None], score, NEG_INF)
        _, sel = lax.top_k(score, n_keep)
        k_sel = gather(k, sel)
        v_sel = gather(v, sel)
        logits = jnp.einsum('bqhd,bqkhd->bqhk', q_b, k_sel).astype(jnp.float32) * ATTN_HEAD_DIM ** -0.5
        dist = pos_b[:, :, None] - gather(positions, sel)
        bias = rel_bias[t5_bucket(dist)].astype(jnp.float32)
        logits = logits + jnp.moveaxis(bias, -1, 2)
        valid = (sel <= t_b[None, :, None])[:, :, None, :]
        p = jax.nn.softmax(jnp.where(valid, logits, NEG_INF), axis=-1)
        o = jnp.einsum('bqhk,bqkhd->bqhd', p.astype(v.dtype), v_sel)
        return o.reshape(b, Q_BLOCK, MIX_W)

    out = lax.map(one_block, (to_blocks(q), to_blocks(qi), to_blocks(wi), to_blocks(positions), t_blocks))
    return jnp.moveaxis(out, 0, 1).reshape(b, s, MIX_W)


def rwkv7_mix(z, mu, w0, w_up, a0, a_up, g_up, k_k, k_a, r_k, ln_w, ln_b):
    b, s, _ = z.shape
    f32 = jnp.float32
    z = z + (token_shift(z) - z) * mu
    r, k, v, wd, ad, gd = split_cols(z, (MIX_W, MIX_W, MIX_W, LORA_DECAY, LORA_ICLR, LORA_GATE))
    log_w = -jax.nn.softplus(-(w0 + jnp.tanh(wd) @ w_up).astype(f32)) - 0.5
    decay = jnp.exp(-jnp.exp(log_w))
    a_f = jax.nn.sigmoid((a0 + ad @ a_up).astype(f32))
    g = jax.nn.sigmoid(gd) @ g_up
    heads = lambda t: t.astype(f32).reshape(b, s, RWKV_HEADS, RWKV_HEAD_DIM)
    kk = heads(k * k_k)
    kk = kk / jnp.maximum(jnp.sqrt(jnp.sum(kk * kk, axis=-1, keepdims=True)), 1e-12)
    k_mod = k.astype(f32) * (1 + (a_f - 1) * k_a)
    r_h, w_h, k_h, v_h, a_h = heads(r), heads(decay), heads(k_mod), heads(v), heads(a_f)

    def step(state, inp):
        r_t, w_t, k_t, v_t, ka_t, kb_t = inp
        sa = jnp.einsum('bhij,bhj->bhi', state, ka_t)
        state = state * w_t[:, :, None, :] + sa[..., None] * kb_t[:, :, None, :] + v_t[..., None] * k_t[:, :, None, :]
        return state, jnp.einsum('bhij,bhj->bhi', state, r_t)

    tm = lambda t: jnp.moveaxis(t, 1, 0)
    s0 = jnp.zeros((b, RWKV_HEADS, RWKV_HEAD_DIM, RWKV_HEAD_DIM), f32)
    _, y = lax.scan(step, s0, (tm(r_h), tm(w_h), tm(k_h), tm(v_h), tm(-kk), tm(kk * a_h)))
    y = jnp.moveaxis(y, 0, 1)
    mean = jnp.mean(y, axis=-1, keepdims=True)
    var = jnp.mean(jnp.square(y - mean), axis=-1, keepdims=True)
    hn = (RWKV_HEADS, RWKV_HEAD_DIM)
    y = (y - mean) * lax.rsqrt(var + GN_EPS) * ln_w.astype(f32).reshape(hn) + ln_b.astype(f32).reshape(hn)
    y = y + jnp.sum(r_h * k_h * r_k.astype(f32).reshape(hn), axis=-1, keepdims=True) * v_h
    return (y.reshape(b, s, MIX_W) * g.astype(f32)).astype(z.dtype)


def conv_glu_ffn(h, w_up, conv_w, w_down):
    a, g = jnp.split(h @ w_up, 2, axis=-1)
    return (jax.nn.silu(causal_dwconv(a, conv_w)) * g) @ w_down


def setup_inputs(seed: int = 0) -> dict:
    key = jax.random.key(seed)
    ks = jax.random.split(key, 32)
    nrm = lambda k, shape, scale: jax.random.normal(k, shape, jnp.float32) * scale
    L = DEPTH
    return {
        'x': nrm(ks[0], (BATCH, SEQ, D_MODEL), 1.0),
        'c': nrm(ks[1], (BATCH, D_MODEL), 1.0),
        'positions': jnp.broadcast_to(jnp.arange(SEQ, dtype=jnp.int32), (BATCH, SEQ)),
        'rel_bias': nrm(ks[2], (N_BUCKETS, ATTN_HEADS), 0.5),
        'final_norm': 1.0 + nrm(ks[3], (D_MODEL,), 0.02),
        'ada_w': nrm(ks[4], (L, D_MODEL, 6 * D_MODEL), 0.5 * D_MODEL ** -0.5),
        'ada_b': nrm(ks[5], (L, 6 * D_MODEL), 0.02),
        'norm_mix': 1.0 + nrm(ks[6], (L, D_MODEL), 0.02),
        'w_in': nrm(ks[7], (L, D_MODEL, IN_COLS), D_MODEL ** -0.5),
        'rwkv_mu': jax.random.uniform(ks[8], (L, RWKV_COLS), jnp.float32),
        'rwkv_w0': jax.random.uniform(ks[9], (L, MIX_W), jnp.float32, -4.0, 0.0),
        'rwkv_w_up': nrm(ks[10], (L, LORA_DECAY, MIX_W), 0.1),
        'rwkv_a0': nrm(ks[11], (L, MIX_W), 0.1),
        'rwkv_a_up': nrm(ks[12], (L, LORA_ICLR, MIX_W), LORA_ICLR ** -0.5),
        'rwkv_g_up': nrm(ks[13], (L, LORA_GATE, MIX_W), LORA_GATE ** -0.5),
        'rwkv_k_k': 0.85 + nrm(ks[14], (L, MIX_W), 0.1),
        'rwkv_k_a': 1.0 + nrm(ks[15], (L, MIX_W), 0.1),
        'rwkv_r_k': nrm(ks[16], (L, MIX_W), 0.1),
        'rwkv_ln_w': 1.0 + nrm(ks[17], (L, MIX_W), 0.02),
        'rwkv_ln_b': nrm(ks[18], (L, MIX_W), 0.02),
        'sc_conv_w': nrm(ks[19], (L, CONV_CH, CONV_W), CONV_W ** -0.5),
        'w_branch': nrm(ks[20], (L, N_BRANCH, MIX_W, D_MODEL), MIX_W ** -0.5),
        'w_o': nrm(ks[21], (L, D_MODEL, D_MODEL), D_MODEL ** -0.5),
        'norm_ffn': 1.0 + nrm(ks[22], (L, D_MODEL), 0.02),
        'ffn_w_up': nrm(ks[23], (L, D_MODEL, 2 * D_FF), D_MODEL ** -0.5),
        'ffn_conv_w': nrm(ks[24], (L, D_FF, CONV_W), CONV_W ** -0.5),
        'ffn_w_down': nrm(ks[25], (L, D_FF, D_MODEL), D_FF ** -0.5),
    }


def reference(x, c, positions, rel_bias, final_norm, ada_w, ada_b, norm_mix, w_in,
              rwkv_mu, rwkv_w0, rwkv_w_up, rwkv_a0, rwkv_a_up, rwkv_g_up, rwkv_k_k, rwkv_k_a,
              rwkv_r_k, rwkv_ln_w, rwkv_ln_b, sc_conv_w, w_branch, w_o, norm_ffn,
              ffn_w_up, ffn_conv_w, ffn_w_down):
    b, s, _ = x.shape
    for l in range(DEPTH):
        mod = (c @ ada_w[l] + ada_b[l])[:, None, :]
        sh1, sc1, g1, sh2, sc2, g2 = jnp.split(mod, 6, axis=-1)

        h = rmsnorm(x, norm_mix[l]) * (1 + sc1) + sh1
        z = h @ w_in[l]
        z_attn, z_rwkv, z_conv, z_gate = split_cols(z, (ATTN_COLS, RWKV_COLS, CONV_COLS, GATE_COLS))
        o_attn = dsa_attention(z_attn, positions, rel_bias)
        o_rwkv = rwkv7_mix(z_rwkv, rwkv_mu[l], rwkv_w0[l], rwkv_w_up[l], rwkv_a0[l], rwkv_a_up[l],
                           rwkv_g_up[l], rwkv_k_k[l], rwkv_k_a[l], rwkv_r_k[l], rwkv_ln_w[l], rwkv_ln_b[l])
        c_b, c_c, c_x = split_cols(z_conv, (CONV_CH, CONV_CH, CONV_CH))
        o_conv = c_b * causal_dwconv(c_c * c_x, sc_conv_w[l])
        gates = jax.nn.sigmoid(z_gate).reshape(b, s, N_BRANCH, D_MODEL)
        merged = sum(gates[:, :, i] * (o @ w_branch[l, i]) for i, o in enumerate((o_attn, o_rwkv, o_conv)))
        x = x + g1 * (merged @ w_o[l])

        h = rmsnorm(x, norm_ffn[l]) * (1 + sc2) + sh2
        x = x + g2 * conv_glu_ffn(h, ffn_w_up[l], ffn_conv_w[l], ffn_w_down[l])
    return rmsnorm(x, final_norm)
```

```python
import math
from contextlib import ExitStack

import numpy as np
import ml_dtypes
import concourse.bass as bass
import concourse.mybir as mybir
from concourse.bass_utils import run_bass_kernel_spmd

F32 = mybir.dt.float32
BF16 = mybir.dt.bfloat16
AF = mybir.ActivationFunctionType
ALU = mybir.AluOpType
AX = mybir.AxisListType

T = 4096
D = 1024
NT = 32
DEPTH = 2
IN_COLS = 8520
DFF = 2816
NEG = -1.0e30
CH = 64
NCH = T // CH
EDEC = math.exp(-0.5)

COMPUTE = ("pe", "act", "dve", "pool")
QUEUES = ("sp", "act", "pool")
NDSEM = 12


class Sched:
    def __init__(self, nc, es):
        self.nc = nc
        self.es = es
        self.ops = []
        self.last_w = {}
        self.readers = {}
        self.nbuf = 0
        self.gen = 0
        self.pending = []

    def sb(self, shape, dt, es=None, name="sb"):
        self.nbuf += 1
        return (es or self.es).enter_context(self.nc.sbuf_tensor(f"{name}_{self.nbuf}", list(shape), dt))

    def ps(self, shape, dt, es=None, name="ps"):
        self.nbuf += 1
        return (es or self.es).enter_context(self.nc.psum_tensor(f"{name}_{self.nbuf}", list(shape), dt))

    def barrier(self):
        for p in list(self.pending):
            self._flush(p)
        self.gen += 1
        self.last_w = {}
        self.readers = {}

    def add(self, eng, fn, reads=(), writes=(), dma=False):
        if self.pending:
            ws = set(writes)
            rs = set(reads)
            for p in list(self.pending):
                _, (_q, _fn, prd, pwr) = p
                if ws.intersection(prd) or ws.intersection(pwr) or rs.intersection(pwr):
                    self._flush(p)
        idx = len(self.ops)
        deps = set()
        for k in reads:
            j = self.last_w.get(k)
            if j is not None:
                deps.add(j)
        for k in writes:
            j = self.last_w.get(k)
            if j is not None:
                deps.add(j)
            for r in self.readers.get(k, ()):
                deps.add(r)
        for k in writes:
            self.last_w[k] = idx
            self.readers[k] = []
        for k in reads:
            lst = self.readers.setdefault(k, [])
            if not dma:
                lst[:] = [r for r in lst if not (self.ops[r]["eng"] == eng and not self.ops[r]["dma"])]
            lst.append(idx)
        deps.discard(idx)
        self.ops.append(dict(eng=eng, fn=fn, deps=deps, dma=dma, gen=self.gen))
        return idx

    def dma(self, q, out, in_, reads=(), writes=(), slow=False, defer=0):
        kw = {"allow_slow_non_contiguous": True} if slow else {}
        fn = lambda e: e.dma_start(out=out, in_=in_, **kw)
        if defer > 0:
            self.pending.append([defer, (q, fn, tuple(reads), tuple(writes))])
            return None
        idx = self.add(q, fn, reads, writes, dma=True)
        for p in list(self.pending):
            p[0] -= 1
            if p[0] <= 0:
                self._flush(p)
        return idx

    def _flush(self, p):
        if p in self.pending:
            self.pending.remove(p)
            q, fn, reads, writes = p[1]
            self.add(q, fn, reads, writes, dma=True)

    def emit(self):
        nc = self.nc
        es = self.es
        csem = {e: es.enter_context(nc.semaphore(f"c_{e}")) for e in COMPUTE}
        dsem = {q: [es.enter_context(nc.semaphore(f"d_{q}{i}")) for i in range(NDSEM)] for q in QUEUES}
        needed = [False] * len(self.ops)
        last_in_gen = {}
        for i, op in enumerate(self.ops):
            if op["dma"]:
                needed[i] = True
            else:
                last_in_gen[(op["eng"], op["gen"])] = i
            for j in op["deps"]:
                oj = self.ops[j]
                if (not oj["dma"]) and (not op["dma"]) and oj["eng"] == "pe" and op["eng"] == "pe":
                    continue
                needed[j] = True
        for i in last_in_gen.values():
            needed[i] = True
        self.needed = needed
        ccount = {e: 0 for e in COMPUTE}
        dcount = {q: 0 for q in QUEUES}
        dval = {q: [0] * NDSEM for q in QUEUES}
        done = []
        snaps = {}
        cur_gen = 0
        for i, op in enumerate(self.ops):
            if op["gen"] != cur_gen:
                cur_gen = op["gen"]
                snaps[cur_gen] = (dict(ccount), {q: list(v) for q, v in dval.items()})
            if op["dma"]:
                q = op["eng"]
                n = dcount[q]
                dcount[q] += 1
                k = n % NDSEM
                s = dsem[q][k]
                op["prev"] = (s, dval[q][k]) if dval[q][k] > 0 else None
                dval[q][k] += 16
                done.append((s, dval[q][k]))
            else:
                e = op["eng"]
                if needed[i]:
                    ccount[e] += 1
                done.append((csem[e], ccount[e]))
        streams = {e: [] for e in ("pe", "act", "dve", "pool", "sp")}
        for i, op in enumerate(self.ops):
            streams[op["eng"]].append(i)
        self.stats = {e: len(v) for e, v in streams.items()}
        self.stats["signaled"] = sum(needed)

        def run_stream(ename, eng):
            waited = {}
            my_gen = 0

            def do_wait(s, v):
                if v <= 0 or waited.get(id(s), 0) >= v:
                    return
                waited[id(s)] = v
                eng.wait_ge(s, v)

            for i in streams[ename]:
                op = self.ops[i]
                if op["gen"] != my_gen:
                    my_gen = op["gen"]
                    cc, dv = snaps[my_gen]
                    for e in COMPUTE:
                        if e != ename or e != "pe":
                            do_wait(csem[e], cc[e])
                    for q in QUEUES:
                        for k in range(NDSEM):
                            do_wait(dsem[q][k], dv[q][k])
                waits = []
                for j in sorted(op["deps"]):
                    oj = self.ops[j]
                    if (not oj["dma"]) and oj["eng"] == ename and ename == "pe" and not op["dma"]:
                        continue
                    waits.append(done[j])
                if op["dma"] and op["prev"] is not None:
                    waits.append(op["prev"])
                for s, v in waits:
                    do_wait(s, v)
                ins = op["fn"](eng)
                s, v = done[i]
                if needed[i]:
                    ins.then_inc(s, 16 if op["dma"] else 1)
            if ename in QUEUES:
                for k in range(NDSEM):
                    if dval[ename][k] > 0:
                        eng.wait_ge(dsem[ename][k], dval[ename][k])

        with nc.Block() as block:
            @block.tensor
            def _(eng):
                run_stream("pe", eng)

            @block.scalar
            def _(eng):
                run_stream("act", eng)

            @block.vector
            def _(eng):
                run_stream("dve", eng)

            @block.gpsimd
            def _(eng):
                run_stream("pool", eng)

            @block.sync
            def _(eng):
                run_stream("sp", eng)


def mm(S, out, lhsT, rhs, start=True, stop=True, r=(), w=()):
    S.add("pe", lambda e: e.matmul(out, lhsT, rhs, start=start, stop=stop), r, w)


def tr(S, out, in_, ident, r=(), w=()):
    S.add("pe", lambda e: e.transpose(out, in_, ident), r, w)


def act(S, out, in_, func, r=(), w=(), bias=None, scale=None, accum=None):
    kw = {}
    if bias is not None:
        kw["bias"] = bias
    if scale is not None:
        kw["scale"] = scale
    if accum is not None:
        kw["accum_out"] = accum
    S.add("act", lambda e: e.activation(out, in_, func, **kw), r, w)


def ts(S, eng, out, in0, s1, op0, s2=None, op1=None, r=(), w=(), accum=None):
    kw = {}
    if op1 is not None:
        kw["op1"] = op1
    if accum is not None:
        kw["accum_out"] = accum
    S.add(eng, lambda e: e.tensor_scalar(out, in0, s1, s2, op0, **kw), r, w)


def tt(S, eng, out, in0, in1, op, r=(), w=()):
    S.add(eng, lambda e: e.tensor_tensor(out, in0, in1, op), r, w)


def stt(S, eng, out, in0, scalar, in1, op0, op1, r=(), w=()):
    S.add(eng, lambda e: e.scalar_tensor_tensor(out, in0, scalar, in1, op0, op1), r, w)


def cp(S, eng, out, in_, r=(), w=()):
    if eng == "act":
        S.add("act", lambda e: e.activation(out, in_, AF.Copy), r, w)
    else:
        S.add(eng, lambda e: e.tensor_copy(out, in_), r, w)


def mset(S, eng, ap, val, w=()):
    S.add(eng, lambda e: e.memset(ap, val), (), w)


ZQ, ZK, ZQI, ZKI, ZRW, ZCV, ZGT, ZROWS = 0, 512, 1024, 1536, 1664, 3456, 4992, 8064
WQ, WK, WV, WQI, WKI, WWI, WRW, WCV, WGT = 0, 512, 1024, 1536, 2048, 2112, 2120, 3912, 5448


def t5_bucket_np(n):
    n = np.maximum(n, 0)
    nf = np.maximum(n, 1).astype(np.float32)
    large = 16 + (np.log(nf / np.float32(16)) / np.float32(math.log(128 / 16)) * np.float32(16)).astype(np.int32)
    large = np.minimum(large, 31)
    return np.where(n < 16, n, large)


def make_consts():
    bf = ml_dtypes.bfloat16
    c = {}
    c["ident_bf"] = np.eye(128, dtype=np.float32).astype(bf)
    c["ident_f"] = np.eye(128, dtype=np.float32)
    q = np.arange(128)[:, None]
    s = np.arange(128)[None, :]
    c["tri"] = (s <= q).astype(np.float32).astype(bf)
    blk = np.zeros((128, 128), np.float32)
    blk[:64, :64] = 1
    blk[64:, 64:] = 1
    c["blk_bf"] = blk.astype(bf)
    c["blkmean_f"] = (blk / 64.0).astype(np.float32)
    rm = np.ones((128, T), np.float32)
    rm[:, ::CH] = 0
    c["rmask"] = rm
    p = np.arange(64)[:, None]
    f = np.arange(64)[None, :]
    su = (p < f).astype(np.float32)
    sl = (f < p).astype(np.float32)
    ui = (p <= f).astype(np.float32)
    i64 = np.eye(64, dtype=np.float32)
    c["m64"] = np.stack([np.tile(m, (1, 8)) for m in (su, sl, ui, i64)], axis=1).astype(np.float32)
    u = np.arange(384)
    bk = t5_bucket_np(u - 127)
    oh = np.zeros((32, 384), np.float32)
    oh[bk, u] = 1.0
    oh[:, :127] = 0.0
    c["ohb"] = oh
    return c


CONST_SPECS = [("ident_bf", [128, 128], BF16), ("ident_f", [128, 128], F32), ("tri", [128, 128], BF16),
               ("blk_bf", [128, 128], BF16), ("blkmean_f", [128, 128], F32), ("rmask", [128, T], F32),
               ("m64", [64, 4, 512], F32), ("ohb", [32, 384], F32)]

INPUT_SPECS = [
    ("x", [T, D]), ("c", [D]), ("rel_bias", [32, 8]), ("final_norm", [D]),
    ("ada_w", [DEPTH, D, 6 * D]), ("ada_b", [DEPTH, 6 * D]), ("norm_mix", [DEPTH, D]),
    ("w_in", [DEPTH, D, IN_COLS]), ("rwkv_mu", [DEPTH, 1792]), ("rwkv_w0", [DEPTH, 512]),
    ("rwkv_w_up", [DEPTH, 64, 512]), ("rwkv_a0", [DEPTH, 512]), ("rwkv_a_up", [DEPTH, 64, 512]),
    ("rwkv_g_up", [DEPTH, 128, 512]), ("rwkv_k_k", [DEPTH, 512]), ("rwkv_k_a", [DEPTH, 512]),
    ("rwkv_r_k", [DEPTH, 512]), ("rwkv_ln_w", [DEPTH, 512]), ("rwkv_ln_b", [DEPTH, 512]),
    ("sc_conv_w", [DEPTH, 512, 3]), ("w_branch", [DEPTH, 3, 512, D]), ("w_o", [DEPTH, D, D]),
    ("norm_ffn", [DEPTH, D]), ("ffn_w_up", [DEPTH, D, 2 * DFF]), ("ffn_conv_w", [DEPTH, DFF, 3]),
    ("ffn_w_down", [DEPTH, DFF, D]),
]


class Ctx:
    pass


def col_ap(vec_ap, n):
    return vec_ap.rearrange("(j p o) -> p j o", p=128, o=1)


def stage_consts(C):
    S = C.S
    K = Ctx()
    C.K = K
    for name, shape, dt in CONST_SPECS:
        if name in ("rmask", "ohb"):
            continue
        tl = S.sb(shape, dt, name="k_" + name)
        S.dma("sp", tl[:], C.cin[name], writes=["k_" + name])
        setattr(K, name, tl)
    K.ones_bf = S.sb([128, 128], BF16, name="k_ones")
    mset(S, "pool", K.ones_bf[:], 1.0, w=["k_ones"])
    K.eps = S.sb([128, 1], F32, name="k_eps")
    mset(S, "pool", K.eps[:], 1e-6, w=["k_eps"])
    K.eps_gn = S.sb([128, 1], F32, name="k_epsgn")
    mset(S, "pool", K.eps_gn[:], 64e-5, w=["k_epsgn"])
    S.barrier()


def stage_mod(C, l):
    S = C.S
    inp = C.inp
    with ExitStack() as st:
        ccol = S.sb([128, 8, 1], F32, st)
        S.dma("sp", ccol[:], col_ap(inp["c"], 8), writes=["ccol"], slow=True)
        adab = S.sb([1, 6 * D], F32, st)
        S.dma("sp", adab[:], inp["ada_b"][l].rearrange("(o n) -> o n", o=1), writes=["adab"])
        row = S.sb([1, 6 * D], F32, st)
        aw = [S.sb([128, 6 * D], F32, st) for _ in range(2)]
        ps = [S.ps([128, 512], F32, st) for _ in range(6)]
        for half in range(2):
            for k in range(8):
                b = aw[k % 2]
                S.dma("sp", b[:], inp["ada_w"][l, k * 128:(k + 1) * 128, :], writes=[f"aw{k % 2}"])
                for jj in range(6):
                    j = half * 6 + jj
                    mm(S, ps[jj][0:1, :], ccol[:, k, :], b[:, j * 512:(j + 1) * 512], start=(k == 0), stop=(k == 7),
                       r=[f"aw{k % 2}", "ccol"], w=[f"psm{jj}"])
            for jj in range(6):
                j = half * 6 + jj
                tt(S, "dve", row[0:1, j * 512:(j + 1) * 512], ps[jj][0:1, :], adab[0:1, j * 512:(j + 1) * 512], ALU.add,
                   r=[f"psm{jj}", "adab"], w=["row"])
        S.dma("sp", C.modrow[l].rearrange("(o n) -> o n", o=1), row[:], reads=["row"], writes=["modrow"])
    S.barrier()


def load_mod(C, l, st):
    S = C.S
    inp = C.inp
    M = Ctx()
    modcol = S.sb([128, 48, 1], F32, st)
    S.dma("sp", modcol[:], col_ap(C.modrow[l], 48), writes=["modcol"], slow=True)
    nm = S.sb([128, 8, 1], F32, st)
    nf = S.sb([128, 8, 1], F32, st)
    S.dma("sp", nm[:], col_ap(inp["norm_mix"][l], 8), writes=["nm"], slow=True)
    S.dma("sp", nf[:], col_ap(inp["norm_ffn"][l], 8), writes=["nf"], slow=True)
    M.A1 = S.sb([128, 8, 1], F32, st)
    M.A2 = S.sb([128, 8, 1], F32, st)
    stt(S, "dve", M.A1[:], modcol[:, 8:16, :], 1.0, nm[:], ALU.add, ALU.mult, r=["modcol", "nm"], w=["A1"])
    stt(S, "dve", M.A2[:], modcol[:, 32:40, :], 1.0, nf[:], ALU.add, ALU.mult, r=["modcol", "nf"], w=["A2"])
    M.modcol = modcol
    M.g1 = S.sb([128, D], F32, st)
    M.g2 = S.sb([128, D], F32, st)
    S.dma("sp", M.g1[:], C.modrow[l][2 * D:3 * D].partition_broadcast(128), writes=["g1bc"])
    S.dma("sp", M.g2[:], C.modrow[l][5 * D:6 * D].partition_broadcast(128), writes=["g2bc"])
    S.barrier()
    return M


def stage_norm(C, x_ap, Acol, Bcol, hT, st_outer):
    S = C.S
    K = C.K
    with ExitStack() as st:
        xt = [S.sb([128, D], F32, st) for _ in range(2)]
        junk = S.sb([128, D], BF16, st)
        xn = [S.sb([128, D], BF16, st) for _ in range(2)]
        ss = [S.sb([128, 1], F32, st) for _ in range(2)]
        sd = [S.sb([128, 1], F32, st) for _ in range(2)]
        rs = [S.sb([128, 1], F32, st) for _ in range(2)]
        pT = [S.ps([128, 8, 128], BF16, st) for _ in range(2)]
        S.dma("sp", xt[0][:], x_ap[0:128, :], writes=["xt0"])
        for i in range(NT):
            b = i % 2
            if i + 1 < NT:
                S.dma("sp", xt[1 - b][:], x_ap[(i + 1) * 128:(i + 2) * 128, :], writes=[f"xt{1 - b}"])
            act(S, junk[:], xt[b][:], AF.Square, r=[f"xt{b}"], w=["junk", f"ss{b}"], accum=ss[b][:])
            act(S, sd[b][:], ss[b][:], AF.Sqrt, r=[f"ss{b}"], w=[f"sd{b}"], bias=K.eps[:], scale=1.0 / D)
            S.add("dve", (lambda o, i_: (lambda e: e.reciprocal(o, i_)))(rs[b][:], sd[b][:]), [f"sd{b}"], [f"rs{b}"])
            ts(S, "dve", xn[b][:], xt[b][:], rs[b][:], ALU.mult, r=[f"xt{b}", f"rs{b}"], w=[f"xn{b}"])
            for k in range(8):
                tr(S, pT[b][:, k, :], xn[b][:, k * 128:(k + 1) * 128], K.ident_bf[:], r=[f"xn{b}"], w=[f"pT{b}"])
            for k in range(8):
                eng = "dve" if k % 2 == 0 else "pool"
                if eng == "pool":
                    act(S, hT[:, k, i * 128:(i + 1) * 128], pT[b][:, k, :], AF.Identity, r=[f"pT{b}"], w=[f"hT{i}"],
                        bias=Bcol[:, k, :], scale=Acol[:, k, :])
                else:
                    ts(S, "dve", hT[:, k, i * 128:(i + 1) * 128], pT[b][:, k, :], Acol[:, k, :], ALU.mult, s2=Bcol[:, k, :],
                       op1=ALU.add, r=[f"pT{b}"], w=[f"hT{i}"])
    S.barrier()


def proj_fm(C, hT, w_ap, blocks, out_ap, st, scale=None):
    S = C.S
    wst = [S.sb([128, 8, 512], F32, st) for _ in range(2)]
    wbf = [S.sb([128, 8, 512], BF16, st) for _ in range(2)]
    zrow = [S.sb([128, T], BF16, st) for _ in range(2)]
    ps = [S.ps([128, 512], F32, st) for _ in range(4)]
    wv = w_ap.rearrange("(k p) c -> p k c", p=128)
    row0 = 0
    nev = 0
    nz = 0
    for bi, segs in enumerate(blocks):
        b = bi % 2
        off = 0
        for (c0, nc_) in segs:
            S.dma("sp", wst[b][:, :, off:off + nc_], wv[:, :, c0:c0 + nc_], writes=[f"wst{b}"])
            off += nc_
        cp(S, "pool", wbf[b][:, :, 0:off], wst[b][:, :, 0:off], r=[f"wst{b}"], w=[f"wbf{b}"])
        for cc in range(off // 128):
            zb = nz % 2
            nz += 1
            for tb in range(8):
                p = ps[nev % 4]
                pk = f"psp{nev % 4}"
                for k in range(8):
                    mm(S, p[:], wbf[b][:, k, cc * 128:(cc + 1) * 128], hT[:, k, tb * 512:(tb + 1) * 512],
                       start=(k == 0), stop=(k == 7), r=[f"wbf{b}", "hT"], w=[pk])
                if nev % 2 == 0:
                    act(S, zrow[zb][:, tb * 512:(tb + 1) * 512], p[:], AF.Copy, r=[pk], w=[f"zrow{zb}"])
                else:
                    cp(S, "dve", zrow[zb][:, tb * 512:(tb + 1) * 512], p[:], r=[pk], w=[f"zrow{zb}"])
                nev += 1
            S.dma("sp", out_ap[row0:row0 + 128, :], zrow[zb][:], reads=[f"zrow{zb}"], writes=["zout"], defer=2)
            row0 += 128


def stage_inproj(C, l, hT):
    S = C.S
    w = C.inp["w_in"][l]
    with ExitStack() as st:
        blocks = [[(WQ, 512)], [(WK, 512)], [(WQI, 512)], [(WKI, 64), (WKI, 64)]]
        blocks += [[(WRW + i * 512, 512)] for i in range(3)] + [[(WRW + 1536, 256)]]
        blocks += [[(WCV + i * 512, 512)] for i in range(3)]
        blocks += [[(WGT + i * 512, 512)] for i in range(6)]
        proj_fm(C, hT, w, blocks, C.zT, st)
    S.barrier()
    with ExitStack() as st:
        wv = w.rearrange("(k p) c -> p k c", p=128)
        wst = S.sb([128, 8, 520], F32, st)
        wbf = S.sb([128, 8, 520], BF16, st)
        S.dma("sp", wst[:, :, 0:512], wv[:, :, WV:WV + 512], writes=["wvst"])
        S.dma("sp", wst[:, :, 512:520], wv[:, :, WWI:WWI + 8], writes=["wvst"])
        cp(S, "pool", wbf[:], wst[:], r=["wvst"], w=["wvbf"])
        vt = [S.sb([128, 512], BF16, st) for _ in range(2)]
        wit = S.sb([128, NT, 8], F32, st)
        ps = [S.ps([128, 512], F32, st) for _ in range(2)]
        ps2 = [S.ps([128, 512], F32, st) for _ in range(2)]
        for i in range(NT):
            b = i % 2
            for k in range(8):
                mm(S, ps[b][:], hT[:, k, i * 128:(i + 1) * 128], wbf[:, k, 0:512], start=(k == 0), stop=(k == 7),
                   r=["wvbf"], w=[f"psv{b}"])
            for k in range(8):
                mm(S, ps2[b][:, 0:8], hT[:, k, i * 128:(i + 1) * 128], wbf[:, k, 512:520], start=(k == 0), stop=(k == 7),
                   r=["wvbf"], w=[f"psw{b}"])
            act(S, vt[b][:], ps[b][:], AF.Copy, r=[f"psv{b}"], w=[f"vt{b}"])
            ts(S, "dve", wit[:, i, :], ps2[b][:, 0:8], 8.0 ** -0.5, ALU.mult, r=[f"psw{b}"], w=["wit"])
            S.dma("sp", C.vtok[i * 128:(i + 1) * 128, :], vt[b][:], reads=[f"vt{b}"], writes=["vtok"])
        S.dma("sp", C.witok.rearrange("(i p) h -> p i h", p=128), wit[:], reads=["wit"], writes=["witok"])
    S.barrier()


def make_ctx(nc, es, debug_out=()):
    C = Ctx()
    C.nc = nc
    C.S = Sched(nc, es)
    C.inp = {}
    for name, shape in INPUT_SPECS:
        C.inp[name] = nc.dram_tensor(name, shape, F32, kind="ExternalInput").ap()
    C.cin = {}
    for name, shape, dt in CONST_SPECS:
        C.cin[name] = nc.dram_tensor("k_" + name, shape, dt, kind="ExternalInput").ap()
    C.debug_out = set(debug_out)

    def dram(name, shape, dt):
        kind = "ExternalOutput" if name in C.debug_out else "Internal"
        return nc.dram_tensor(name, list(shape), dt, kind=kind).ap()

    C.dram = dram
    C.modrow = [dram(f"modrow{l}", [6 * D], F32) for l in range(DEPTH)]
    C.zT = dram("zT", [ZROWS, T], BF16)
    C.vtok = dram("vtok", [T, 512], BF16)
    C.witok = dram("witok", [T, 8], F32)
    C.oT = dram("oT", [1536, T], BF16)
    C.uT = dram("uT", [DFF, T], BF16)
    C.xs = [dram(f"xs{i}", [T, D], F32) for i in range(2 * DEPTH)]
    C.maskT = dram("maskT", [128, 528 * 128], BF16)
    C.zdT = dram("zdT", [8, 128, 384], F32)
    NR = 512
    for nm in ("rRT", "rAT", "rBT", "rKT", "rBH", "rKH", "rVT", "rBV", "rG"):
        setattr(C, nm, dram(nm, [NR, T], BF16))
    C.rYT = dram("rYT", [NR, T], F32)
    C.rGC = dram("rGC", [128, 4 * NCH], F32)
    return C


def host_inputs(inputs, b):
    m = {}
    for name, shape in INPUT_SPECS:
        a = np.asarray(inputs[name])
        if name in ("x", "c"):
            a = a[b]
        m[name] = np.ascontiguousarray(a, dtype=np.float32)
    for k, v in make_consts().items():
        m["k_" + k] = v
    return m


def conv3(S, eng, acc, src, wcol, keys_r, key_w):
    ts(S, eng, acc[:, :], src[:, 2:T + 2], wcol[:, 2:3], ALU.mult, r=keys_r, w=[key_w])
    stt(S, eng, acc[:, :], src[:, 1:T + 1], wcol[:, 1:2], acc[:, :], ALU.mult, ALU.add, r=keys_r + [key_w], w=[key_w])
    stt(S, eng, acc[:, :], src[:, 0:T], wcol[:, 0:1], acc[:, :], ALU.mult, ALU.add, r=keys_r + [key_w], w=[key_w])


def stage_conv(C, l):
    S = C.S
    with ExitStack() as st:
        wc = S.sb([128, 4, 3], F32, st)
        S.dma("sp", wc[:], C.inp["sc_conv_w"][l].rearrange("(c p) j -> p c j", p=128), writes=["wc"])
        zb = [S.sb([128, T], BF16, st) for _ in range(2)]
        zc = [S.sb([128, T], BF16, st) for _ in range(2)]
        zx = [S.sb([128, T], BF16, st) for _ in range(2)]
        pp = [S.sb([128, T + 2], F32, st) for _ in range(2)]
        acc = [S.sb([128, T], F32, st) for _ in range(2)]
        ob = [S.sb([128, T], BF16, st) for _ in range(2)]
        for b in range(2):
            mset(S, "pool", pp[b][:, 0:2], 0.0, w=[f"pp{b}"])
        for cc in range(4):
            b = cc % 2
            S.dma("sp", zb[b][:], C.zT[ZCV + cc * 128:ZCV + (cc + 1) * 128, :], writes=[f"zb{b}"])
            S.dma("sp", zc[b][:], C.zT[ZCV + 512 + cc * 128:ZCV + 512 + (cc + 1) * 128, :], writes=[f"zc{b}"])
            S.dma("sp", zx[b][:], C.zT[ZCV + 1024 + cc * 128:ZCV + 1024 + (cc + 1) * 128, :], writes=[f"zx{b}"])
            tt(S, "pool", pp[b][:, 2:T + 2], zc[b][:], zx[b][:], ALU.mult, r=[f"zc{b}", f"zx{b}"], w=[f"pp{b}"])
            conv3(S, "dve", acc[b], pp[b], wc[:, cc, :], [f"pp{b}", "wc"], f"acc{b}")
            tt(S, "pool", ob[b][:], acc[b][:], zb[b][:], ALU.mult, r=[f"acc{b}", f"zb{b}"], w=[f"ob{b}"])
            S.dma("sp", C.oT[1024 + cc * 128:1024 + (cc + 1) * 128, :], ob[b][:], reads=[f"ob{b}"], writes=["oT"], defer=3)
    S.barrier()


def stage_merge(C, l, M, x_in, x_out):
    S = C.S
    inp = C.inp
    with ExitStack() as st:
        wb = S.sb([128, 12, D], BF16, st)
        wo = S.sb([128, 8, D], BF16, st)
        wst = [S.sb([128, 2, D], F32, st) for _ in range(2)]
        for i in range(10):
            b = i % 2
            if i < 6:
                src = inp["w_branch"][l, i // 2].rearrange("(k p) d -> p k d", p=128)[:, (i % 2) * 2:(i % 2) * 2 + 2, :]
                dst = wb[:, i * 2:i * 2 + 2, :]
            else:
                src = inp["w_o"][l].rearrange("(k p) d -> p k d", p=128)[:, (i - 6) * 2:(i - 6) * 2 + 2, :]
                dst = wo[:, (i - 6) * 2:(i - 6) * 2 + 2, :]
            S.dma("sp", wst[b][:], src, writes=[f"wst{b}"])
            cp(S, "pool", dst, wst[b][:], r=[f"wst{b}"], w=["wbo"])
        ot = [S.sb([128, 12, 512], BF16, st) for _ in range(2)]
        gt = [S.sb([128, 24, 512], BF16, st) for _ in range(1)]
        mg = [S.sb([128, 8, 512], BF16, st) for _ in range(2)]
        sg = [S.sb([128, 512], BF16, st) for _ in range(3)]
        mt = [S.sb([128, 512], F32, st) for _ in range(3)]
        m01 = S.sb([128, 512], F32, st)
        xt = [S.sb([128, D], F32, st) for _ in range(2)]
        tmp = [S.sb([128, D], F32, st) for _ in range(2)]
        xo = [S.sb([128, D], F32, st) for _ in range(2)]
        ps = [S.ps([128, 512], F32, st) for _ in range(6)]
        pso = [S.ps([128, 512], F32, st) for _ in range(2)]
        oTv = C.oT.rearrange("(c p) t -> p c t", p=128)
        gTv = C.zT[ZGT:ZGT + 3072, :].rearrange("(c p) t -> p c t", p=128)
        cnt = {"ps": 0, "po": 0, "x": 0}

        def branch(tb):
            b = tb % 2
            S.dma("sp", ot[b][:], oTv[:, :, tb * 512:(tb + 1) * 512], writes=[f"ot{b}"])
            S.dma("sp", gt[0][:], gTv[:, :, tb * 512:(tb + 1) * 512], writes=["gt0"])
            for dc in range(8):
                for i in range(3):
                    p = ps[cnt["ps"] % 6]
                    pk = f"psb{cnt['ps'] % 6}"
                    cnt["ps"] += 1
                    for kc in range(4):
                        mm(S, p[:], wb[:, i * 4 + kc, dc * 128:(dc + 1) * 128], ot[b][:, i * 4 + kc, :], start=(kc == 0),
                           stop=(kc == 3), r=["wbo", f"ot{b}"], w=[pk])
                    act(S, sg[i][:], gt[0][:, i * 8 + dc, :], AF.Sigmoid, r=["gt0"], w=[f"sg{i}"])
                    tt(S, "dve", mt[i][:], p[:], sg[i][:], ALU.mult, r=[pk, f"sg{i}"], w=[f"mt{i}"])
                tt(S, "pool", m01[:], mt[0][:], mt[1][:], ALU.add, r=["mt0", "mt1"], w=["m01"])
                tt(S, "pool", mg[b][:, dc, :], m01[:], mt[2][:], ALU.add, r=["m01", "mt2"], w=[f"mg{b}"])

        def wo_part(tb):
            b = tb % 2
            for t4 in range(4):
                xb = cnt["x"] % 2
                cnt["x"] += 1
                tok0 = tb * 512 + t4 * 128
                S.dma("sp", xt[xb][:], x_in[tok0:tok0 + 128, :], writes=[f"xt{xb}"])
                for nb in range(2):
                    p = pso[cnt["po"] % 2]
                    pk = f"pso{cnt['po'] % 2}"
                    cnt["po"] += 1
                    for dc in range(8):
                        mm(S, p[:], mg[b][:, dc, t4 * 128:(t4 + 1) * 128], wo[:, dc, nb * 512:(nb + 1) * 512], start=(dc == 0),
                           stop=(dc == 7), r=["wbo", f"mg{b}"], w=[pk])
                    tt(S, "dve", tmp[xb][:, nb * 512:(nb + 1) * 512], p[:], M.g1[:, nb * 512:(nb + 1) * 512], ALU.mult,
                       r=[pk, "g1bc"], w=[f"tmp{xb}"])
                tt(S, "pool", xo[xb][:], tmp[xb][:], xt[xb][:], ALU.add, r=[f"tmp{xb}", f"xt{xb}"], w=[f"xo{xb}"])
                S.dma("sp", x_out[tok0:tok0 + 128, :], xo[xb][:], reads=[f"xo{xb}"], writes=["xout"], defer=1)

        branch(0)
        for tb in range(8):
            if tb + 1 < 8:
                branch(tb + 1)
            wo_part(tb)
    S.barrier()


def stage_ffn_up(C, l, hT):
    S = C.S
    with ExitStack() as st:
        wca = S.sb([128, 22, 3], F32, st)
        S.dma("sp", wca[:], C.inp["ffn_conv_w"][l].rearrange("(c p) j -> p c j", p=128), writes=["wca"])
        wv = C.inp["ffn_w_up"][l].rearrange("(k p) c -> p k c", p=128)
        wst = [S.sb([128, 8, 256], F32, st) for _ in range(2)]
        wbf = [S.sb([128, 8, 256], BF16, st) for _ in range(2)]
        arow = [S.sb([128, T + 2], F32, st) for _ in range(2)]
        grow = [S.sb([128, T], BF16, st) for _ in range(2)]
        acc = [S.sb([128, T], F32, st)] * 2
        sl = [S.sb([128, T], BF16, st)] * 2
        ub = [S.sb([128, T], BF16, st) for _ in range(2)]
        ps = [S.ps([128, 512], F32, st) for _ in range(4)]
        for b in range(2):
            mset(S, "pool", arow[b][:, 0:2], 0.0, w=[f"arow{b}"])
        nps = 0
        for kc in range(22):
            b = kc % 2
            S.dma("sp", wst[b][:, :, 0:128], wv[:, :, kc * 128:(kc + 1) * 128], writes=[f"wst{b}"])
            S.dma("sp", wst[b][:, :, 128:256], wv[:, :, DFF + kc * 128:DFF + (kc + 1) * 128], writes=[f"wst{b}"])
            cp(S, "pool", wbf[b][:], wst[b][:], r=[f"wst{b}"], w=[f"wbf{b}"])
            for tb in range(8):
                for half in range(2):
                    p = ps[nps % 4]
                    pk = f"psu{nps % 4}"
                    nps += 1
                    for k in range(8):
                        mm(S, p[:], wbf[b][:, k, half * 128:(half + 1) * 128], hT[:, k, tb * 512:(tb + 1) * 512], start=(k == 0),
                           stop=(k == 7), r=[f"wbf{b}"], w=[pk])
                    if half == 0:
                        act(S, arow[b][:, 2 + tb * 512:2 + (tb + 1) * 512], p[:], AF.Copy, r=[pk], w=[f"arow{b}"])
                    else:
                        cp(S, "act", grow[b][:, tb * 512:(tb + 1) * 512], p[:], r=[pk], w=[f"grow{b}"])
            conv3(S, "dve", acc[b], arow[b], wca[:, kc, :], [f"arow{b}", "wca"], "acc0")
            act(S, sl[b][:], acc[b][:], AF.Silu, r=["acc0"], w=["sl0"])
            tt(S, "pool" if kc % 2 == 0 else "dve", ub[b][:], sl[b][:], grow[b][:], ALU.mult, r=["sl0", f"grow{b}"], w=[f"ub{b}"])
            S.dma("sp", C.uT[kc * 128:(kc + 1) * 128, :], ub[b][:], reads=[f"ub{b}"], writes=["uT"], defer=2)
    S.barrier()


def stage_ffn_down(C, l, M, x_in, x_out):
    S = C.S
    with ExitStack() as st:
        wd = S.sb([128, 22, D], BF16, st)
        wst = [S.sb([128, 2, D], F32, st) for _ in range(2)]
        wv = C.inp["ffn_w_down"][l].rearrange("(k p) d -> p k d", p=128)
        for i in range(11):
            b = i % 2
            S.dma("sp", wst[b][:], wv[:, 2 * i:2 * i + 2, :], writes=[f"wst{b}"])
            cp(S, "pool", wd[:, 2 * i:2 * i + 2, :], wst[b][:], r=[f"wst{b}"], w=["wd"])
        ut = [S.sb([128, 22, 512], BF16, st) for _ in range(2)]
        xt = [S.sb([128, D], F32, st) for _ in range(2)]
        tmp = [S.sb([128, D], F32, st) for _ in range(2)]
        xo = [S.sb([128, D], F32, st) for _ in range(2)]
        pso = [S.ps([128, 512], F32, st) for _ in range(4)]
        uTv = C.uT.rearrange("(c p) t -> p c t", p=128)
        npo = 0
        nx = 0
        for tb in range(8):
            b = tb % 2
            S.dma("sp", ut[b][:], uTv[:, :, tb * 512:(tb + 1) * 512], writes=[f"ut{b}"])
            for t4 in range(4):
                xb = nx % 2
                nx += 1
                tok0 = tb * 512 + t4 * 128
                S.dma("sp", xt[xb][:], x_in[tok0:tok0 + 128, :], writes=[f"xt{xb}"])
                for nb in range(2):
                    p = pso[npo % 4]
                    pk = f"pso{npo % 4}"
                    npo += 1
                    for kc in range(22):
                        mm(S, p[:], ut[b][:, kc, t4 * 128:(t4 + 1) * 128], wd[:, kc, nb * 512:(nb + 1) * 512], start=(kc == 0),
                           stop=(kc == 21), r=["wd", f"ut{b}"], w=[pk])
                    tt(S, "dve", tmp[xb][:, nb * 512:(nb + 1) * 512], p[:], M.g2[:, nb * 512:(nb + 1) * 512], ALU.mult,
                       r=[pk, "g2bc"], w=[f"tmp{xb}"])
                tt(S, "pool", xo[xb][:], tmp[xb][:], xt[xb][:], ALU.add, r=[f"tmp{xb}", f"xt{xb}"], w=[f"xo{xb}"])
                S.dma("sp", x_out[tok0:tok0 + 128, :], xo[xb][:], reads=[f"xo{xb}"], writes=["xout"], defer=1)
    S.barrier()


def stage_final(C, x_in, out_ap):
    S = C.S
    K = C.K
    with ExitStack() as st:
        fn = S.sb([128, D], F32, st)
        S.dma("sp", fn[:], C.inp["final_norm"].partition_broadcast(128), writes=["fn"])
        xt = [S.sb([128, D], F32, st) for _ in range(2)]
        junk = S.sb([128, D], BF16, st)
        y1 = [S.sb([128, D], F32, st) for _ in range(2)]
        y2 = [S.sb([128, D], F32, st) for _ in range(2)]
        ss = [S.sb([128, 1], F32, st) for _ in range(2)]
        sd = [S.sb([128, 1], F32, st) for _ in range(2)]
        rs = [S.sb([128, 1], F32, st) for _ in range(2)]
        for i in range(NT):
            b = i % 2
            S.dma("sp", xt[b][:], x_in[i * 128:(i + 1) * 128, :], writes=[f"xt{b}"])
            act(S, junk[:], xt[b][:], AF.Square, r=[f"xt{b}"], w=["junk", f"ss{b}"], accum=ss[b][:])
            act(S, sd[b][:], ss[b][:], AF.Sqrt, r=[f"ss{b}"], w=[f"sd{b}"], bias=K.eps[:], scale=1.0 / D)
            S.add("dve", (lambda o, i_: (lambda e: e.reciprocal(o, i_)))(rs[b][:], sd[b][:]), [f"sd{b}"], [f"rs{b}"])
            ts(S, "dve", y1[b][:], xt[b][:], rs[b][:], ALU.mult, r=[f"xt{b}", f"rs{b}"], w=[f"y1{b}"])
            tt(S, "pool", y2[b][:], y1[b][:], fn[:], ALU.mult, r=[f"y1{b}", "fn"], w=[f"y2{b}"])
            S.dma("sp", out_ap[i * 128:(i + 1) * 128, :], y2[b][:], reads=[f"y2{b}"], writes=["yout"], defer=1)
    S.barrier()


def stage_bias(C):
    S = C.S
    K = C.K
    K.corrD = S.sb([128, 8, 128], BF16, name="corrD")
    K.corrO = S.sb([128, 8, 128], BF16, name="corrO")
    K.rb31 = S.sb([128, 8], F32, name="rb31")
    with ExitStack() as st:
        rb = S.sb([32, 8], F32, st)
        ohb = S.sb([32, 384], F32, st)
        ones32 = S.sb([32, 128], F32, st)
        nrb = S.sb([128, 8], F32, st)
        S.dma("sp", rb[:], C.inp["rel_bias"], writes=["rb"])
        S.dma("sp", ohb[:], C.cin["ohb"], writes=["ohb"])
        S.dma("sp", K.rb31[:], C.inp["rel_bias"][31].partition_broadcast(128), writes=["rb31"])
        mset(S, "pool", ones32[:], 1.0, w=["ones32"])
        ts(S, "dve", nrb[:], K.rb31[:], -1.0, ALU.mult, r=["rb31"], w=["nrb"])
        lh = [S.sb([32, 128], F32, st) for _ in range(2)]
        zs = [S.sb([128, 384], F32, st) for _ in range(2)]
        ps = [S.ps([128, 512], F32, st) for _ in range(2)]
        for h in range(8):
            b = h % 2
            ts(S, "dve", lh[b][:], ones32[:], rb[:, h:h + 1], ALU.mult, r=["ones32", "rb"], w=[f"lh{b}"])
            mm(S, ps[b][:, 0:384], lh[b][:], ohb[:], r=[f"lh{b}", "ohb"], w=[f"psz{b}"])
            cp(S, "dve", zs[b][:], ps[b][:, 0:384], r=[f"psz{b}"], w=[f"zs{b}"])
            S.dma("sp", C.zdT[h], zs[b][:], reads=[f"zs{b}"], writes=["zdT"])
        S.barrier()
        td = [S.sb([128, 128], F32, st) for _ in range(2)]
        n = 0
        for h in range(8):
            for which, off0 in (("D", 127), ("O", 255)):
                b = n % 2
                n += 1
                src = bass.AP(tensor=C.zdT.tensor, offset=h * 128 * 384 + off0, ap=[[383, 128], [1, 128]])
                S.dma("sp", td[b][:], src, writes=[f"td{b}"])
                dst = (K.corrD if which == "D" else K.corrO)[:, h, :]
                act(S, dst, td[b][:], AF.Exp, r=[f"td{b}", "nrb"], w=["corr"], bias=nrb[:, h:h + 1])
    S.barrier()


NIT = 14
_FILL = {}


def _fill_reg(e):
    if id(e) not in _FILL:
        _FILL[id(e)] = e.to_reg(NEG)
    return _FILL[id(e)]


def stage_index(C, l):
    S = C.S
    K = C.K
    with ExitStack() as st:
        qiT = S.sb([128, 4, T], BF16, st)
        kiT = S.sb([128, T], BF16, st)
        wi = S.sb([128, NT, 8], F32, st)
        S.dma("sp", qiT[:], C.zT[ZQI:ZQI + 512, :].rearrange("(c p) t -> p c t", p=128), writes=["qiT"])
        S.dma("sp", kiT[:], C.zT[ZKI:ZKI + 128, :], writes=["kiT"])
        S.dma("sp", wi[:], C.witok.rearrange("(i p) h -> p i h", p=128), writes=["wi"])
        Iacc = [S.sb([128, T], F32, st) for _ in range(2)]
        rl = [S.sb([128, 512], F32, st) for _ in range(3)]
        cmpj = S.sb([128, T], BF16, st)
        mask = [S.sb([128, T], BF16, st) for _ in range(2)]
        mT = [S.sb([128, NT, 128], BF16, st) for _ in range(2)]
        sm = {nm: [S.sb([128, 1], F32, st) for _ in range(2)] for nm in ("rmax", "rmin", "lo", "w", "mid", "cnt", "ge", "sgn", "tot")}
        cmpa = S.sb([128, T], BF16, st)
        pw2 = S.sb([128, NIT], F32, st)
        for k_ in range(NIT):
            mset(S, "pool", pw2[:, k_:k_ + 1], 2.0 ** -(k_ + 1), w=["pw2"])
        wtab = [S.sb([128, NIT], F32, st) for _ in range(2)]
        psI = [S.ps([128, 512], F32, st) for _ in range(3)]
        psT = [S.ps([128, 4, 128], BF16, st) for _ in range(2)]
        off = 0
        n = 0
        ng = 0
        for i in range(NT):
            L = (i + 1) * 128
            b = i % 2
            mk = f"mask{b}"
            if i >= 2:
                nchunk = (L + 511) // 512
                for h in range(8):
                    hp, hc = h % 2, h // 2
                    r0 = hp * 64
                    for ch in range(nchunk):
                        w_ = min(512, L - ch * 512)
                        p = psI[n % 3]
                        pk = f"psI{n % 3}"
                        rt = rl[n % 3]
                        rk = f"rl{n % 3}"
                        n += 1
                        mm(S, p[:, 0:w_], qiT[r0:r0 + 64, hc, i * 128:(i + 1) * 128], kiT[r0:r0 + 64, ch * 512:ch * 512 + w_],
                           r=["qiT", "kiT"], w=[pk])
                        act(S, rt[:, 0:w_], p[:, 0:w_], AF.Relu, r=[pk], w=[rk], scale=0.125)
                        ik = f"I{b}_{ch}"
                        dst = Iacc[b][:, ch * 512:ch * 512 + w_]
                        if h == 0:
                            ts(S, "dve", dst, rt[:, 0:w_], wi[:, i, 0:1], ALU.mult, r=[rk, "wi"], w=[ik])
                        else:
                            stt(S, "dve", dst, rt[:, 0:w_], wi[:, i, h:h + 1], dst, ALU.mult, ALU.add, r=[rk, "wi", ik], w=[ik])
                allI = [f"I{b}_{ch}" for ch in range(nchunk)]
                dg = Iacc[b][:, i * 128:L]
                S.add("pool", (lambda o: (lambda e: e.affine_select(out=o, in_=o, pattern=[[-1, 128]], compare_op=ALU.is_ge,
                                                                    fill=_fill_reg(e), base=0, channel_multiplier=1)))(dg),
                      allI, allI)
                rmax, rmin, lo, wd_, mid, cnt, ge = (sm[nm][b] for nm in ("rmax", "rmin", "lo", "w", "mid", "cnt", "ge"))
                sk = f"sm{b}"
                S.add("dve", (lambda o, a: (lambda e: e.tensor_reduce(out=o, in_=a, axis=AX.X, op=ALU.max)))(rmax[:], Iacc[b][:, 0:L]),
                      allI, [sk + "rmax"])
                S.add("dve", (lambda o, a: (lambda e: e.tensor_reduce(out=o, in_=a, axis=AX.X, op=ALU.min)))(rmin[:], Iacc[b][:, 0:i * 128]),
                      allI, [sk + "rmin"])
                ts(S, "dve", lo[:], rmin[:], -1.0, ALU.add, r=[sk + "rmin"], w=[sk + "lo"])
                tt(S, "dve", wd_[:], rmax[:], lo[:], ALU.subtract, r=[sk + "rmax", sk + "lo"], w=[sk + "w"])
                ts(S, "dve", wtab[b][:], pw2[:], wd_[:], ALU.mult, r=["pw2", sk + "w"], w=[sk + "wtab"])
                stt(S, "dve", mid[:], wd_[:], 0.5, lo[:], ALU.mult, ALU.add, r=[sk + "w", sk + "lo"], w=[sk + "mid"])
                La = ((L // 128) // 2) * 128
                sgn, tot = sm["sgn"][b], sm["tot"][b]
                for it in range(NIT):
                    ts(S, "dve", cmpj[:, 0:La], Iacc[b][:, 0:La], mid[:], ALU.is_gt, op1=ALU.add, r=allI + [sk + "mid"],
                       w=["cmpj", sk + "cnt"], accum=cnt[:])
                    act(S, cmpa[:, La:L], Iacc[b][:, La:L], AF.Sign, r=allI + [sk + "mid"], w=["cmpa", sk + "sgn"], bias=mid[:],
                        scale=-1.0, accum=sgn[:])
                    stt(S, "dve", tot[:], sgn[:], -0.5, cnt[:], ALU.mult, ALU.add, r=[sk + "sgn", sk + "cnt"], w=[sk + "tot"])
                    ts(S, "dve", ge[:], tot[:], 255.5 - 0.5 * (L - La), ALU.is_gt, s2=0.5, op1=ALU.subtract, r=[sk + "tot"],
                       w=[sk + "ge"])
                    stt(S, "dve", mid[:], ge[:], wtab[b][:, it:it + 1], mid[:], ALU.mult, ALU.add,
                        r=[sk + "ge", sk + "wtab", sk + "mid"], w=[sk + "mid"])
                ts(S, "dve", mask[b][:, 0:L], Iacc[b][:, 0:L], mid[:], ALU.is_gt, r=allI + [sk + "mid"], w=[mk])
            elif i == 0:
                cp(S, "dve", mask[b][:, 0:128], K.tri[:], r=["k_tri"], w=[mk])
            else:
                cp(S, "dve", mask[b][:, 0:128], K.ones_bf[:], r=["k_ones"], w=[mk])
                cp(S, "dve", mask[b][:, 128:256], K.tri[:], r=["k_tri"], w=[mk])
            for g in range((i + 4) // 4):
                nb_ = min(4, i + 1 - 4 * g)
                pt = psT[ng % 2]
                ptk = f"psT{ng % 2}"
                ng += 1
                for jj in range(nb_):
                    tr(S, pt[:, jj, :], mask[b][:, (4 * g + jj) * 128:(4 * g + jj + 1) * 128], K.ident_bf[:], r=[mk], w=[ptk])
                cp(S, "act", mT[b][:, 4 * g:4 * g + nb_, :], pt[:, 0:nb_, :], r=[ptk], w=[f"mT{b}"])
            S.dma("sp", C.maskT[:, off * 128:(off + i + 1) * 128].rearrange("p (j q) -> p j q", q=128), mT[b][:, 0:i + 1, :],
                  reads=[f"mT{b}"], writes=["maskT"])
            off += i + 1
    S.barrier()


def stage_attn(C, l):
    S = C.S
    K = C.K
    with ExitStack() as st:
        qT = S.sb([128, 4, T], BF16, st)
        kT = S.sb([128, 4, T], BF16, st)
        V = S.sb([128, NT, 512], BF16, st)
        S.dma("sp", qT[:], C.zT[ZQ:ZQ + 512, :].rearrange("(c p) t -> p c t", p=128), writes=["qT"])
        S.dma("sp", kT[:], C.zT[ZK:ZK + 512, :].rearrange("(c p) t -> p c t", p=128), writes=["kT"])
        S.dma("sp", V[:], C.vtok.rearrange("(j p) c -> p j c", p=128), writes=["V"])
        mk = [S.sb([128, NT, 128], BF16, st) for _ in range(2)]
        E = [S.sb([128, 4, 128], BF16, st) for _ in range(3)]
        P = [S.sb([128, 4, 128], BF16, st) for _ in range(3)]
        rec = [S.sb([128, 128], F32, st) for _ in range(2)]
        ob = [S.sb([128, 4, 128], BF16, st) for _ in range(2)]
        psS = [S.ps([128, 4, 128], F32, st) for _ in range(3)]
        psN = [S.ps([128, 512], F32, st) for _ in range(2)]
        psD = [S.ps([128, 512], F32, st) for _ in range(2)]
        oTv = C.oT[0:512, :].rearrange("(c p) t -> p c t", p=128)
        offs = []
        off = 0
        for i in range(NT):
            offs.append(off)
            off += i + 1

        def load_mask(i):
            b = i % 2
            nblk = i + 1
            S.dma("sp", mk[b][:, 0:nblk, :],
                  C.maskT[:, offs[i] * 128:(offs[i] + nblk) * 128].rearrange("p (j q) -> p j q", q=128), writes=[f"mk{b}"])

        items = []
        for i in range(NT):
            for h in range(8):
                ng_ = (i + 4) // 4
                for g in range(ng_):
                    items.append((i, h, g, g == ng_ - 1))

        def emit_st(n, it):
            i, h, g, _ = it
            hp, hc = h % 2, h // 2
            r0 = hp * 64
            nb_ = min(4, i + 1 - 4 * g)
            ps_ = psS[n % 3]
            for jj in range(nb_):
                j = 4 * g + jj
                mm(S, ps_[:, jj, :], kT[r0:r0 + 64, hc, j * 128:(j + 1) * 128], qT[r0:r0 + 64, hc, i * 128:(i + 1) * 128],
                   r=["qT", "kT"], w=[f"psS{n % 3}"])

        def emit_rest(n, it):
            i, h, g, last = it
            b = i % 2
            hp, hc = h % 2, h // 2
            r0 = hp * 64
            nb_ = min(4, i + 1 - 4 * g)
            ps_ = psS[n % 3]
            psk = f"psS{n % 3}"
            e_, ek = E[n % 3], f"E{n % 3}"
            p_, pk = P[n % 3], f"P{n % 3}"
            pn, pd = psN[hc % 2], psD[hc % 2]
            pnk, pdk = f"psN{hc % 2}", f"psD{hc % 2}"
            act(S, e_[:, 0:nb_, :], ps_[:, 0:nb_, :], AF.Exp, r=[psk, "rb31"], w=[ek], bias=K.rb31[:, h:h + 1], scale=0.125)
            tt(S, "dve", p_[:, 0:nb_, :], e_[:, 0:nb_, :], mk[b][:, 4 * g:4 * g + nb_, :], ALU.mult, r=[ek, f"mk{b}"], w=[pk])
            for jj in range(nb_):
                j = 4 * g + jj
                if j == i:
                    tt(S, "pool", p_[:, jj, :], p_[:, jj, :], K.corrD[:, h, :], ALU.mult, r=[pk, "corr"], w=[pk])
                elif j == i - 1:
                    tt(S, "pool", p_[:, jj, :], p_[:, jj, :], K.corrO[:, h, :], ALU.mult, r=[pk, "corr"], w=[pk])
            for jj in range(nb_):
                j = 4 * g + jj
                mm(S, pn[r0:r0 + 64, 0:128], V[:, j, h * 64:(h + 1) * 64], p_[:, jj, :], start=(j == 0), stop=(j == i),
                   r=["V", pk], w=[pnk])
            mm(S, pd[r0:r0 + 64, 0:nb_ * 128], K.ones_bf[:, 0:64], p_[:].rearrange("p j q -> p (j q)")[:, 0:nb_ * 128],
               start=(g == 0), stop=last, r=["k_ones", pk], w=[pdk])
            if last and hp == 1:
                rc = rec[hc % 2]
                rck = f"rec{hc % 2}"
                nj = min(4, i + 1)
                S.add("dve", (lambda o, a: (lambda e: e.tensor_reduce(out=o, in_=a, axis=AX.X, op=ALU.add)))(
                    rc[:], pd[:, 0:nj * 128].rearrange("p (j q) -> p q j", j=nj)), [pdk], [rck])
                S.add("dve", (lambda o, a: (lambda e: e.reciprocal(o, a)))(rc[:], rc[:]), [rck], [rck])
                tt(S, "dve", ob[b][:, hc, :], pn[:, 0:128], rc[:], ALU.mult, r=[pnk, rck], w=[f"ob{b}"])
            if last and h == 7:
                S.dma("sp", oTv[:, :, i * 128:(i + 1) * 128], ob[b][:], reads=[f"ob{b}"], writes=["oT"])

        load_mask(0)
        load_mask(1)
        emit_st(0, items[0])
        for n, it in enumerate(items):
            if n + 1 < len(items):
                nxt = items[n + 1]
                emit_st(n + 1, nxt)
            emit_rest(n, it)
            i, h, g, last = it
            if last and h == 7 and i + 2 < NT:
                load_mask(i + 2)
    S.barrier()


TB = 1024
NLEV = 4


def stage_rwkv_prep(C, l):
    S = C.S
    K = C.K
    inp = C.inp
    with ExitStack() as st:
        def colp(name, n):
            t_ = S.sb([128, n, 1], F32, st)
            S.dma("sp", t_[:], col_ap(inp[name][l], n), writes=["c_" + name], slow=True)
            return t_
        mu = colp("rwkv_mu", 14)
        w0 = colp("rwkv_w0", 4)
        a0 = colp("rwkv_a0", 4)
        kkc = colp("rwkv_k_k", 4)
        kac = colp("rwkv_k_a", 4)
        rkc = colp("rwkv_r_k", 4)
        omka = S.sb([128, 4, 1], F32, st)
        ts(S, "dve", omka[:], kac[:], -1.0, ALU.mult, s2=1.0, op1=ALU.add, r=["c_rwkv_k_a"], w=["omka"])
        wa_st = S.sb([128, 512], F32, st)
        gu_st = S.sb([128, 512], F32, st)
        wa = S.sb([128, 512], BF16, st)
        gu = S.sb([128, 512], BF16, st)
        S.dma("sp", wa_st[0:64, :], inp["rwkv_w_up"][l], writes=["wa_st"])
        S.dma("sp", wa_st[64:128, :], inp["rwkv_a_up"][l], writes=["wa_st"])
        S.dma("sp", gu_st[:], inp["rwkv_g_up"][l], writes=["gu_st"])
        cp(S, "dve", wa[:], wa_st[:], r=["wa_st"], w=["wa"])
        cp(S, "dve", gu[:], gu_st[:], r=["gu_st"], w=["gu"])
        rmask = S.sb([128, TB], F32, st)
        S.dma("sp", rmask[:], C.cin["rmask"][:, 0:TB], writes=["rmask"])
        gC = S.sb([128, 4, NCH], F32, st)
        xwa = S.sb([128, T], BF16, st)
        sg = S.sb([128, T], BF16, st)
        raw = [S.sb([128, TB + 1], BF16, st) for _ in range(3)]
        F = {nm: S.sb([128, TB], F32, st, name=nm) for nm in
             ("zr", "zk", "zv", "d", "sgw", "af", "lw", "cum", "ex", "epos", "eneg", "eex", "eh", "kx", "nrm", "kk", "t1",
              "kmod", "bq")}
        H = {nm: S.sb([128, TB], BF16, st, name=nm) for nm in
             ("sq", "prod", "g", "RT", "AT", "BT", "KT", "BH", "KH", "VT", "BV")}
        ps = [S.ps([128, 512], F32, st) for _ in range(4)]
        nps = [0]

        def newps():
            i_ = nps[0] % 4
            nps[0] += 1
            return ps[i_], f"psr{i_}"

        def lerp(ci, t0, rawt, rk, out, ok):
            row0 = ZRW + ci * 128
            if t0 == 0:
                mset(S, "pool", rawt[:, 0:1], 0.0, w=[rk])
                S.dma("sp", rawt[:, 1:TB + 1], C.zT[row0:row0 + 128, 0:TB], writes=[rk])
            else:
                S.dma("sp", rawt[:, 0:TB + 1], C.zT[row0:row0 + 128, t0 - 1:t0 + TB], writes=[rk])
            tt(S, "dve", F["d"][:], rawt[:, 0:TB], rawt[:, 1:TB + 1], ALU.subtract, r=[rk], w=["d"])
            stt(S, "dve", out, F["d"][:], mu[:, ci, :], rawt[:, 1:TB + 1], ALU.mult, ALU.add, r=["d", rk, "c_rwkv_mu"], w=[ok])

        for tb in range(T // TB):
            t0 = tb * TB
            lerp(12, t0, raw[0], "raw0", F["zr"][:], "zr")
            act(S, xwa[0:64, t0:t0 + TB], F["zr"][0:64, :], AF.Tanh, r=["zr"], w=["xwa"])
            cp(S, "dve", xwa[64:128, t0:t0 + TB], F["zr"][64:128, :], r=["zr"], w=["xwa"])
            lerp(13, t0, raw[1], "raw1", F["zk"][:], "zk")
            act(S, sg[:, t0:t0 + TB], F["zk"][:], AF.Sigmoid, r=["zk"], w=["sg"])

        for pc in range(4):
            pcs = slice(pc * 128, (pc + 1) * 128)
            for tb in range(T // TB):
                t0 = tb * TB
                lerp(pc, t0, raw[0], "raw0", F["zr"][:], "zr")
                lerp(4 + pc, t0, raw[1], "raw1", F["zk"][:], "zk")
                lerp(8 + pc, t0, raw[2], "raw2", F["zv"][:], "zv")
                for sb in range(TB // 512):
                    c0 = sb * 512
                    tcs = slice(t0 + c0, t0 + c0 + 512)
                    p, pk = newps()
                    mm(S, p[:], wa[0:64, pcs], xwa[0:64, tcs], r=["wa", "xwa"], w=[pk])
                    act(S, F["sgw"][:, c0:c0 + 512], p[:], AF.Sigmoid, r=[pk, "c_rwkv_w0"], w=["sgw"], bias=w0[:, pc, :])
                    p, pk = newps()
                    mm(S, p[:], wa[64:128, pcs], xwa[64:128, tcs], r=["wa", "xwa"], w=[pk])
                    act(S, F["af"][:, c0:c0 + 512], p[:], AF.Sigmoid, r=[pk, "c_rwkv_a0"], w=["af"], bias=a0[:, pc, :])
                    p, pk = newps()
                    mm(S, p[:], gu[:, pcs], sg[:, tcs], r=["gu", "sg"], w=[pk])
                    cp(S, "dve", H["g"][:, c0:c0 + 512], p[:], r=[pk], w=["g"])
                ts(S, "pool", F["lw"][:], F["sgw"][:], -EDEC, ALU.mult, r=["sgw"], w=["lw"])
                S.add("dve", (lambda o, a, b_: (lambda e: e.tensor_tensor_scan(o, a, b_, 0.0, ALU.mult, ALU.add)))(
                    F["cum"][:], rmask[:], F["lw"][:]), ["rmask", "lw"], ["cum"])
                cumv = F["cum"][:].rearrange("p (c t) -> p c t", t=CH)
                act(S, gC[:, pc, tb * (TB // CH):(tb + 1) * (TB // CH)], F["cum"][:, CH - 1:TB:CH], AF.Exp, r=["cum"], w=["gC"])
                act(S, F["epos"][:], F["cum"][:], AF.Exp, r=["cum"], w=["epos"])
                act(S, F["eneg"][:], F["cum"][:], AF.Exp, r=["cum"], w=["eneg"], scale=-1.0)
                tt(S, "pool", F["ex"][:], F["cum"][:], F["lw"][:], ALU.subtract, r=["cum", "lw"], w=["ex"])
                act(S, F["eex"][:], F["ex"][:], AF.Exp, r=["ex"], w=["eex"])
                tt(S, "dve", F["eh"][:].rearrange("p (c t) -> p c t", t=CH), cumv[:, :, CH - 1:CH].broadcast_to([128, TB // CH, CH]),
                   cumv, ALU.subtract, r=["cum"], w=["ehx"])
                act(S, F["eh"][:], F["eh"][:], AF.Exp, r=["ehx"], w=["eh"])
                ts(S, "dve", F["kx"][:], F["zk"][:], kkc[:, pc, :], ALU.mult, r=["zk", "c_rwkv_k_k"], w=["kx"])
                tt(S, "pool", H["sq"][:], F["kx"][:], F["kx"][:], ALU.mult, r=["kx"], w=["sq"])
                for sb in range(TB // 512):
                    c0 = sb * 512
                    p, pk = newps()
                    mm(S, p[:], K.blk_bf[:], H["sq"][:, c0:c0 + 512], r=["k_blk_bf", "sq"], w=[pk])
                    act(S, F["nrm"][:, c0:c0 + 512], p[:], AF.Sqrt, r=[pk], w=["nrm"])
                ts(S, "dve", F["nrm"][:], F["nrm"][:], 1e-12, ALU.max, r=["nrm"], w=["nrm"])
                S.add("dve", (lambda o, a: (lambda e: e.reciprocal(o, a)))(F["nrm"][:], F["nrm"][:]), ["nrm"], ["nrm"])
                tt(S, "dve", F["kk"][:], F["kx"][:], F["nrm"][:], ALU.mult, r=["kx", "nrm"], w=["kk"])
                ts(S, "dve", F["t1"][:], F["af"][:], kac[:, pc, :], ALU.mult, s2=omka[:, pc, :], op1=ALU.add,
                   r=["af", "c_rwkv_k_a", "omka"], w=["t1"])
                tt(S, "pool", F["kmod"][:], F["zk"][:], F["t1"][:], ALU.mult, r=["zk", "t1"], w=["kmod"])
                tt(S, "pool", F["bq"][:], F["kk"][:], F["af"][:], ALU.mult, r=["kk", "af"], w=["bq"])
                tt(S, "pool", H["RT"][:], F["zr"][:], F["epos"][:], ALU.mult, r=["zr", "epos"], w=["RT"])
                stt(S, "dve", H["AT"][:], F["kk"][:], -1.0, F["eex"][:], ALU.mult, ALU.mult, r=["kk", "eex"], w=["AT"])
                tt(S, "pool", H["BT"][:], F["bq"][:], F["eneg"][:], ALU.mult, r=["bq", "eneg"], w=["BT"])
                tt(S, "pool", H["KT"][:], F["kmod"][:], F["eneg"][:], ALU.mult, r=["kmod", "eneg"], w=["KT"])
                tt(S, "pool", H["BH"][:], F["bq"][:], F["eh"][:], ALU.mult, r=["bq", "eh"], w=["BH"])
                tt(S, "pool", H["KH"][:], F["kmod"][:], F["eh"][:], ALU.mult, r=["kmod", "eh"], w=["KH"])
                cp(S, "act", H["VT"][:], F["zv"][:], r=["zv"], w=["VT"])
                stt(S, "dve", H["prod"][:], F["zr"][:], rkc[:, pc, :], F["kmod"][:], ALU.mult, ALU.mult,
                    r=["zr", "kmod", "c_rwkv_r_k"], w=["prod"])
                for sb in range(TB // 512):
                    c0 = sb * 512
                    p, pk = newps()
                    mm(S, p[:], K.blk_bf[:], H["prod"][:, c0:c0 + 512], r=["k_blk_bf", "prod"], w=[pk])
                    tt(S, "dve", H["BV"][:, c0:c0 + 512], p[:], F["zv"][:, c0:c0 + 512], ALU.mult, r=[pk, "zv"], w=["BV"])
                for nm, dst in (("RT", C.rRT), ("AT", C.rAT), ("BT", C.rBT), ("KT", C.rKT), ("BH", C.rBH), ("KH", C.rKH),
                                ("VT", C.rVT), ("BV", C.rBV), ("g", C.rG)):
                    S.dma("sp", dst[pcs, t0:t0 + TB], H[nm][:], reads=[nm], writes=["d_" + nm], defer=3)
        S.dma("sp", C.rGC.rearrange("p (a c) -> p a c", a=4), gC[:], reads=["gC"], writes=["rGC"])
    S.barrier()


def stage_rwkv_scan(C, l, limit=None):
    S = C.S
    K = C.K
    with ExitStack() as st:
        gC = S.sb([128, 4, NCH], F32, st)
        S.dma("sp", gC[:], C.rGC.rearrange("p (a c) -> p a c", a=4), writes=["gC"])
        Sf = S.sb([128, 4, CH], F32, st)
        Sb = S.sb([128, 4, CH], BF16, st)
        mset(S, "pool", Sf[:], 0.0, w=["Sf"])
        mset(S, "pool", Sb[:], 0.0, w=["Sb"])
        names = ("BT", "KT", "BH", "KH", "VT")
        srcs = dict(RT=C.rRT, AT=C.rAT, BT=C.rBT, KT=C.rKT, BH=C.rBH, KH=C.rKH, VT=C.rVT)
        G = {nm: [S.sb([128, 4, 512], BF16, st) for _ in range(2)] for nm in names}
        GM = {nm: [[S.sb([128, 4, 512], BF16, st) for _ in range(2)] for _hp in range(2)] for nm in ("AT", "RT")}
        for nm in ("AT", "RT"):
            for hp_ in range(2):
                for gb_ in range(2):
                    mset(S, "pool", GM[nm][hp_][gb_][:], 0.0, w=[f"GM{nm}{hp_}{gb_}"])
        yg = [S.sb([128, 4, 512], F32, st) for _ in range(2)]
        tokt = {nm: [S.sb([64, 512], BF16, st) for _ in range(2)] for nm in ("BH", "KH", "VT")}
        Xt = [S.sb([64, 512], BF16, st) for _ in range(2)]
        Yt = [S.sb([64, 512], BF16, st) for _ in range(2)]
        Pm = [[S.sb([64, 512], BF16, st) for _ in range(2)] for _par in range(2)]
        Lak = [S.sb([64, 512], BF16, st) for _ in range(2)]
        Mrb = [S.sb([64, 512], BF16, st) for _ in range(2)]
        Mrk = [S.sb([64, 512], BF16, st) for _ in range(2)]
        Wt = S.sb([64, 512], BF16, st)
        Ut = S.sb([64, 512], BF16, st)
        psp = [S.ps([128, 512], F32, st) for _ in range(5)]
        pss = [S.ps([128, 512], F32, st) for _ in range(3)]
        npp = [0]
        nss = [0]

        def newps():
            i_ = npp[0] % 5
            npp[0] += 1
            return psp[i_], f"psp{i_}"

        def newss():
            i_ = nss[0] % 3
            nss[0] += 1
            return pss[i_], f"pss{i_}"

        def load_group(g):
            gb = g % 2
            for nm in names:
                S.dma("sp", G[nm][gb][:], srcs[nm].rearrange("(c p) t -> p c t", p=128)[:, :, g * 512:(g + 1) * 512],
                      writes=[f"G{nm}{gb}"])
            for nm in ("AT", "RT"):
                for hp_ in range(2):
                    rs_ = slice(hp_ * 64, hp_ * 64 + 64)
                    S.dma("sp", GM[nm][hp_][gb][rs_, :, :],
                          srcs[nm].rearrange("(c p) t -> p c t", p=128)[rs_, :, g * 512:(g + 1) * 512],
                          writes=[f"GM{nm}{hp_}{gb}"])

        SU = K.m64[:, 0, :]
        SL = K.m64[:, 1, :]
        UI = K.m64[:, 2, :]
        I64 = K.m64[:, 3, :]
        nchunks = NCH if limit is None else limit
        fin = {}

        def prep(c):
            g, ci = c // 8, c % 8
            gb = g % 2
            par = c % 2
            tsl = slice(ci * CH, (ci + 1) * CH)
            gk = lambda nm: f"G{nm}{gb}"
            for nm in ("BH", "KH", "VT"):
                ptr_, pk = newps()
                pv = ptr_[:].bitcast(BF16)
                for pc in range(4):
                    tr(S, pv[0:64, pc * 128:(pc + 1) * 128], G[nm][gb][:, pc, tsl], K.ident_bf[:], r=[gk(nm)], w=[pk])
                cp(S, "act", tokt[nm][par][:], pv[0:64, 0:512], r=[pk], w=[f"tok{nm}{par}"])
            pX, pXk = newps()
            pY, pYk = newps()
            for h in range(8):
                hp, pc = h % 2, h // 2
                hc = slice(h * 64, (h + 1) * 64)
                A = GM["AT"][hp][gb][:, pc, tsl]
                Bt = G["BT"][gb][:, pc, tsl]
                ak = f"GMAT{hp}{gb}"
                mm(S, pX[0:64, hc], Bt, A, r=[gk("BT"), ak], w=[pXk])
                mm(S, pY[0:64, hc], A, Bt, r=[gk("BT"), ak], w=[pYk])
            X, Xk = Xt[0], "X0"
            Y, Yk = Yt[0], "Y0"
            tt(S, "dve", X[:], pX[0:64, :], SU, ALU.mult, r=[pXk, "k_m64"], w=[Xk])
            tt(S, "dve", Y[:], pY[0:64, :], SL, ALU.mult, r=[pYk, "k_m64"], w=[Yk])
            pL, pLk = newps()
            pRB, pRBk = newps()
            pRK, pRKk = newps()
            for h in range(8):
                hp, pc = h % 2, h // 2
                hc = slice(h * 64, (h + 1) * 64)
                A = GM["AT"][hp][gb][:, pc, tsl]
                Bt = G["BT"][gb][:, pc, tsl]
                Kt = G["KT"][gb][:, pc, tsl]
                R = GM["RT"][hp][gb][:, pc, tsl]
                ak = f"GMAT{hp}{gb}"
                rk_ = f"GMRT{hp}{gb}"
                mm(S, pL[0:64, hc], Kt, A, r=[gk("KT"), ak], w=[pLk])
                mm(S, pRB[0:64, hc], Bt, R, r=[gk("BT"), rk_], w=[pRBk])
                mm(S, pRK[0:64, hc], Kt, R, r=[gk("KT"), rk_], w=[pRKk])
            tt(S, "dve", Lak[par][:], pL[0:64, :], SU, ALU.mult, r=[pLk, "k_m64"], w=[f"Lak{par}"])
            tt(S, "dve", Mrb[par][:], pRB[0:64, :], UI, ALU.mult, r=[pRBk, "k_m64"], w=[f"Mrb{par}"])
            tt(S, "dve", Mrk[par][:], pRK[0:64, :], UI, ALU.mult, r=[pRKk, "k_m64"], w=[f"Mrk{par}"])
            P_, Pk = Pm[par][0], f"P{par}0"
            tt(S, "pool", P_[:], X[:], I64, ALU.add, r=[Xk, "k_m64"], w=[Pk])
            for k in range(1, NLEV + 1):
                nb = k % 2
                pYn, pYnk = newps()
                for h in range(8):
                    hc = slice(h * 64, (h + 1) * 64)
                    mm(S, pYn[0:64, hc], X[:, hc], Y[:, hc], r=[Xk, Yk], w=[pYnk])
                if k < NLEV:
                    pXn, pXnk = newps()
                    for h in range(8):
                        hc = slice(h * 64, (h + 1) * 64)
                        mm(S, pXn[0:64, hc], Y[:, hc], X[:, hc], r=[Xk, Yk], w=[pXnk])
                Yn, Ynk = Yt[nb], f"Y{nb}"
                cp(S, "act", Yn[:], pYn[0:64, :], r=[pYnk], w=[Ynk])
                if k < NLEV:
                    Xn, Xnk = Xt[nb], f"X{nb}"
                    cp(S, "dve", Xn[:], pXn[0:64, :], r=[pXnk], w=[Xnk])
                pP, pPk = newps()
                for h in range(8):
                    hc = slice(h * 64, (h + 1) * 64)
                    mm(S, pP[0:64, hc], Yn[:, hc], P_[:, hc], r=[Ynk, Pk], w=[pPk])
                Pn, Pnk = Pm[par][nb], f"P{par}{nb}"
                tt(S, "dve", Pn[:], pP[0:64, :], P_[:], ALU.add, r=[pPk, Pk], w=[Pnk])
                P_, Pk = Pn, Pnk
                Y, Yk = Yn, Ynk
                if k < NLEV:
                    X, Xk = Xn, Xnk
            fin[c] = (P_, Pk)

        def seq(c):
            g, ci = c // 8, c % 8
            gb = g % 2
            par = c % 2
            tsl = slice(ci * CH, (ci + 1) * CH)
            P_, Pk = fin[c]
            BHt, BHk = tokt["BH"][par], f"tokBH{par}"
            KHt, KHk = tokt["KH"][par], f"tokKH{par}"
            Vt, Vk = tokt["VT"][par], f"tokVT{par}"
            pW, pWk = newss()
            for h in range(8):
                hp, pc = h % 2, h // 2
                hc = slice(h * 64, (h + 1) * 64)
                mm(S, pW[0:64, hc], Lak[par][:, hc], Vt[:, hc], start=True, stop=False, r=[f"Lak{par}", Vk], w=[pWk])
                mm(S, pW[0:64, hc], GM["AT"][hp][gb][:, pc, tsl], Sb[:, pc, :], start=False, stop=True,
                   r=[f"GMAT{hp}{gb}", "Sb"], w=[pWk])
            cp(S, "act", Wt[:], pW[0:64, :], r=[pWk], w=["Wt"])
            pU, pUk = newss()
            for h in range(8):
                hc = slice(h * 64, (h + 1) * 64)
                mm(S, pU[0:64, hc], P_[:, hc], Wt[:, hc], r=[Pk, "Wt"], w=[pUk])
            cp(S, "act", Ut[:], pU[0:64, :], r=[pUk], w=["Ut"])
            pYT, pYTk = newss()
            pSn, pSnk = newss()
            for h in range(8):
                hp, pc = h % 2, h // 2
                r0 = hp * 64
                hc = slice(h * 64, (h + 1) * 64)
                oc = slice(pc * 64, (pc + 1) * 64)
                mm(S, pSn[r0:r0 + 64, oc], BHt[:, hc], Ut[:, hc], start=True, stop=False, r=[BHk, "Ut"], w=[pSnk])
                mm(S, pSn[r0:r0 + 64, oc], KHt[:, hc], Vt[:, hc], start=False, stop=True, r=[KHk, Vk], w=[pSnk])
            for h in range(8):
                hp, pc = h % 2, h // 2
                r0 = hp * 64
                hc = slice(h * 64, (h + 1) * 64)
                oc = slice(pc * 64, (pc + 1) * 64)
                mm(S, pYT[r0:r0 + 64, oc], Sb[:, pc, :], GM["RT"][hp][gb][:, pc, tsl], start=True, stop=False,
                   r=["Sb", f"GMRT{hp}{gb}"], w=[pYTk])
                mm(S, pYT[r0:r0 + 64, oc], Ut[:, hc], Mrb[par][:, hc], start=False, stop=False, r=["Ut", f"Mrb{par}"], w=[pYTk])
                mm(S, pYT[r0:r0 + 64, oc], Vt[:, hc], Mrk[par][:, hc], start=False, stop=True, r=[Vk, f"Mrk{par}"], w=[pYTk])
            for pc in range(4):
                stt(S, "dve", Sf[:, pc, :], Sf[:, pc, :], gC[:, pc, c:c + 1], pSn[:, pc * 64:(pc + 1) * 64], ALU.mult, ALU.add,
                    r=["Sf", "gC", pSnk], w=["Sf"])
            cp(S, "pool", Sb[:], Sf[:], r=["Sf"], w=["Sb"])
            cp(S, "act", yg[gb][:, :, tsl], pYT[:, 0:256].rearrange("p (a t) -> p a t", t=CH), r=[pYTk], w=[f"yg{gb}"])
            if ci == 7:
                S.dma("sp", C.rYT.rearrange("(c p) t -> p c t", p=128)[:, :, g * 512:(g + 1) * 512], yg[gb][:],
                      reads=[f"yg{gb}"], writes=["rYT"])

        def record(fn):
            lst = []
            S.add = lambda *a, **k: lst.append((a, k))
            try:
                fn()
            finally:
                del S.add
            return lst

        def replay(lst):
            for a, k in lst:
                S.add(*a, **k)

        load_group(0)
        replay(record(lambda: prep(0)))
        for c in range(nchunks):
            if c % 8 == 0 and c // 8 + 1 < NCH // 8:
                load_group(c // 8 + 1)
            ls = record(lambda: seq(c))
            lp = record(lambda: prep(c + 1)) if c + 1 < nchunks else []
            merged = []
            ns_, np_ = len(ls), len(lp)
            ip = 0
            for i_s, op_ in enumerate(ls):
                tgt = (i_s * np_) // max(ns_, 1)
                while ip < tgt:
                    merged.append(lp[ip])
                    ip += 1
                merged.append(op_)
            merged.extend(lp[ip:])
            replay(merged)
    S.barrier()


def stage_rwkv_post(C, l):
    S = C.S
    K = C.K
    inp = C.inp
    PB = 2048
    with ExitStack() as st:
        lnw = S.sb([128, 4, 1], F32, st)
        lnb = S.sb([128, 4, 1], F32, st)
        S.dma("sp", lnw[:], col_ap(inp["rwkv_ln_w"][l], 4), writes=["lnw"], slow=True)
        S.dma("sp", lnb[:], col_ap(inp["rwkv_ln_b"][l], 4), writes=["lnb"], slow=True)
        y = [S.sb([128, PB], F32, st) for _ in range(2)]
        bv = [S.sb([128, PB], BF16, st) for _ in range(2)]
        gg = [S.sb([128, PB], BF16, st) for _ in range(2)]
        cent = S.sb([128, PB], F32, st)
        sq = S.sb([128, PB], F32, st)
        sd = S.sb([128, PB], F32, st)
        ob = [S.sb([128, PB], BF16, st) for _ in range(2)]
        ps = [S.ps([128, 512], F32, st) for _ in range(4)]
        n = 0
        it = 0
        for pc in range(4):
            pcs = slice(pc * 128, (pc + 1) * 128)
            for blk in range(T // PB):
                b = it % 2
                it += 1
                t0 = blk * PB
                S.dma("sp", y[b][:], C.rYT[pcs, t0:t0 + PB], writes=[f"y{b}"])
                S.dma("sp", bv[b][:], C.rBV[pcs, t0:t0 + PB], writes=[f"bv{b}"])
                S.dma("sp", gg[b][:], C.rG[pcs, t0:t0 + PB], writes=[f"gg{b}"])
                for sb in range(PB // 512):
                    cs = slice(sb * 512, (sb + 1) * 512)
                    p, pk = ps[n % 4], f"psq{n % 4}"
                    n += 1
                    mm(S, p[:], K.blkmean_f[:], y[b][:, cs], r=["k_blkmean_f", f"y{b}"], w=[pk])
                    tt(S, "dve", cent[:, cs], y[b][:, cs], p[:], ALU.subtract, r=[pk, f"y{b}"], w=["cent"])
                    tt(S, "pool", sq[:, cs], cent[:, cs], cent[:, cs], ALU.mult, r=["cent"], w=["sq"])
                    p, pk = ps[n % 4], f"psq{n % 4}"
                    n += 1
                    mm(S, p[:], K.blkmean_f[:], sq[:, cs], r=["k_blkmean_f", "sq"], w=[pk])
                    act(S, sd[:, cs], p[:], AF.Sqrt, r=[pk], w=["sd"], bias=K.eps_gn[:])
                S.add("dve", (lambda o, a: (lambda e: e.reciprocal(o, a)))(sd[:], sd[:]), ["sd"], ["sd"])
                tt(S, "dve", cent[:], cent[:], sd[:], ALU.mult, r=["cent", "sd"], w=["cent"])
                ts(S, "dve", cent[:], cent[:], lnw[:, pc, :], ALU.mult, s2=lnb[:, pc, :], op1=ALU.add, r=["cent", "lnw", "lnb"],
                   w=["cent"])
                tt(S, "pool", cent[:], cent[:], bv[b][:], ALU.add, r=["cent", f"bv{b}"], w=["cent"])
                tt(S, "pool", ob[b][:], cent[:], gg[b][:], ALU.mult, r=["cent", f"gg{b}"], w=[f"ob{b}"])
                S.dma("sp", C.oT[512 + pc * 128:512 + (pc + 1) * 128, t0:t0 + PB], ob[b][:], reads=[f"ob{b}"], writes=["oT"], defer=3)
    S.barrier()


def build_layer(C, l, x_in, x_mid, x_out, stages=None):
    S = C.S
    stage_mod(C, l)
    with ExitStack() as lst:
        M = load_mod(C, l, lst)
        with ExitStack() as hst:
            hT = S.sb([128, 8, T], BF16, hst)
            stage_norm(C, x_in, M.A1, M.modcol[:, 0:8, :], hT, hst)
            stage_inproj(C, l, hT)
        stage_index(C, l)
        stage_attn(C, l)
        stage_rwkv_prep(C, l)
        stage_rwkv_scan(C, l)
        stage_rwkv_post(C, l)
        stage_conv(C, l)
        stage_merge(C, l, M, x_in, x_mid)
        with ExitStack() as hst:
            hT = S.sb([128, 8, T], BF16, hst)
            stage_norm(C, x_mid, M.A2, M.modcol[:, 24:32, :], hT, hst)
            stage_ffn_up(C, l, hT)
        stage_ffn_down(C, l, M, x_mid, x_out)


def build_program(debug_out=()):
    nc = bass.Bass("TRN2", target_bir_lowering=False)
    out = nc.dram_tensor("out", [T, D], F32, kind="ExternalOutput").ap()
    with ExitStack() as es:
        C = make_ctx(nc, es, debug_out)
        stage_consts(C)
        stage_bias(C)
        x = C.inp["x"]
        for l in range(DEPTH):
            build_layer(C, l, x, C.xs[2 * l], C.xs[2 * l + 1])
            x = C.xs[2 * l + 1]
        stage_final(C, x, out)
        C.S.emit()
        stats = C.S.stats
    return nc, stats


_PROG = None


def kernel(**inputs):
    global _PROG
    pos = np.asarray(inputs["positions"])
    if not np.array_equal(pos, np.broadcast_to(np.arange(T, dtype=pos.dtype), pos.shape)):
        raise NotImplementedError("kernel is specialised (at build time) for positions == arange(SEQ)")
    if _PROG is None:
        _PROG = build_program()[0]
    nb = np.asarray(inputs["x"]).shape[0]
    in_maps = [host_inputs(inputs, b) for b in range(nb)]
    res = run_bass_kernel_spmd(_PROG, in_maps, core_ids=list(range(nb)))
    return np.stack([np.asarray(r["out"], dtype=np.float32) for r in res.results], axis=0)
```

```python
import math
from contextlib import ExitStack

import numpy as np
import ml_dtypes
import concourse.bass as bass
import concourse.mybir as mybir
from concourse.bass_utils import run_bass_kernel_spmd

F32 = mybir.dt.float32
BF16 = mybir.dt.bfloat16
AF = mybir.ActivationFunctionType
ALU = mybir.AluOpType
AX = mybir.AxisListType

T = 4096
D = 1024
NT = 32
DEPTH = 2
IN_COLS = 8520
DFF = 2816
NEG = -1.0e30
CH = 64
NCH = T // CH
EDEC = math.exp(-0.5)

COMPUTE = ("pe", "act", "dve", "pool")
QUEUES = ("sp", "act", "pool")
NDSEM = 12


class Sched:
    def __init__(self, nc, es):
        self.nc = nc
        self.es = es
        self.ops = []
        self.last_w = {}
        self.readers = {}
        self.nbuf = 0
        self.gen = 0
        self.pending = []

    def sb(self, shape, dt, es=None, name="sb"):
        self.nbuf += 1
        return (es or self.es).enter_context(self.nc.sbuf_tensor(f"{name}_{self.nbuf}", list(shape), dt))

    def ps(self, shape, dt, es=None, name="ps"):
        self.nbuf += 1
        return (es or self.es).enter_context(self.nc.psum_tensor(f"{name}_{self.nbuf}", list(shape), dt))

    def barrier(self):
        for p in list(self.pending):
            self._flush(p)
        self.gen += 1
        self.last_w = {}
        self.readers = {}

    def add(self, eng, fn, reads=(), writes=(), dma=False):
        if self.pending:
            ws = set(writes)
            rs = set(reads)
            for p in list(self.pending):
                _, (_q, _fn, prd, pwr) = p
                if ws.intersection(prd) or ws.intersection(pwr) or rs.intersection(pwr):
                    self._flush(p)
        idx = len(self.ops)
        deps = set()
        for k in reads:
            j = self.last_w.get(k)
            if j is not None:
                deps.add(j)
        for k in writes:
            j = self.last_w.get(k)
            if j is not None:
                deps.add(j)
            for r in self.readers.get(k, ()):
                deps.add(r)
        for k in writes:
            self.last_w[k] = idx
            self.readers[k] = []
        for k in reads:
            lst = self.readers.setdefault(k, [])
            if not dma:
                lst[:] = [r for r in lst if not (self.ops[r]["eng"] == eng and not self.ops[r]["dma"])]
            lst.append(idx)
        deps.discard(idx)
        self.ops.append(dict(eng=eng, fn=fn, deps=deps, dma=dma, gen=self.gen))
        return idx

    def dma(self, q, out, in_, reads=(), writes=(), slow=False, defer=0):
        kw = {"allow_slow_non_contiguous": True} if slow else {}
        fn = lambda e: e.dma_start(out=out, in_=in_, **kw)
        if defer > 0:
            self.pending.append([defer, (q, fn, tuple(reads), tuple(writes))])
            return None
        idx = self.add(q, fn, reads, writes, dma=True)
        for p in list(self.pending):
            p[0] -= 1
            if p[0] <= 0:
                self._flush(p)
        return idx

    def _flush(self, p):
        if p in self.pending:
            self.pending.remove(p)
            q, fn, reads, writes = p[1]
            self.add(q, fn, reads, writes, dma=True)

    def emit(self):
        nc = self.nc
        es = self.es
        csem = {e: es.enter_context(nc.semaphore(f"c_{e}")) for e in COMPUTE}
        dsem = {q: [es.enter_context(nc.semaphore(f"d_{q}{i}")) for i in range(NDSEM)] for q in QUEUES}
        needed = [False] * len(self.ops)
        last_in_gen = {}
        for i, op in enumerate(self.ops):
            if op["dma"]:
                needed[i] = True
            else:
                last_in_gen[(op["eng"], op["gen"])] = i
            for j in op["deps"]:
                oj = self.ops[j]
                if (not oj["dma"]) and (not op["dma"]) and oj["eng"] == "pe" and op["eng"] == "pe":
                    continue
                needed[j] = True
        for i in last_in_gen.values():
            needed[i] = True
        self.needed = needed
        ccount = {e: 0 for e in COMPUTE}
        dcount = {q: 0 for q in QUEUES}
        dval = {q: [0] * NDSEM for q in QUEUES}
        done = []
        snaps = {}
        cur_gen = 0
        for i, op in enumerate(self.ops):
            if op["gen"] != cur_gen:
                cur_gen = op["gen"]
                snaps[cur_gen] = (dict(ccount), {q: list(v) for q, v in dval.items()})
            if op["dma"]:
                q = op["eng"]
                n = dcount[q]
                dcount[q] += 1
                k = n % NDSEM
                s = dsem[q][k]
                op["prev"] = (s, dval[q][k]) if dval[q][k] > 0 else None
                dval[q][k] += 16
                done.append((s, dval[q][k]))
            else:
                e = op["eng"]
                if needed[i]:
                    ccount[e] += 1
                done.append((csem[e], ccount[e]))
        streams = {e: [] for e in ("pe", "act", "dve", "pool", "sp")}
        for i, op in enumerate(self.ops):
            streams[op["eng"]].append(i)
        self.stats = {e: len(v) for e, v in streams.items()}
        self.stats["signaled"] = sum(needed)

        def run_stream(ename, eng):
            waited = {}
            my_gen = 0

            def do_wait(s, v):
                if v <= 0 or waited.get(id(s), 0) >= v:
                    return
                waited[id(s)] = v
                eng.wait_ge(s, v)

            for i in streams[ename]:
                op = self.ops[i]
                if op["gen"] != my_gen:
                    my_gen = op["gen"]
                    cc, dv = snaps[my_gen]
                    for e in COMPUTE:
                        if e != ename or e != "pe":
                            do_wait(csem[e], cc[e])
                    for q in QUEUES:
                        for k in range(NDSEM):
                            do_wait(dsem[q][k], dv[q][k])
                waits = []
                for j in sorted(op["deps"]):
                    oj = self.ops[j]
                    if (not oj["dma"]) and oj["eng"] == ename and ename == "pe" and not op["dma"]:
                        continue
                    waits.append(done[j])
                if op["dma"] and op["prev"] is not None:
                    waits.append(op["prev"])
                for s, v in waits:
                    do_wait(s, v)
                ins = op["fn"](eng)
                s, v = done[i]
                if needed[i]:
                    ins.then_inc(s, 16 if op["dma"] else 1)
            if ename in QUEUES:
                for k in range(NDSEM):
                    if dval[ename][k] > 0:
                        eng.wait_ge(dsem[ename][k], dval[ename][k])

        with nc.Block() as block:
            @block.tensor
            def _(eng):
                run_stream("pe", eng)

            @block.scalar
            def _(eng):
                run_stream("act", eng)

            @block.vector
            def _(eng):
                run_stream("dve", eng)

            @block.gpsimd
            def _(eng):
                run_stream("pool", eng)

            @block.sync
            def _(eng):
                run_stream("sp", eng)


def mm(S, out, lhsT, rhs, start=True, stop=True, r=(), w=()):
    S.add("pe", lambda e: e.matmul(out, lhsT, rhs, start=start, stop=stop), r, w)


def tr(S, out, in_, ident, r=(), w=()):
    S.add("pe", lambda e: e.transpose(out, in_, ident), r, w)


def act(S, out, in_, func, r=(), w=(), bias=None, scale=None, accum=None):
    kw = {}
    if bias is not None:
        kw["bias"] = bias
    if scale is not None:
        kw["scale"] = scale
    if accum is not None:
        kw["accum_out"] = accum
    S.add("act", lambda e: e.activation(out, in_, func, **kw), r, w)


def ts(S, eng, out, in0, s1, op0, s2=None, op1=None, r=(), w=(), accum=None):
    kw = {}
    if op1 is not None:
        kw["op1"] = op1
    if accum is not None:
        kw["accum_out"] = accum
    S.add(eng, lambda e: e.tensor_scalar(out, in0, s1, s2, op0, **kw), r, w)


def tt(S, eng, out, in0, in1, op, r=(), w=()):
    S.add(eng, lambda e: e.tensor_tensor(out, in0, in1, op), r, w)


def stt(S, eng, out, in0, scalar, in1, op0, op1, r=(), w=()):
    S.add(eng, lambda e: e.scalar_tensor_tensor(out, in0, scalar, in1, op0, op1), r, w)


def cp(S, eng, out, in_, r=(), w=()):
    if eng == "act":
        S.add("act", lambda e: e.activation(out, in_, AF.Copy), r, w)
    else:
        S.add(eng, lambda e: e.tensor_copy(out, in_), r, w)


def mset(S, eng, ap, val, w=()):
    S.add(eng, lambda e: e.memset(ap, val), (), w)


ZQ, ZK, ZQI, ZKI, ZRW, ZCV, ZGT, ZROWS = 0, 512, 1024, 1536, 1664, 3456, 4992, 8064
WQ, WK, WV, WQI, WKI, WWI, WRW, WCV, WGT = 0, 512, 1024, 1536, 2048, 2112, 2120, 3912, 5448


def t5_bucket_np(n):
    n = np.maximum(n, 0)
    nf = np.maximum(n, 1).astype(np.float32)
    large = 16 + (np.log(nf / np.float32(16)) / np.float32(math.log(128 / 16)) * np.float32(16)).astype(np.int32)
    large = np.minimum(large, 31)
    return np.where(n < 16, n, large)


def make_consts():
    bf = ml_dtypes.bfloat16
    c = {}
    c["ident_bf"] = np.eye(128, dtype=np.float32).astype(bf)
    c["ident_f"] = np.eye(128, dtype=np.float32)
    q = np.arange(128)[:, None]
    s = np.arange(128)[None, :]
    c["tri"] = (s <= q).astype(np.float32).astype(bf)
    blk = np.zeros((128, 128), np.float32)
    blk[:64, :64] = 1
    blk[64:, 64:] = 1
    c["blk_bf"] = blk.astype(bf)
    c["blkmean_f"] = (blk / 64.0).astype(np.float32)
    rm = np.ones((128, T), np.float32)
    rm[:, ::CH] = 0
    c["rmask"] = rm
    p = np.arange(64)[:, None]
    f = np.arange(64)[None, :]
    su = (p < f).astype(np.float32)
    sl = (f < p).astype(np.float32)
    ui = (p <= f).astype(np.float32)
    i64 = np.eye(64, dtype=np.float32)
    c["m64"] = np.stack([np.tile(m, (1, 8)) for m in (su, sl, ui, i64)], axis=1).astype(np.float32)
    u = np.arange(384)
    bk = t5_bucket_np(u - 127)
    oh = np.zeros((32, 384), np.float32)
    oh[bk, u] = 1.0
    oh[:, :127] = 0.0
    c["ohb"] = oh
    return c


CONST_SPECS = [("ident_bf", [128, 128], BF16), ("ident_f", [128, 128], F32), ("tri", [128, 128], BF16),
               ("blk_bf", [128, 128], BF16), ("blkmean_f", [128, 128], F32), ("rmask", [128, T], F32),
               ("m64", [64, 4, 512], F32), ("ohb", [32, 384], F32)]

INPUT_SPECS = [
    ("x", [T, D]), ("c", [D]), ("rel_bias", [32, 8]), ("final_norm", [D]),
    ("ada_w", [DEPTH, D, 6 * D]), ("ada_b", [DEPTH, 6 * D]), ("norm_mix", [DEPTH, D]),
    ("w_in", [DEPTH, D, IN_COLS]), ("rwkv_mu", [DEPTH, 1792]), ("rwkv_w0", [DEPTH, 512]),
    ("rwkv_w_up", [DEPTH, 64, 512]), ("rwkv_a0", [DEPTH, 512]), ("rwkv_a_up", [DEPTH, 64, 512]),
    ("rwkv_g_up", [DEPTH, 128, 512]), ("rwkv_k_k", [DEPTH, 512]), ("rwkv_k_a", [DEPTH, 512]),
    ("rwkv_r_k", [DEPTH, 512]), ("rwkv_ln_w", [DEPTH, 512]), ("rwkv_ln_b", [DEPTH, 512]),
    ("sc_conv_w", [DEPTH, 512, 3]), ("w_branch", [DEPTH, 3, 512, D]), ("w_o", [DEPTH, D, D]),
    ("norm_ffn", [DEPTH, D]), ("ffn_w_up", [DEPTH, D, 2 * DFF]), ("ffn_conv_w", [DEPTH, DFF, 3]),
    ("ffn_w_down", [DEPTH, DFF, D]),
]


class Ctx:
    pass


def col_ap(vec_ap, n):
    return vec_ap.rearrange("(j p o) -> p j o", p=128, o=1)


def stage_consts(C):
    S = C.S
    K = Ctx()
    C.K = K
    for name, shape, dt in CONST_SPECS:
        if name in ("rmask", "ohb"):
            continue
        tl = S.sb(shape, dt, name="k_" + name)
        S.dma("sp", tl[:], C.cin[name], writes=["k_" + name])
        setattr(K, name, tl)
    K.ones_bf = S.sb([128, 128], BF16, name="k_ones")
    mset(S, "pool", K.ones_bf[:], 1.0, w=["k_ones"])
    K.eps = S.sb([128, 1], F32, name="k_eps")
    mset(S, "pool", K.eps[:], 1e-6, w=["k_eps"])
    K.eps_gn = S.sb([128, 1], F32, name="k_epsgn")
    mset(S, "pool", K.eps_gn[:], 64e-5, w=["k_epsgn"])
    S.barrier()


def stage_mod(C, l):
    S = C.S
    inp = C.inp
    with ExitStack() as st:
        ccol = S.sb([128, 8, 1], F32, st)
        S.dma("sp", ccol[:], col_ap(inp["c"], 8), writes=["ccol"], slow=True)
        adab = S.sb([1, 6 * D], F32, st)
        S.dma("sp", adab[:], inp["ada_b"][l].rearrange("(o n) -> o n", o=1), writes=["adab"])
        row = S.sb([1, 6 * D], F32, st)
        aw = [S.sb([128, 6 * D], F32, st) for _ in range(2)]
        ps = [S.ps([128, 512], F32, st) for _ in range(6)]
        for half in range(2):
            for k in range(8):
                b = aw[k % 2]
                S.dma("sp", b[:], inp["ada_w"][l, k * 128:(k + 1) * 128, :], writes=[f"aw{k % 2}"])
                for jj in range(6):
                    j = half * 6 + jj
                    mm(S, ps[jj][0:1, :], ccol[:, k, :], b[:, j * 512:(j + 1) * 512], start=(k == 0), stop=(k == 7),
                       r=[f"aw{k % 2}", "ccol"], w=[f"psm{jj}"])
            for jj in range(6):
                j = half * 6 + jj
                tt(S, "dve", row[0:1, j * 512:(j + 1) * 512], ps[jj][0:1, :], adab[0:1, j * 512:(j + 1) * 512], ALU.add,
                   r=[f"psm{jj}", "adab"], w=["row"])
        S.dma("sp", C.modrow[l].rearrange("(o n) -> o n", o=1), row[:], reads=["row"], writes=["modrow"])
    S.barrier()


def load_mod(C, l, st):
    S = C.S
    inp = C.inp
    M = Ctx()
    modcol = S.sb([128, 48, 1], F32, st)
    S.dma("sp", modcol[:], col_ap(C.modrow[l], 48), writes=["modcol"], slow=True)
    nm = S.sb([128, 8, 1], F32, st)
    nf = S.sb([128, 8, 1], F32, st)
    S.dma("sp", nm[:], col_ap(inp["norm_mix"][l], 8), writes=["nm"], slow=True)
    S.dma("sp", nf[:], col_ap(inp["norm_ffn"][l], 8), writes=["nf"], slow=True)
    M.A1 = S.sb([128, 8, 1], F32, st)
    M.A2 = S.sb([128, 8, 1], F32, st)
    stt(S, "dve", M.A1[:], modcol[:, 8:16, :], 1.0, nm[:], ALU.add, ALU.mult, r=["modcol", "nm"], w=["A1"])
    stt(S, "dve", M.A2[:], modcol[:, 32:40, :], 1.0, nf[:], ALU.add, ALU.mult, r=["modcol", "nf"], w=["A2"])
    M.modcol = modcol
    M.g1 = S.sb([128, D], F32, st)
    M.g2 = S.sb([128, D], F32, st)
    S.dma("sp", M.g1[:], C.modrow[l][2 * D:3 * D].partition_broadcast(128), writes=["g1bc"])
    S.dma("sp", M.g2[:], C.modrow[l][5 * D:6 * D].partition_broadcast(128), writes=["g2bc"])
    S.barrier()
    return M


def stage_norm(C, x_ap, Acol, Bcol, hT, st_outer):
    S = C.S
    K = C.K
    with ExitStack() as st:
        xt = [S.sb([128, D], F32, st) for _ in range(2)]
        junk = S.sb([128, D], BF16, st)
        xn = [S.sb([128, D], BF16, st) for _ in range(2)]
        ss = [S.sb([128, 1], F32, st) for _ in range(2)]
        sd = [S.sb([128, 1], F32, st) for _ in range(2)]
        rs = [S.sb([128, 1], F32, st) for _ in range(2)]
        pT = [S.ps([128, 8, 128], BF16, st) for _ in range(2)]
        S.dma("sp", xt[0][:], x_ap[0:128, :], writes=["xt0"])
        for i in range(NT):
            b = i % 2
            if i + 1 < NT:
                S.dma("sp", xt[1 - b][:], x_ap[(i + 1) * 128:(i + 2) * 128, :], writes=[f"xt{1 - b}"])
            act(S, junk[:], xt[b][:], AF.Square, r=[f"xt{b}"], w=["junk", f"ss{b}"], accum=ss[b][:])
            act(S, sd[b][:], ss[b][:], AF.Sqrt, r=[f"ss{b}"], w=[f"sd{b}"], bias=K.eps[:], scale=1.0 / D)
            S.add("dve", (lambda o, i_: (lambda e: e.reciprocal(o, i_)))(rs[b][:], sd[b][:]), [f"sd{b}"], [f"rs{b}"])
            ts(S, "dve", xn[b][:], xt[b][:], rs[b][:], ALU.mult, r=[f"xt{b}", f"rs{b}"], w=[f"xn{b}"])
            for k in range(8):
                tr(S, pT[b][:, k, :], xn[b][:, k * 128:(k + 1) * 128], K.ident_bf[:], r=[f"xn{b}"], w=[f"pT{b}"])
            for k in range(8):
                eng = "dve" if k % 2 == 0 else "pool"
                if eng == "pool":
                    act(S, hT[:, k, i * 128:(i + 1) * 128], pT[b][:, k, :], AF.Identity, r=[f"pT{b}"], w=[f"hT{i}"],
                        bias=Bcol[:, k, :], scale=Acol[:, k, :])
                else:
                    ts(S, "dve", hT[:, k, i * 128:(i + 1) * 128], pT[b][:, k, :], Acol[:, k, :], ALU.mult, s2=Bcol[:, k, :],
                       op1=ALU.add, r=[f"pT{b}"], w=[f"hT{i}"])
    S.barrier()


def proj_fm(C, hT, w_ap, blocks, out_ap, st, scale=None):
    S = C.S
    wst = [S.sb([128, 8, 512], F32, st) for _ in range(2)]
    wbf = [S.sb([128, 8, 512], BF16, st) for _ in range(2)]
    zrow = [S.sb([128, T], BF16, st) for _ in range(2)]
    ps = [S.ps([128, 512], F32, st) for _ in range(4)]
    wv = w_ap.rearrange("(k p) c -> p k c", p=128)
    row0 = 0
    nev = 0
    nz = 0
    for bi, segs in enumerate(blocks):
        b = bi % 2
        off = 0
        for (c0, nc_) in segs:
            S.dma("sp", wst[b][:, :, off:off + nc_], wv[:, :, c0:c0 + nc_], writes=[f"wst{b}"])
            off += nc_
        cp(S, "pool", wbf[b][:, :, 0:off], wst[b][:, :, 0:off], r=[f"wst{b}"], w=[f"wbf{b}"])
        for cc in range(off // 128):
            zb = nz % 2
            nz += 1
            for tb in range(8):
                p = ps[nev % 4]
                pk = f"psp{nev % 4}"
                for k in range(8):
                    mm(S, p[:], wbf[b][:, k, cc * 128:(cc + 1) * 128], hT[:, k, tb * 512:(tb + 1) * 512],
                       start=(k == 0), stop=(k == 7), r=[f"wbf{b}", "hT"], w=[pk])
                if nev % 2 == 0:
                    act(S, zrow[zb][:, tb * 512:(tb + 1) * 512], p[:], AF.Copy, r=[pk], w=[f"zrow{zb}"])
                else:
                    cp(S, "dve", zrow[zb][:, tb * 512:(tb + 1) * 512], p[:], r=[pk], w=[f"zrow{zb}"])
                nev += 1
            S.dma("sp", out_ap[row0:row0 + 128, :], zrow[zb][:], reads=[f"zrow{zb}"], writes=["zout"], defer=2)
            row0 += 128


def stage_inproj(C, l, hT):
    S = C.S
    w = C.inp["w_in"][l]
    with ExitStack() as st:
        blocks = [[(WQ, 512)], [(WK, 512)], [(WQI, 512)], [(WKI, 64), (WKI, 64)]]
        blocks += [[(WRW + i * 512, 512)] for i in range(3)] + [[(WRW + 1536, 256)]]
        blocks += [[(WCV + i * 512, 512)] for i in range(3)]
        blocks += [[(WGT + i * 512, 512)] for i in range(6)]
        proj_fm(C, hT, w, blocks, C.zT, st)
    S.barrier()
    with ExitStack() as st:
        wv = w.rearrange("(k p) c -> p k c", p=128)
        wst = S.sb([128, 8, 520], F32, st)
        wbf = S.sb([128, 8, 520], BF16, st)
        S.dma("sp", wst[:, :, 0:512], wv[:, :, WV:WV + 512], writes=["wvst"])
        S.dma("sp", wst[:, :, 512:520], wv[:, :, WWI:WWI + 8], writes=["wvst"])
        cp(S, "pool", wbf[:], wst[:], r=["wvst"], w=["wvbf"])
        vt = [S.sb([128, 512], BF16, st) for _ in range(2)]
        wit = S.sb([128, NT, 8], F32, st)
        ps = [S.ps([128, 512], F32, st) for _ in range(2)]
        ps2 = [S.ps([128, 512], F32, st) for _ in range(2)]
        for i in range(NT):
            b = i % 2
            for k in range(8):
                mm(S, ps[b][:], hT[:, k, i * 128:(i + 1) * 128], wbf[:, k, 0:512], start=(k == 0), stop=(k == 7),
                   r=["wvbf"], w=[f"psv{b}"])
            for k in range(8):
                mm(S, ps2[b][:, 0:8], hT[:, k, i * 128:(i + 1) * 128], wbf[:, k, 512:520], start=(k == 0), stop=(k == 7),
                   r=["wvbf"], w=[f"psw{b}"])
            act(S, vt[b][:], ps[b][:], AF.Copy, r=[f"psv{b}"], w=[f"vt{b}"])
            ts(S, "dve", wit[:, i, :], ps2[b][:, 0:8], 8.0 ** -0.5, ALU.mult, r=[f"psw{b}"], w=["wit"])
            S.dma("sp", C.vtok[i * 128:(i + 1) * 128, :], vt[b][:], reads=[f"vt{b}"], writes=["vtok"])
        S.dma("sp", C.witok.rearrange("(i p) h -> p i h", p=128), wit[:], reads=["wit"], writes=["witok"])
    S.barrier()


def make_ctx(nc, es, debug_out=()):
    C = Ctx()
    C.nc = nc
    C.S = Sched(nc, es)
    C.inp = {}
    for name, shape in INPUT_SPECS:
        C.inp[name] = nc.dram_tensor(name, shape, F32, kind="ExternalInput").ap()
    C.cin = {}
    for name, shape, dt in CONST_SPECS:
        C.cin[name] = nc.dram_tensor("k_" + name, shape, dt, kind="ExternalInput").ap()
    C.debug_out = set(debug_out)

    def dram(name, shape, dt):
        kind = "ExternalOutput" if name in C.debug_out else "Internal"
        return nc.dram_tensor(name, list(shape), dt, kind=kind).ap()

    C.dram = dram
    C.modrow = [dram(f"modrow{l}", [6 * D], F32) for l in range(DEPTH)]
    C.zT = dram("zT", [ZROWS, T], BF16)
    C.vtok = dram("vtok", [T, 512], BF16)
    C.witok = dram("witok", [T, 8], F32)
    C.oT = dram("oT", [1536, T], BF16)
    C.uT = dram("uT", [DFF, T], BF16)
    C.xs = [dram(f"xs{i}", [T, D], F32) for i in range(2 * DEPTH)]
    C.maskT = dram("maskT", [128, 528 * 128], BF16)
    C.zdT = dram("zdT", [8, 128, 384], F32)
    NR = 512
    for nm in ("rRT", "rAT", "rBT", "rKT", "rBH", "rKH", "rVT", "rBV", "rG"):
        setattr(C, nm, dram(nm, [NR, T], BF16))
    C.rYT = dram("rYT", [NR, T], F32)
    C.rGC = dram("rGC", [128, 4 * NCH], F32)
    return C


def host_inputs(inputs, b):
    m = {}
    for name, shape in INPUT_SPECS:
        a = np.asarray(inputs[name])
        if name in ("x", "c"):
            a = a[b]
        m[name] = np.ascontiguousarray(a, dtype=np.float32)
    for k, v in make_consts().items():
        m["k_" + k] = v
    return m


def conv3(S, eng, acc, src, wcol, keys_r, key_w):
    ts(S, eng, acc[:, :], src[:, 2:T + 2], wcol[:, 2:3], ALU.mult, r=keys_r, w=[key_w])
    stt(S, eng, acc[:, :], src[:, 1:T + 1], wcol[:, 1:2], acc[:, :], ALU.mult, ALU.add, r=keys_r + [key_w], w=[key_w])
    stt(S, eng, acc[:, :], src[:, 0:T], wcol[:, 0:1], acc[:, :], ALU.mult, ALU.add, r=keys_r + [key_w], w=[key_w])


def stage_conv(C, l):
    S = C.S
    with ExitStack() as st:
        wc = S.sb([128, 4, 3], F32, st)
        S.dma("sp", wc[:], C.inp["sc_conv_w"][l].rearrange("(c p) j -> p c j", p=128), writes=["wc"])
        zb = [S.sb([128, T], BF16, st) for _ in range(2)]
        zc = [S.sb([128, T], BF16, st) for _ in range(2)]
        zx = [S.sb([128, T], BF16, st) for _ in range(2)]
        pp = [S.sb([128, T + 2], F32, st) for _ in range(2)]
        acc = [S.sb([128, T], F32, st) for _ in range(2)]
        ob = [S.sb([128, T], BF16, st) for _ in range(2)]
        for b in range(2):
            mset(S, "pool", pp[b][:, 0:2], 0.0, w=[f"pp{b}"])
        for cc in range(4):
            b = cc % 2
            S.dma("sp", zb[b][:], C.zT[ZCV + cc * 128:ZCV + (cc + 1) * 128, :], writes=[f"zb{b}"])
            S.dma("sp", zc[b][:], C.zT[ZCV + 512 + cc * 128:ZCV + 512 + (cc + 1) * 128, :], writes=[f"zc{b}"])
            S.dma("sp", zx[b][:], C.zT[ZCV + 1024 + cc * 128:ZCV + 1024 + (cc + 1) * 128, :], writes=[f"zx{b}"])
            tt(S, "pool", pp[b][:, 2:T + 2], zc[b][:], zx[b][:], ALU.mult, r=[f"zc{b}", f"zx{b}"], w=[f"pp{b}"])
            conv3(S, "dve", acc[b], pp[b], wc[:, cc, :], [f"pp{b}", "wc"], f"acc{b}")
            tt(S, "pool", ob[b][:], acc[b][:], zb[b][:], ALU.mult, r=[f"acc{b}", f"zb{b}"], w=[f"ob{b}"])
            S.dma("sp", C.oT[1024 + cc * 128:1024 + (cc + 1) * 128, :], ob[b][:], reads=[f"ob{b}"], writes=["oT"], defer=3)
    S.barrier()


def stage_merge(C, l, M, x_in, x_out):
    S = C.S
    inp = C.inp
    with ExitStack() as st:
        wb = S.sb([128, 12, D], BF16, st)
        wo = S.sb([128, 8, D], BF16, st)
        wst = [S.sb([128, 2, D], F32, st) for _ in range(2)]
        for i in range(10):
            b = i % 2
            if i < 6:
                src = inp["w_branch"][l, i // 2].rearrange("(k p) d -> p k d", p=128)[:, (i % 2) * 2:(i % 2) * 2 + 2, :]
                dst = wb[:, i * 2:i * 2 + 2, :]
            else:
                src = inp["w_o"][l].rearrange("(k p) d -> p k d", p=128)[:, (i - 6) * 2:(i - 6) * 2 + 2, :]
                dst = wo[:, (i - 6) * 2:(i - 6) * 2 + 2, :]
            S.dma("sp", wst[b][:], src, writes=[f"wst{b}"])
            cp(S, "pool", dst, wst[b][:], r=[f"wst{b}"], w=["wbo"])
        ot = [S.sb([128, 12, 512], BF16, st) for _ in range(2)]
        gt = [S.sb([128, 24, 512], BF16, st) for _ in range(1)]
        mg = [S.sb([128, 8, 512], BF16, st) for _ in range(2)]
        sg = [S.sb([128, 512], BF16, st) for _ in range(3)]
        mt = [S.sb([128, 512], F32, st) for _ in range(3)]
        m01 = S.sb([128, 512], F32, st)
        xt = [S.sb([128, D], F32, st) for _ in range(2)]
        tmp = [S.sb([128, D], F32, st) for _ in range(2)]
        xo = [S.sb([128, D], F32, st) for _ in range(2)]
        ps = [S.ps([128, 512], F32, st) for _ in range(6)]
        pso = [S.ps([128, 512], F32, st) for _ in range(2)]
        oTv = C.oT.rearrange("(c p) t -> p c t", p=128)
        gTv = C.zT[ZGT:ZGT + 3072, :].rearrange("(c p) t -> p c t", p=128)
        cnt = {"ps": 0, "po": 0, "x": 0}

        def branch(tb):
            b = tb % 2
            S.dma("sp", ot[b][:], oTv[:, :, tb * 512:(tb + 1) * 512], writes=[f"ot{b}"])
            S.dma("sp", gt[0][:], gTv[:, :, tb * 512:(tb + 1) * 512], writes=["gt0"])
            for dc in range(8):
                for i in range(3):
                    p = ps[cnt["ps"] % 6]
                    pk = f"psb{cnt['ps'] % 6}"
                    cnt["ps"] += 1
                    for kc in range(4):
                        mm(S, p[:], wb[:, i * 4 + kc, dc * 128:(dc + 1) * 128], ot[b][:, i * 4 + kc, :], start=(kc == 0),
                           stop=(kc == 3), r=["wbo", f"ot{b}"], w=[pk])
                    act(S, sg[i][:], gt[0][:, i * 8 + dc, :], AF.Sigmoid, r=["gt0"], w=[f"sg{i}"])
                    tt(S, "dve", mt[i][:], p[:], sg[i][:], ALU.mult, r=[pk, f"sg{i}"], w=[f"mt{i}"])
                tt(S, "pool", m01[:], mt[0][:], mt[1][:], ALU.add, r=["mt0", "mt1"], w=["m01"])
                tt(S, "pool", mg[b][:, dc, :], m01[:], mt[2][:], ALU.add, r=["m01", "mt2"], w=[f"mg{b}"])

        def wo_part(tb):
            b = tb % 2
            for t4 in range(4):
                xb = cnt["x"] % 2
                cnt["x"] += 1
                tok0 = tb * 512 + t4 * 128
                S.dma("sp", xt[xb][:], x_in[tok0:tok0 + 128, :], writes=[f"xt{xb}"])
                for nb in range(2):
                    p = pso[cnt["po"] % 2]
                    pk = f"pso{cnt['po'] % 2}"
                    cnt["po"] += 1
                    for dc in range(8):
                        mm(S, p[:], mg[b][:, dc, t4 * 128:(t4 + 1) * 128], wo[:, dc, nb * 512:(nb + 1) * 512], start=(dc == 0),
                           stop=(dc == 7), r=["wbo", f"mg{b}"], w=[pk])
                    tt(S, "dve", tmp[xb][:, nb * 512:(nb + 1) * 512], p[:], M.g1[:, nb * 512:(nb + 1) * 512], ALU.mult,
                       r=[pk, "g1bc"], w=[f"tmp{xb}"])
                tt(S, "pool", xo[xb][:], tmp[xb][:], xt[xb][:], ALU.add, r=[f"tmp{xb}", f"xt{xb}"], w=[f"xo{xb}"])
                S.dma("sp", x_out[tok0:tok0 + 128, :], xo[xb][:], reads=[f"xo{xb}"], writes=["xout"], defer=1)

        branch(0)
        for tb in range(8):
            if tb + 1 < 8:
                branch(tb + 1)
            wo_part(tb)
    S.barrier()


def stage_ffn_up(C, l, hT):
    S = C.S
    with ExitStack() as st:
        wca = S.sb([128, 22, 3], F32, st)
        S.dma("sp", wca[:], C.inp["ffn_conv_w"][l].rearrange("(c p) j -> p c j", p=128), writes=["wca"])
        wv = C.inp["ffn_w_up"][l].rearrange("(k p) c -> p k c", p=128)
        wst = [S.sb([128, 8, 256], F32, st) for _ in range(2)]
        wbf = [S.sb([128, 8, 256], BF16, st) for _ in range(2)]
        arow = [S.sb([128, T + 2], F32, st) for _ in range(2)]
        grow = [S.sb([128, T], BF16, st) for _ in range(2)]
        acc = [S.sb([128, T], F32, st)] * 2
        sl = [S.sb([128, T], BF16, st)] * 2
        ub = [S.sb([128, T], BF16, st) for _ in range(2)]
        ps = [S.ps([128, 512], F32, st) for _ in range(4)]
        for b in range(2):
            mset(S, "pool", arow[b][:, 0:2], 0.0, w=[f"arow{b}"])
        nps = 0
        for kc in range(22):
            b = kc % 2
            S.dma("sp", wst[b][:, :, 0:128], wv[:, :, kc * 128:(kc + 1) * 128], writes=[f"wst{b}"])
            S.dma("sp", wst[b][:, :, 128:256], wv[:, :, DFF + kc * 128:DFF + (kc + 1) * 128], writes=[f"wst{b}"])
            cp(S, "pool", wbf[b][:], wst[b][:], r=[f"wst{b}"], w=[f"wbf{b}"])
            for tb in range(8):
                for half in range(2):
                    p = ps[nps % 4]
                    pk = f"psu{nps % 4}"
                    nps += 1
                    for k in range(8):
                        mm(S, p[:], wbf[b][:, k, half * 128:(half + 1) * 128], hT[:, k, tb * 512:(tb + 1) * 512], start=(k == 0),
                           stop=(k == 7), r=[f"wbf{b}"], w=[pk])
                    if half == 0:
                        act(S, arow[b][:, 2 + tb * 512:2 + (tb + 1) * 512], p[:], AF.Copy, r=[pk], w=[f"arow{b}"])
                    else:
                        cp(S, "act", grow[b][:, tb * 512:(tb + 1) * 512], p[:], r=[pk], w=[f"grow{b}"])
            conv3(S, "dve", acc[b], arow[b], wca[:, kc, :], [f"arow{b}", "wca"], "acc0")
            act(S, sl[b][:], acc[b][:], AF.Silu, r=["acc0"], w=["sl0"])
            tt(S, "pool" if kc % 2 == 0 else "dve", ub[b][:], sl[b][:], grow[b][:], ALU.mult, r=["sl0", f"grow{b}"], w=[f"ub{b}"])
            S.dma("sp", C.uT[kc * 128:(kc + 1) * 128, :], ub[b][:], reads=[f"ub{b}"], writes=["uT"], defer=2)
    S.barrier()


def stage_ffn_down(C, l, M, x_in, x_out):
    S = C.S
    with ExitStack() as st:
        wd = S.sb([128, 22, D], BF16, st)
        wst = [S.sb([128, 2, D], F32, st) for _ in range(2)]
        wv = C.inp["ffn_w_down"][l].rearrange("(k p) d -> p k d", p=128)
        for i in range(11):
            b = i % 2
            S.dma("sp", wst[b][:], wv[:, 2 * i:2 * i + 2, :], writes=[f"wst{b}"])
            cp(S, "pool", wd[:, 2 * i:2 * i + 2, :], wst[b][:], r=[f"wst{b}"], w=["wd"])
        ut = [S.sb([128, 22, 512], BF16, st) for _ in range(2)]
        xt = [S.sb([128, D], F32, st) for _ in range(2)]
        tmp = [S.sb([128, D], F32, st) for _ in range(2)]
        xo = [S.sb([128, D], F32, st) for _ in range(2)]
        pso = [S.ps([128, 512], F32, st) for _ in range(4)]
        uTv = C.uT.rearrange("(c p) t -> p c t", p=128)
        npo = 0
        nx = 0
        for tb in range(8):
            b = tb % 2
            S.dma("sp", ut[b][:], uTv[:, :, tb * 512:(tb + 1) * 512], writes=[f"ut{b}"])
            for t4 in range(4):
                xb = nx % 2
                nx += 1
                tok0 = tb * 512 + t4 * 128
                S.dma("sp", xt[xb][:], x_in[tok0:tok0 + 128, :], writes=[f"xt{xb}"])
                for nb in range(2):
                    p = pso[npo % 4]
                    pk = f"pso{npo % 4}"
                    npo += 1
                    for kc in range(22):
                        mm(S, p[:], ut[b][:, kc, t4 * 128:(t4 + 1) * 128], wd[:, kc, nb * 512:(nb + 1) * 512], start=(kc == 0),
                           stop=(kc == 21), r=["wd", f"ut{b}"], w=[pk])
                    tt(S, "dve", tmp[xb][:, nb * 512:(nb + 1) * 512], p[:], M.g2[:, nb * 512:(nb + 1) * 512], ALU.mult,
                       r=[pk, "g2bc"], w=[f"tmp{xb}"])
                tt(S, "pool", xo[xb][:], tmp[xb][:], xt[xb][:], ALU.add, r=[f"tmp{xb}", f"xt{xb}"], w=[f"xo{xb}"])
                S.dma("sp", x_out[tok0:tok0 + 128, :], xo[xb][:], reads=[f"xo{xb}"], writes=["xout"], defer=1)
    S.barrier()


def stage_final(C, x_in, out_ap):
    S = C.S
    K = C.K
    with ExitStack() as st:
        fn = S.sb([128, D], F32, st)
        S.dma("sp", fn[:], C.inp["final_norm"].partition_broadcast(128), writes=["fn"])
        xt = [S.sb([128, D], F32, st) for _ in range(2)]
        junk = S.sb([128, D], BF16, st)
        y1 = [S.sb([128, D], F32, st) for _ in range(2)]
        y2 = [S.sb([128, D], F32, st) for _ in range(2)]
        ss = [S.sb([128, 1], F32, st) for _ in range(2)]
        sd = [S.sb([128, 1], F32, st) for _ in range(2)]
        rs = [S.sb([128, 1], F32, st) for _ in range(2)]
        for i in range(NT):
            b = i % 2
            S.dma("sp", xt[b][:], x_in[i * 128:(i + 1) * 128, :], writes=[f"xt{b}"])
            act(S, junk[:], xt[b][:], AF.Square, r=[f"xt{b}"], w=["junk", f"ss{b}"], accum=ss[b][:])
            act(S, sd[b][:], ss[b][:], AF.Sqrt, r=[f"ss{b}"], w=[f"sd{b}"], bias=K.eps[:], scale=1.0 / D)
            S.add("dve", (lambda o, i_: (lambda e: e.reciprocal(o, i_)))(rs[b][:], sd[b][:]), [f"sd{b}"], [f"rs{b}"])
            ts(S, "dve", y1[b][:], xt[b][:], rs[b][:], ALU.mult, r=[f"xt{b}", f"rs{b}"], w=[f"y1{b}"])
            tt(S, "pool", y2[b][:], y1[b][:], fn[:], ALU.mult, r=[f"y1{b}", "fn"], w=[f"y2{b}"])
            S.dma("sp", out_ap[i * 128:(i + 1) * 128, :], y2[b][:], reads=[f"y2{b}"], writes=["yout"], defer=1)
    S.barrier()


def stage_bias(C):
    S = C.S
    K = C.K
    K.corrD = S.sb([128, 8, 128], BF16, name="corrD")
    K.corrO = S.sb([128, 8, 128], BF16, name="corrO")
    K.rb31 = S.sb([128, 8], F32, name="rb31")
    with ExitStack() as st:
        rb = S.sb([32, 8], F32, st)
        ohb = S.sb([32, 384], F32, st)
        ones32 = S.sb([32, 128], F32, st)
        nrb = S.sb([128, 8], F32, st)
        S.dma("sp", rb[:], C.inp["rel_bias"], writes=["rb"])
        S.dma("sp", ohb[:], C.cin["ohb"], writes=["ohb"])
        S.dma("sp", K.rb31[:], C.inp["rel_bias"][31].partition_broadcast(128), writes=["rb31"])
        mset(S, "pool", ones32[:], 1.0, w=["ones32"])
        ts(S, "dve", nrb[:], K.rb31[:], -1.0, ALU.mult, r=["rb31"], w=["nrb"])
        lh = [S.sb([32, 128], F32, st) for _ in range(2)]
        zs = [S.sb([128, 384], F32, st) for _ in range(2)]
        ps = [S.ps([128, 512], F32, st) for _ in range(2)]
        for h in range(8):
            b = h % 2
            ts(S, "dve", lh[b][:], ones32[:], rb[:, h:h + 1], ALU.mult, r=["ones32", "rb"], w=[f"lh{b}"])
            mm(S, ps[b][:, 0:384], lh[b][:], ohb[:], r=[f"lh{b}", "ohb"], w=[f"psz{b}"])
            cp(S, "dve", zs[b][:], ps[b][:, 0:384], r=[f"psz{b}"], w=[f"zs{b}"])
            S.dma("sp", C.zdT[h], zs[b][:], reads=[f"zs{b}"], writes=["zdT"])
        S.barrier()
        td = [S.sb([128, 128], F32, st) for _ in range(2)]
        n = 0
        for h in range(8):
            for which, off0 in (("D", 127), ("O", 255)):
                b = n % 2
                n += 1
                src = bass.AP(tensor=C.zdT.tensor, offset=h * 128 * 384 + off0, ap=[[383, 128], [1, 128]])
                S.dma("sp", td[b][:], src, writes=[f"td{b}"])
                dst = (K.corrD if which == "D" else K.corrO)[:, h, :]
                act(S, dst, td[b][:], AF.Exp, r=[f"td{b}", "nrb"], w=["corr"], bias=nrb[:, h:h + 1])
    S.barrier()


NIT = 14
_FILL = {}


def _fill_reg(e):
    if id(e) not in _FILL:
        _FILL[id(e)] = e.to_reg(NEG)
    return _FILL[id(e)]


def stage_index(C, l):
    S = C.S
    K = C.K
    with ExitStack() as st:
        qiT = S.sb([128, 4, T], BF16, st)
        kiT = S.sb([128, T], BF16, st)
        wi = S.sb([128, NT, 8], F32, st)
        S.dma("sp", qiT[:], C.zT[ZQI:ZQI + 512, :].rearrange("(c p) t -> p c t", p=128), writes=["qiT"])
        S.dma("sp", kiT[:], C.zT[ZKI:ZKI + 128, :], writes=["kiT"])
        S.dma("sp", wi[:], C.witok.rearrange("(i p) h -> p i h", p=128), writes=["wi"])
        Iacc = [S.sb([128, T], F32, st) for _ in range(2)]
        rl = [S.sb([128, 512], F32, st) for _ in range(3)]
        cmpj = S.sb([128, T], BF16, st)
        mask = [S.sb([128, T], BF16, st) for _ in range(2)]
        mT = [S.sb([128, NT, 128], BF16, st) for _ in range(2)]
        sm = {nm: [S.sb([128, 1], F32, st) for _ in range(2)] for nm in ("rmax", "rmin", "lo", "w", "mid", "cnt", "ge", "sgn", "tot")}
        cmpa = S.sb([128, T], BF16, st)
        pw2 = S.sb([128, NIT], F32, st)
        for k_ in range(NIT):
            mset(S, "pool", pw2[:, k_:k_ + 1], 2.0 ** -(k_ + 1), w=["pw2"])
        wtab = [S.sb([128, NIT], F32, st) for _ in range(2)]
        psI = [S.ps([128, 512], F32, st) for _ in range(3)]
        psT = [S.ps([128, 4, 128], BF16, st) for _ in range(2)]
        off = 0
        n = 0
        ng = 0
        for i in range(NT):
            L = (i + 1) * 128
            b = i % 2
            mk = f"mask{b}"
            if i >= 2:
                nchunk = (L + 511) // 512
                for h in range(8):
                    hp, hc = h % 2, h // 2
                    r0 = hp * 64
                    for ch in range(nchunk):
                        w_ = min(512, L - ch * 512)
                        p = psI[n % 3]
                        pk = f"psI{n % 3}"
                        rt = rl[n % 3]
                        rk = f"rl{n % 3}"
                        n += 1
                        mm(S, p[:, 0:w_], qiT[r0:r0 + 64, hc, i * 128:(i + 1) * 128], kiT[r0:r0 + 64, ch * 512:ch * 512 + w_],
                           r=["qiT", "kiT"], w=[pk])
                        act(S, rt[:, 0:w_], p[:, 0:w_], AF.Relu, r=[pk], w=[rk], scale=0.125)
                        ik = f"I{b}_{ch}"
                        dst = Iacc[b][:, ch * 512:ch * 512 + w_]
                        if h == 0:
                            ts(S, "dve", dst, rt[:, 0:w_], wi[:, i, 0:1], ALU.mult, r=[rk, "wi"], w=[ik])
                        else:
                            stt(S, "dve", dst, rt[:, 0:w_], wi[:, i, h:h + 1], dst, ALU.mult, ALU.add, r=[rk, "wi", ik], w=[ik])
                allI = [f"I{b}_{ch}" for ch in range(nchunk)]
                dg = Iacc[b][:, i * 128:L]
                S.add("pool", (lambda o: (lambda e: e.affine_select(out=o, in_=o, pattern=[[-1, 128]], compare_op=ALU.is_ge,
                                                                    fill=_fill_reg(e), base=0, channel_multiplier=1)))(dg),
                      allI, allI)
                rmax, rmin, lo, wd_, mid, cnt, ge = (sm[nm][b] for nm in ("rmax", "rmin", "lo", "w", "mid", "cnt", "ge"))
                sk = f"sm{b}"
                S.add("dve", (lambda o, a: (lambda e: e.tensor_reduce(out=o, in_=a, axis=AX.X, op=ALU.max)))(rmax[:], Iacc[b][:, 0:L]),
                      allI, [sk + "rmax"])
                S.add("dve", (lambda o, a: (lambda e: e.tensor_reduce(out=o, in_=a, axis=AX.X, op=ALU.min)))(rmin[:], Iacc[b][:, 0:i * 128]),
                      allI, [sk + "rmin"])
                ts(S, "dve", lo[:], rmin[:], -1.0, ALU.add, r=[sk + "rmin"], w=[sk + "lo"])
                tt(S, "dve", wd_[:], rmax[:], lo[:], ALU.subtract, r=[sk + "rmax", sk + "lo"], w=[sk + "w"])
                ts(S, "dve", wtab[b][:], pw2[:], wd_[:], ALU.mult, r=["pw2", sk + "w"], w=[sk + "wtab"])
                stt(S, "dve", mid[:], wd_[:], 0.5, lo[:], ALU.mult, ALU.add, r=[sk + "w", sk + "lo"], w=[sk + "mid"])
                La = ((L // 128) // 2) * 128
                sgn, tot = sm["sgn"][b], sm["tot"][b]
                for it in range(NIT):
                    ts(S, "dve", cmpj[:, 0:La], Iacc[b][:, 0:La], mid[:], ALU.is_gt, op1=ALU.add, r=allI + [sk + "mid"],
                       w=["cmpj", sk + "cnt"], accum=cnt[:])
                    act(S, cmpa[:, La:L], Iacc[b][:, La:L], AF.Sign, r=allI + [sk + "mid"], w=["cmpa", sk + "sgn"], bias=mid[:],
                        scale=-1.0, accum=sgn[:])
                    stt(S, "dve", tot[:], sgn[:], -0.5, cnt[:], ALU.mult, ALU.add, r=[sk + "sgn", sk + "cnt"], w=[sk + "tot"])
                    ts(S, "dve", ge[:], tot[:], 255.5 - 0.5 * (L - La), ALU.is_gt, s2=0.5, op1=ALU.subtract, r=[sk + "tot"],
                       w=[sk + "ge"])
                    stt(S, "dve", mid[:], ge[:], wtab[b][:, it:it + 1], mid[:], ALU.mult, ALU.add,
                        r=[sk + "ge", sk + "wtab", sk + "mid"], w=[sk + "mid"])
                ts(S, "dve", mask[b][:, 0:L], Iacc[b][:, 0:L], mid[:], ALU.is_gt, r=allI + [sk + "mid"], w=[mk])
            elif i == 0:
                cp(S, "dve", mask[b][:, 0:128], K.tri[:], r=["k_tri"], w=[mk])
            else:
                cp(S, "dve", mask[b][:, 0:128], K.ones_bf[:], r=["k_ones"], w=[mk])
                cp(S, "dve", mask[b][:, 128:256], K.tri[:], r=["k_tri"], w=[mk])
            for g in range((i + 4) // 4):
                nb_ = min(4, i + 1 - 4 * g)
                pt = psT[ng % 2]
                ptk = f"psT{ng % 2}"
                ng += 1
                for jj in range(nb_):
                    tr(S, pt[:, jj, :], mask[b][:, (4 * g + jj) * 128:(4 * g + jj + 1) * 128], K.ident_bf[:], r=[mk], w=[ptk])
                cp(S, "act", mT[b][:, 4 * g:4 * g + nb_, :], pt[:, 0:nb_, :], r=[ptk], w=[f"mT{b}"])
            S.dma("sp", C.maskT[:, off * 128:(off + i + 1) * 128].rearrange("p (j q) -> p j q", q=128), mT[b][:, 0:i + 1, :],
                  reads=[f"mT{b}"], writes=["maskT"])
            off += i + 1
    S.barrier()


def stage_attn(C, l):
    S = C.S
    K = C.K
    with ExitStack() as st:
        qT = S.sb([128, 4, T], BF16, st)
        kT = S.sb([128, 4, T], BF16, st)
        V = S.sb([128, NT, 512], BF16, st)
        S.dma("sp", qT[:], C.zT[ZQ:ZQ + 512, :].rearrange("(c p) t -> p c t", p=128), writes=["qT"])
        S.dma("sp", kT[:], C.zT[ZK:ZK + 512, :].rearrange("(c p) t -> p c t", p=128), writes=["kT"])
        S.dma("sp", V[:], C.vtok.rearrange("(j p) c -> p j c", p=128), writes=["V"])
        mk = [S.sb([128, NT, 128], BF16, st) for _ in range(2)]
        E = [S.sb([128, 4, 128], BF16, st) for _ in range(3)]
        P = [S.sb([128, 4, 128], BF16, st) for _ in range(3)]
        rec = [S.sb([128, 128], F32, st) for _ in range(2)]
        ob = [S.sb([128, 4, 128], BF16, st) for _ in range(2)]
        psS = [S.ps([128, 4, 128], F32, st) for _ in range(3)]
        psN = [S.ps([128, 512], F32, st) for _ in range(2)]
        psD = [S.ps([128, 512], F32, st) for _ in range(2)]
        oTv = C.oT[0:512, :].rearrange("(c p) t -> p c t", p=128)
        offs = []
        off = 0
        for i in range(NT):
            offs.append(off)
            off += i + 1

        def load_mask(i):
            b = i % 2
            nblk = i + 1
            S.dma("sp", mk[b][:, 0:nblk, :],
                  C.maskT[:, offs[i] * 128:(offs[i] + nblk) * 128].rearrange("p (j q) -> p j q", q=128), writes=[f"mk{b}"])

        items = []
        for i in range(NT):
            for h in range(8):
                ng_ = (i + 4) // 4
                for g in range(ng_):
                    items.append((i, h, g, g == ng_ - 1))

        def emit_st(n, it):
            i, h, g, _ = it
            hp, hc = h % 2, h // 2
            r0 = hp * 64
            nb_ = min(4, i + 1 - 4 * g)
            ps_ = psS[n % 3]
            for jj in range(nb_):
                j = 4 * g + jj
                mm(S, ps_[:, jj, :], kT[r0:r0 + 64, hc, j * 128:(j + 1) * 128], qT[r0:r0 + 64, hc, i * 128:(i + 1) * 128],
                   r=["qT", "kT"], w=[f"psS{n % 3}"])

        def emit_rest(n, it):
            i, h, g, last = it
            b = i % 2
            hp, hc = h % 2, h // 2
            r0 = hp * 64
            nb_ = min(4, i + 1 - 4 * g)
            ps_ = psS[n % 3]
            psk = f"psS{n % 3}"
            e_, ek = E[n % 3], f"E{n % 3}"
            p_, pk = P[n % 3], f"P{n % 3}"
            pn, pd = psN[hc % 2], psD[hc % 2]
            pnk, pdk = f"psN{hc % 2}", f"psD{hc % 2}"
            act(S, e_[:, 0:nb_, :], ps_[:, 0:nb_, :], AF.Exp, r=[psk, "rb31"], w=[ek], bias=K.rb31[:, h:h + 1], scale=0.125)
            tt(S, "dve", p_[:, 0:nb_, :], e_[:, 0:nb_, :], mk[b][:, 4 * g:4 * g + nb_, :], ALU.mult, r=[ek, f"mk{b}"], w=[pk])
            for jj in range(nb_):
                j = 4 * g + jj
                if j == i:
                    tt(S, "pool", p_[:, jj, :], p_[:, jj, :], K.corrD[:, h, :], ALU.mult, r=[pk, "corr"], w=[pk])
                elif j == i - 1:
                    tt(S, "pool", p_[:, jj, :], p_[:, jj, :], K.corrO[:, h, :], ALU.mult, r=[pk, "corr"], w=[pk])
            for jj in range(nb_):
                j = 4 * g + jj
                mm(S, pn[r0:r0 + 64, 0:128], V[:, j, h * 64:(h + 1) * 64], p_[:, jj, :], start=(j == 0), stop=(j == i),
                   r=["V", pk], w=[pnk])
            mm(S, pd[r0:r0 + 64, 0:nb_ * 128], K.ones_bf[:, 0:64], p_[:].rearrange("p j q -> p (j q)")[:, 0:nb_ * 128],
               start=(g == 0), stop=last, r=["k_ones", pk], w=[pdk])
            if last and hp == 1:
                rc = rec[hc % 2]
                rck = f"rec{hc % 2}"
                nj = min(4, i + 1)
                S.add("dve", (lambda o, a: (lambda e: e.tensor_reduce(out=o, in_=a, axis=AX.X, op=ALU.add)))(
                    rc[:], pd[:, 0:nj * 128].rearrange("p (j q) -> p q j", j=nj)), [pdk], [rck])
                S.add("dve", (lambda o, a: (lambda e: e.reciprocal(o, a)))(rc[:], rc[:]), [rck], [rck])
                tt(S, "dve", ob[b][:, hc, :], pn[:, 0:128], rc[:], ALU.mult, r=[pnk, rck], w=[f"ob{b}"])
            if last and h == 7:
                S.dma("sp", oTv[:, :, i * 128:(i + 1) * 128], ob[b][:], reads=[f"ob{b}"], writes=["oT"])

        load_mask(0)
        load_mask(1)
        emit_st(0, items[0])
        emit_st(1, items[1])
        for n, it in enumerate(items):
            if n + 2 < len(items):
                emit_st(n + 2, items[n + 2])
            emit_rest(n, it)
            i, h, g, last = it
            if last and h == 7 and i + 2 < NT:
                load_mask(i + 2)
    S.barrier()


TB = 1024
NLEV = 4


def stage_rwkv_prep(C, l):
    S = C.S
    K = C.K
    inp = C.inp
    with ExitStack() as st:
        def colp(name, n):
            t_ = S.sb([128, n, 1], F32, st)
            S.dma("sp", t_[:], col_ap(inp[name][l], n), writes=["c_" + name], slow=True)
            return t_
        mu = colp("rwkv_mu", 14)
        w0 = colp("rwkv_w0", 4)
        a0 = colp("rwkv_a0", 4)
        kkc = colp("rwkv_k_k", 4)
        kac = colp("rwkv_k_a", 4)
        rkc = colp("rwkv_r_k", 4)
        omka = S.sb([128, 4, 1], F32, st)
        ts(S, "dve", omka[:], kac[:], -1.0, ALU.mult, s2=1.0, op1=ALU.add, r=["c_rwkv_k_a"], w=["omka"])
        wa_st = S.sb([128, 512], F32, st)
        gu_st = S.sb([128, 512], F32, st)
        wa = S.sb([128, 512], BF16, st)
        gu = S.sb([128, 512], BF16, st)
        S.dma("sp", wa_st[0:64, :], inp["rwkv_w_up"][l], writes=["wa_st"])
        S.dma("sp", wa_st[64:128, :], inp["rwkv_a_up"][l], writes=["wa_st"])
        S.dma("sp", gu_st[:], inp["rwkv_g_up"][l], writes=["gu_st"])
        cp(S, "dve", wa[:], wa_st[:], r=["wa_st"], w=["wa"])
        cp(S, "dve", gu[:], gu_st[:], r=["gu_st"], w=["gu"])
        rmask = S.sb([128, TB], F32, st)
        S.dma("sp", rmask[:], C.cin["rmask"][:, 0:TB], writes=["rmask"])
        gC = S.sb([128, 4, NCH], F32, st)
        xwa = S.sb([128, T], BF16, st)
        sg = S.sb([128, T], BF16, st)
        raw = [S.sb([128, TB + 1], BF16, st) for _ in range(3)]
        F = {nm: S.sb([128, TB], F32, st, name=nm) for nm in
             ("zr", "zk", "zv", "d", "sgw", "af", "lw", "cum", "ex", "epos", "eneg", "eex", "eh", "kx", "nrm", "kk", "t1",
              "kmod", "bq")}
        H = {nm: S.sb([128, TB], BF16, st, name=nm) for nm in
             ("sq", "prod", "g", "RT", "AT", "BT", "KT", "BH", "KH", "VT", "BV")}
        ps = [S.ps([128, 512], F32, st) for _ in range(4)]
        nps = [0]

        def newps():
            i_ = nps[0] % 4
            nps[0] += 1
            return ps[i_], f"psr{i_}"

        def lerp(ci, t0, rawt, rk, out, ok):
            row0 = ZRW + ci * 128
            if t0 == 0:
                mset(S, "pool", rawt[:, 0:1], 0.0, w=[rk])
                S.dma("sp", rawt[:, 1:TB + 1], C.zT[row0:row0 + 128, 0:TB], writes=[rk])
            else:
                S.dma("sp", rawt[:, 0:TB + 1], C.zT[row0:row0 + 128, t0 - 1:t0 + TB], writes=[rk])
            tt(S, "dve", F["d"][:], rawt[:, 0:TB], rawt[:, 1:TB + 1], ALU.subtract, r=[rk], w=["d"])
            stt(S, "dve", out, F["d"][:], mu[:, ci, :], rawt[:, 1:TB + 1], ALU.mult, ALU.add, r=["d", rk, "c_rwkv_mu"], w=[ok])

        for tb in range(T // TB):
            t0 = tb * TB
            lerp(12, t0, raw[0], "raw0", F["zr"][:], "zr")
            act(S, xwa[0:64, t0:t0 + TB], F["zr"][0:64, :], AF.Tanh, r=["zr"], w=["xwa"])
            cp(S, "dve", xwa[64:128, t0:t0 + TB], F["zr"][64:128, :], r=["zr"], w=["xwa"])
            lerp(13, t0, raw[1], "raw1", F["zk"][:], "zk")
            act(S, sg[:, t0:t0 + TB], F["zk"][:], AF.Sigmoid, r=["zk"], w=["sg"])

        for pc in range(4):
            pcs = slice(pc * 128, (pc + 1) * 128)
            for tb in range(T // TB):
                t0 = tb * TB
                lerp(pc, t0, raw[0], "raw0", F["zr"][:], "zr")
                lerp(4 + pc, t0, raw[1], "raw1", F["zk"][:], "zk")
                lerp(8 + pc, t0, raw[2], "raw2", F["zv"][:], "zv")
                for sb in range(TB // 512):
                    c0 = sb * 512
                    tcs = slice(t0 + c0, t0 + c0 + 512)
                    p, pk = newps()
                    mm(S, p[:], wa[0:64, pcs], xwa[0:64, tcs], r=["wa", "xwa"], w=[pk])
                    act(S, F["sgw"][:, c0:c0 + 512], p[:], AF.Sigmoid, r=[pk, "c_rwkv_w0"], w=["sgw"], bias=w0[:, pc, :])
                    p, pk = newps()
                    mm(S, p[:], wa[64:128, pcs], xwa[64:128, tcs], r=["wa", "xwa"], w=[pk])
                    act(S, F["af"][:, c0:c0 + 512], p[:], AF.Sigmoid, r=[pk, "c_rwkv_a0"], w=["af"], bias=a0[:, pc, :])
                    p, pk = newps()
                    mm(S, p[:], gu[:, pcs], sg[:, tcs], r=["gu", "sg"], w=[pk])
                    cp(S, "dve", H["g"][:, c0:c0 + 512], p[:], r=[pk], w=["g"])
                ts(S, "pool", F["lw"][:], F["sgw"][:], -EDEC, ALU.mult, r=["sgw"], w=["lw"])
                S.add("dve", (lambda o, a, b_: (lambda e: e.tensor_tensor_scan(o, a, b_, 0.0, ALU.mult, ALU.add)))(
                    F["cum"][:], rmask[:], F["lw"][:]), ["rmask", "lw"], ["cum"])
                cumv = F["cum"][:].rearrange("p (c t) -> p c t", t=CH)
                act(S, gC[:, pc, tb * (TB // CH):(tb + 1) * (TB // CH)], F["cum"][:, CH - 1:TB:CH], AF.Exp, r=["cum"], w=["gC"])
                act(S, F["epos"][:], F["cum"][:], AF.Exp, r=["cum"], w=["epos"])
                act(S, F["eneg"][:], F["cum"][:], AF.Exp, r=["cum"], w=["eneg"], scale=-1.0)
                tt(S, "pool", F["ex"][:], F["cum"][:], F["lw"][:], ALU.subtract, r=["cum", "lw"], w=["ex"])
                act(S, F["eex"][:], F["ex"][:], AF.Exp, r=["ex"], w=["eex"])
                tt(S, "dve", F["eh"][:].rearrange("p (c t) -> p c t", t=CH), cumv[:, :, CH - 1:CH].broadcast_to([128, TB // CH, CH]),
                   cumv, ALU.subtract, r=["cum"], w=["ehx"])
                act(S, F["eh"][:], F["eh"][:], AF.Exp, r=["ehx"], w=["eh"])
                ts(S, "dve", F["kx"][:], F["zk"][:], kkc[:, pc, :], ALU.mult, r=["zk", "c_rwkv_k_k"], w=["kx"])
                tt(S, "pool", H["sq"][:], F["kx"][:], F["kx"][:], ALU.mult, r=["kx"], w=["sq"])
                for sb in range(TB // 512):
                    c0 = sb * 512
                    p, pk = newps()
                    mm(S, p[:], K.blk_bf[:], H["sq"][:, c0:c0 + 512], r=["k_blk_bf", "sq"], w=[pk])
                    act(S, F["nrm"][:, c0:c0 + 512], p[:], AF.Sqrt, r=[pk], w=["nrm"])
                ts(S, "dve", F["nrm"][:], F["nrm"][:], 1e-12, ALU.max, r=["nrm"], w=["nrm"])
                S.add("dve", (lambda o, a: (lambda e: e.reciprocal(o, a)))(F["nrm"][:], F["nrm"][:]), ["nrm"], ["nrm"])
                tt(S, "dve", F["kk"][:], F["kx"][:], F["nrm"][:], ALU.mult, r=["kx", "nrm"], w=["kk"])
                ts(S, "dve", F["t1"][:], F["af"][:], kac[:, pc, :], ALU.mult, s2=omka[:, pc, :], op1=ALU.add,
                   r=["af", "c_rwkv_k_a", "omka"], w=["t1"])
                tt(S, "pool", F["kmod"][:], F["zk"][:], F["t1"][:], ALU.mult, r=["zk", "t1"], w=["kmod"])
                tt(S, "pool", F["bq"][:], F["kk"][:], F["af"][:], ALU.mult, r=["kk", "af"], w=["bq"])
                tt(S, "pool", H["RT"][:], F["zr"][:], F["epos"][:], ALU.mult, r=["zr", "epos"], w=["RT"])
                stt(S, "dve", H["AT"][:], F["kk"][:], -1.0, F["eex"][:], ALU.mult, ALU.mult, r=["kk", "eex"], w=["AT"])
                tt(S, "pool", H["BT"][:], F["bq"][:], F["eneg"][:], ALU.mult, r=["bq", "eneg"], w=["BT"])
                tt(S, "pool", H["KT"][:], F["kmod"][:], F["eneg"][:], ALU.mult, r=["kmod", "eneg"], w=["KT"])
                tt(S, "pool", H["BH"][:], F["bq"][:], F["eh"][:], ALU.mult, r=["bq", "eh"], w=["BH"])
                tt(S, "pool", H["KH"][:], F["kmod"][:], F["eh"][:], ALU.mult, r=["kmod", "eh"], w=["KH"])
                cp(S, "act", H["VT"][:], F["zv"][:], r=["zv"], w=["VT"])
                stt(S, "dve", H["prod"][:], F["zr"][:], rkc[:, pc, :], F["kmod"][:], ALU.mult, ALU.mult,
                    r=["zr", "kmod", "c_rwkv_r_k"], w=["prod"])
                for sb in range(TB // 512):
                    c0 = sb * 512
                    p, pk = newps()
                    mm(S, p[:], K.blk_bf[:], H["prod"][:, c0:c0 + 512], r=["k_blk_bf", "prod"], w=[pk])
                    tt(S, "dve", H["BV"][:, c0:c0 + 512], p[:], F["zv"][:, c0:c0 + 512], ALU.mult, r=[pk, "zv"], w=["BV"])
                for nm, dst in (("RT", C.rRT), ("AT", C.rAT), ("BT", C.rBT), ("KT", C.rKT), ("BH", C.rBH), ("KH", C.rKH),
                                ("VT", C.rVT), ("BV", C.rBV), ("g", C.rG)):
                    S.dma("sp", dst[pcs, t0:t0 + TB], H[nm][:], reads=[nm], writes=["d_" + nm], defer=3)
        S.dma("sp", C.rGC.rearrange("p (a c) -> p a c", a=4), gC[:], reads=["gC"], writes=["rGC"])
    S.barrier()


def stage_rwkv_scan(C, l, limit=None):
    S = C.S
    K = C.K
    with ExitStack() as st:
        gC = S.sb([128, 4, NCH], F32, st)
        S.dma("sp", gC[:], C.rGC.rearrange("p (a c) -> p a c", a=4), writes=["gC"])
        Sf = S.sb([128, 4, CH], F32, st)
        Sb = S.sb([128, 4, CH], BF16, st)
        mset(S, "pool", Sf[:], 0.0, w=["Sf"])
        mset(S, "pool", Sb[:], 0.0, w=["Sb"])
        names = ("BT", "KT", "BH", "KH", "VT")
        srcs = dict(RT=C.rRT, AT=C.rAT, BT=C.rBT, KT=C.rKT, BH=C.rBH, KH=C.rKH, VT=C.rVT)
        G = {nm: [S.sb([128, 4, 512], BF16, st) for _ in range(2)] for nm in names}
        GM = {nm: [[S.sb([128, 4, 512], BF16, st) for _ in range(2)] for _hp in range(2)] for nm in ("AT", "RT")}
        for nm in ("AT", "RT"):
            for hp_ in range(2):
                for gb_ in range(2):
                    mset(S, "pool", GM[nm][hp_][gb_][:], 0.0, w=[f"GM{nm}{hp_}{gb_}"])
        yg = [S.sb([128, 4, 512], F32, st) for _ in range(2)]
        tokt = {nm: [S.sb([64, 512], BF16, st) for _ in range(2)] for nm in ("BH", "KH", "VT")}
        Xt = [S.sb([64, 512], BF16, st) for _ in range(2)]
        Yt = [S.sb([64, 512], BF16, st) for _ in range(2)]
        Pm = [[S.sb([64, 512], BF16, st) for _ in range(2)] for _par in range(2)]
        Lak = [S.sb([64, 512], BF16, st) for _ in range(2)]
        Mrb = [S.sb([64, 512], BF16, st) for _ in range(2)]
        Mrk = [S.sb([64, 512], BF16, st) for _ in range(2)]
        Wt = S.sb([64, 512], BF16, st)
        Ut = S.sb([64, 512], BF16, st)
        psp = [S.ps([128, 512], F32, st) for _ in range(5)]
        pss = [S.ps([128, 512], F32, st) for _ in range(3)]
        npp = [0]
        nss = [0]

        def newps():
            i_ = npp[0] % 5
            npp[0] += 1
            return psp[i_], f"psp{i_}"

        def newss():
            i_ = nss[0] % 3
            nss[0] += 1
            return pss[i_], f"pss{i_}"

        def load_group(g):
            gb = g % 2
            for nm in names:
                S.dma("sp", G[nm][gb][:], srcs[nm].rearrange("(c p) t -> p c t", p=128)[:, :, g * 512:(g + 1) * 512],
                      writes=[f"G{nm}{gb}"])
            for nm in ("AT", "RT"):
                for hp_ in range(2):
                    rs_ = slice(hp_ * 64, hp_ * 64 + 64)
                    S.dma("sp", GM[nm][hp_][gb][rs_, :, :],
                          srcs[nm].rearrange("(c p) t -> p c t", p=128)[rs_, :, g * 512:(g + 1) * 512],
                          writes=[f"GM{nm}{hp_}{gb}"])

        SU = K.m64[:, 0, :]
        SL = K.m64[:, 1, :]
        UI = K.m64[:, 2, :]
        I64 = K.m64[:, 3, :]
        nchunks = NCH if limit is None else limit
        fin = {}

        def prep(c):
            g, ci = c // 8, c % 8
            gb = g % 2
            par = c % 2
            tsl = slice(ci * CH, (ci + 1) * CH)
            gk = lambda nm: f"G{nm}{gb}"
            for nm in ("BH", "KH", "VT"):
                ptr_, pk = newps()
                pv = ptr_[:].bitcast(BF16)
                for pc in range(4):
                    tr(S, pv[0:64, pc * 128:(pc + 1) * 128], G[nm][gb][:, pc, tsl], K.ident_bf[:], r=[gk(nm)], w=[pk])
                cp(S, "act", tokt[nm][par][:], pv[0:64, 0:512], r=[pk], w=[f"tok{nm}{par}"])
            pX, pXk = newps()
            pY, pYk = newps()
            for h in range(8):
                hp, pc = h % 2, h // 2
                hc = slice(h * 64, (h + 1) * 64)
                A = GM["AT"][hp][gb][:, pc, tsl]
                Bt = G["BT"][gb][:, pc, tsl]
                ak = f"GMAT{hp}{gb}"
                mm(S, pX[0:64, hc], Bt, A, r=[gk("BT"), ak], w=[pXk])
                mm(S, pY[0:64, hc], A, Bt, r=[gk("BT"), ak], w=[pYk])
            X, Xk = Xt[0], "X0"
            Y, Yk = Yt[0], "Y0"
            tt(S, "dve", X[:], pX[0:64, :], SU, ALU.mult, r=[pXk, "k_m64"], w=[Xk])
            tt(S, "dve", Y[:], pY[0:64, :], SL, ALU.mult, r=[pYk, "k_m64"], w=[Yk])
            pL, pLk = newps()
            pRB, pRBk = newps()
            pRK, pRKk = newps()
            for h in range(8):
                hp, pc = h % 2, h // 2
                hc = slice(h * 64, (h + 1) * 64)
                A = GM["AT"][hp][gb][:, pc, tsl]
                Bt = G["BT"][gb][:, pc, tsl]
                Kt = G["KT"][gb][:, pc, tsl]
                R = GM["RT"][hp][gb][:, pc, tsl]
                ak = f"GMAT{hp}{gb}"
                rk_ = f"GMRT{hp}{gb}"
                mm(S, pL[0:64, hc], Kt, A, r=[gk("KT"), ak], w=[pLk])
                mm(S, pRB[0:64, hc], Bt, R, r=[gk("BT"), rk_], w=[pRBk])
                mm(S, pRK[0:64, hc], Kt, R, r=[gk("KT"), rk_], w=[pRKk])
            tt(S, "dve", Lak[par][:], pL[0:64, :], SU, ALU.mult, r=[pLk, "k_m64"], w=[f"Lak{par}"])
            tt(S, "dve", Mrb[par][:], pRB[0:64, :], UI, ALU.mult, r=[pRBk, "k_m64"], w=[f"Mrb{par}"])
            tt(S, "dve", Mrk[par][:], pRK[0:64, :], UI, ALU.mult, r=[pRKk, "k_m64"], w=[f"Mrk{par}"])
            P_, Pk = Pm[par][0], f"P{par}0"
            tt(S, "pool", P_[:], X[:], I64, ALU.add, r=[Xk, "k_m64"], w=[Pk])
            for k in range(1, NLEV + 1):
                nb = k % 2
                pYn, pYnk = newps()
                for h in range(8):
                    hc = slice(h * 64, (h + 1) * 64)
                    mm(S, pYn[0:64, hc], X[:, hc], Y[:, hc], r=[Xk, Yk], w=[pYnk])
                if k < NLEV:
                    pXn, pXnk = newps()
                    for h in range(8):
                        hc = slice(h * 64, (h + 1) * 64)
                        mm(S, pXn[0:64, hc], Y[:, hc], X[:, hc], r=[Xk, Yk], w=[pXnk])
                Yn, Ynk = Yt[nb], f"Y{nb}"
                cp(S, "act", Yn[:], pYn[0:64, :], r=[pYnk], w=[Ynk])
                if k < NLEV:
                    Xn, Xnk = Xt[nb], f"X{nb}"
                    cp(S, "dve", Xn[:], pXn[0:64, :], r=[pXnk], w=[Xnk])
                pP, pPk = newps()
                for h in range(8):
                    hc = slice(h * 64, (h + 1) * 64)
                    mm(S, pP[0:64, hc], Yn[:, hc], P_[:, hc], r=[Ynk, Pk], w=[pPk])
                Pn, Pnk = Pm[par][nb], f"P{par}{nb}"
                tt(S, "dve", Pn[:], pP[0:64, :], P_[:], ALU.add, r=[pPk, Pk], w=[Pnk])
                P_, Pk = Pn, Pnk
                Y, Yk = Yn, Ynk
                if k < NLEV:
                    X, Xk = Xn, Xnk
            fin[c] = (P_, Pk)

        def seq(c):
            g, ci = c // 8, c % 8
            gb = g % 2
            par = c % 2
            tsl = slice(ci * CH, (ci + 1) * CH)
            P_, Pk = fin[c]
            BHt, BHk = tokt["BH"][par], f"tokBH{par}"
            KHt, KHk = tokt["KH"][par], f"tokKH{par}"
            Vt, Vk = tokt["VT"][par], f"tokVT{par}"
            pW, pWk = newss()
            for h in range(8):
                hp, pc = h % 2, h // 2
                hc = slice(h * 64, (h + 1) * 64)
                mm(S, pW[0:64, hc], Lak[par][:, hc], Vt[:, hc], start=True, stop=False, r=[f"Lak{par}", Vk], w=[pWk])
                mm(S, pW[0:64, hc], GM["AT"][hp][gb][:, pc, tsl], Sb[:, pc, :], start=False, stop=True,
                   r=[f"GMAT{hp}{gb}", "Sb"], w=[pWk])
            cp(S, "act", Wt[:], pW[0:64, :], r=[pWk], w=["Wt"])
            pU, pUk = newss()
            for h in range(8):
                hc = slice(h * 64, (h + 1) * 64)
                mm(S, pU[0:64, hc], P_[:, hc], Wt[:, hc], r=[Pk, "Wt"], w=[pUk])
            cp(S, "act", Ut[:], pU[0:64, :], r=[pUk], w=["Ut"])
            pYT, pYTk = newss()
            pSn, pSnk = newss()
            for h in range(8):
                hp, pc = h % 2, h // 2
                r0 = hp * 64
                hc = slice(h * 64, (h + 1) * 64)
                oc = slice(pc * 64, (pc + 1) * 64)
                mm(S, pSn[r0:r0 + 64, oc], BHt[:, hc], Ut[:, hc], start=True, stop=False, r=[BHk, "Ut"], w=[pSnk])
                mm(S, pSn[r0:r0 + 64, oc], KHt[:, hc], Vt[:, hc], start=False, stop=True, r=[KHk, Vk], w=[pSnk])
            for h in range(8):
                hp, pc = h % 2, h // 2
                r0 = hp * 64
                hc = slice(h * 64, (h + 1) * 64)
                oc = slice(pc * 64, (pc + 1) * 64)
                mm(S, pYT[r0:r0 + 64, oc], Sb[:, pc, :], GM["RT"][hp][gb][:, pc, tsl], start=True, stop=False,
                   r=["Sb", f"GMRT{hp}{gb}"], w=[pYTk])
                mm(S, pYT[r0:r0 + 64, oc], Ut[:, hc], Mrb[par][:, hc], start=False, stop=False, r=["Ut", f"Mrb{par}"], w=[pYTk])
                mm(S, pYT[r0:r0 + 64, oc], Vt[:, hc], Mrk[par][:, hc], start=False, stop=True, r=[Vk, f"Mrk{par}"], w=[pYTk])
            for pc in range(4):
                stt(S, "dve", Sf[:, pc, :], Sf[:, pc, :], gC[:, pc, c:c + 1], pSn[:, pc * 64:(pc + 1) * 64], ALU.mult, ALU.add,
                    r=["Sf", "gC", pSnk], w=["Sf"])
            cp(S, "pool", Sb[:], Sf[:], r=["Sf"], w=["Sb"])
            cp(S, "act", yg[gb][:, :, tsl], pYT[:, 0:256].rearrange("p (a t) -> p a t", t=CH), r=[pYTk], w=[f"yg{gb}"])
            if ci == 7:
                S.dma("sp", C.rYT.rearrange("(c p) t -> p c t", p=128)[:, :, g * 512:(g + 1) * 512], yg[gb][:],
                      reads=[f"yg{gb}"], writes=["rYT"])

        def record(fn):
            lst = []
            S.add = lambda *a, **k: lst.append((a, k))
            try:
                fn()
            finally:
                del S.add
            return lst

        def replay(lst):
            for a, k in lst:
                S.add(*a, **k)

        load_group(0)
        replay(record(lambda: prep(0)))
        for c in range(nchunks):
            if c % 8 == 0 and c // 8 + 1 < NCH // 8:
                load_group(c // 8 + 1)
            ls = record(lambda: seq(c))
            lp = record(lambda: prep(c + 1)) if c + 1 < nchunks else []
            merged = []
            ns_, np_ = len(ls), len(lp)
            ip = 0
            for i_s, op_ in enumerate(ls):
                tgt = (i_s * np_) // max(ns_, 1)
                while ip < tgt:
                    merged.append(lp[ip])
                    ip += 1
                merged.append(op_)
            merged.extend(lp[ip:])
            replay(merged)
    S.barrier()


def stage_rwkv_post(C, l):
    S = C.S
    K = C.K
    inp = C.inp
    PB = 2048
    with ExitStack() as st:
        lnw = S.sb([128, 4, 1], F32, st)
        lnb = S.sb([128, 4, 1], F32, st)
        S.dma("sp", lnw[:], col_ap(inp["rwkv_ln_w"][l], 4), writes=["lnw"], slow=True)
        S.dma("sp", lnb[:], col_ap(inp["rwkv_ln_b"][l], 4), writes=["lnb"], slow=True)
        y = [S.sb([128, PB], F32, st) for _ in range(2)]
        bv = [S.sb([128, PB], BF16, st) for _ in range(2)]
        gg = [S.sb([128, PB], BF16, st) for _ in range(2)]
        cent = S.sb([128, PB], F32, st)
        sq = S.sb([128, PB], F32, st)
        sd = S.sb([128, PB], F32, st)
        ob = [S.sb([128, PB], BF16, st) for _ in range(2)]
        ps = [S.ps([128, 512], F32, st) for _ in range(4)]
        n = 0
        it = 0
        for pc in range(4):
            pcs = slice(pc * 128, (pc + 1) * 128)
            for blk in range(T // PB):
                b = it % 2
                it += 1
                t0 = blk * PB
                S.dma("sp", y[b][:], C.rYT[pcs, t0:t0 + PB], writes=[f"y{b}"])
                S.dma("sp", bv[b][:], C.rBV[pcs, t0:t0 + PB], writes=[f"bv{b}"])
                S.dma("sp", gg[b][:], C.rG[pcs, t0:t0 + PB], writes=[f"gg{b}"])
                for sb in range(PB // 512):
                    cs = slice(sb * 512, (sb + 1) * 512)
                    p, pk = ps[n % 4], f"psq{n % 4}"
                    n += 1
                    mm(S, p[:], K.blkmean_f[:], y[b][:, cs], r=["k_blkmean_f", f"y{b}"], w=[pk])
                    tt(S, "dve", cent[:, cs], y[b][:, cs], p[:], ALU.subtract, r=[pk, f"y{b}"], w=["cent"])
                    tt(S, "pool", sq[:, cs], cent[:, cs], cent[:, cs], ALU.mult, r=["cent"], w=["sq"])
                    p, pk = ps[n % 4], f"psq{n % 4}"
                    n += 1
                    mm(S, p[:], K.blkmean_f[:], sq[:, cs], r=["k_blkmean_f", "sq"], w=[pk])
                    act(S, sd[:, cs], p[:], AF.Sqrt, r=[pk], w=["sd"], bias=K.eps_gn[:])
                S.add("dve", (lambda o, a: (lambda e: e.reciprocal(o, a)))(sd[:], sd[:]), ["sd"], ["sd"])
                tt(S, "dve", cent[:], cent[:], sd[:], ALU.mult, r=["cent", "sd"], w=["cent"])
                ts(S, "dve", cent[:], cent[:], lnw[:, pc, :], ALU.mult, s2=lnb[:, pc, :], op1=ALU.add, r=["cent", "lnw", "lnb"],
                   w=["cent"])
                tt(S, "pool", cent[:], cent[:], bv[b][:], ALU.add, r=["cent", f"bv{b}"], w=["cent"])
                tt(S, "pool", ob[b][:], cent[:], gg[b][:], ALU.mult, r=["cent", f"gg{b}"], w=[f"ob{b}"])
                S.dma("sp", C.oT[512 + pc * 128:512 + (pc + 1) * 128, t0:t0 + PB], ob[b][:], reads=[f"ob{b}"], writes=["oT"], defer=3)
    S.barrier()


def build_layer(C, l, x_in, x_mid, x_out, stages=None):
    S = C.S
    stage_mod(C, l)
    with ExitStack() as lst:
        M = load_mod(C, l, lst)
        with ExitStack() as hst:
            hT = S.sb([128, 8, T], BF16, hst)
            stage_norm(C, x_in, M.A1, M.modcol[:, 0:8, :], hT, hst)
            stage_inproj(C, l, hT)
        stage_index(C, l)
        stage_attn(C, l)
        stage_rwkv_prep(C, l)
        stage_rwkv_scan(C, l)
        stage_rwkv_post(C, l)
        stage_conv(C, l)
        stage_merge(C, l, M, x_in, x_mid)
        with ExitStack() as hst:
            hT = S.sb([128, 8, T], BF16, hst)
            stage_norm(C, x_mid, M.A2, M.modcol[:, 24:32, :], hT, hst)
            stage_ffn_up(C, l, hT)
        stage_ffn_down(C, l, M, x_mid, x_out)


def build_program(debug_out=()):
    nc = bass.Bass("TRN2", target_bir_lowering=False)
    out = nc.dram_tensor("out", [T, D], F32, kind="ExternalOutput").ap()
    with ExitStack() as es:
        C = make_ctx(nc, es, debug_out)
        stage_consts(C)
        stage_bias(C)
        x = C.inp["x"]
        for l in range(DEPTH):
            build_layer(C, l, x, C.xs[2 * l], C.xs[2 * l + 1])
            x = C.xs[2 * l + 1]
        stage_final(C, x, out)
        C.S.emit()
        stats = C.S.stats
    return nc, stats


_PROG = None


def kernel(**inputs):
    global _PROG
    pos = np.asarray(inputs["positions"])
    if not np.array_equal(pos, np.broadcast_to(np.arange(T, dtype=pos.dtype), pos.shape)):
        raise NotImplementedError("kernel is specialised (at build time) for positions == arange(SEQ)")
    if _PROG is None:
        _PROG = build_program()[0]
    nb = np.asarray(inputs["x"]).shape[0]
    in_maps = [host_inputs(inputs, b) for b in range(nb)]
    res = run_bass_kernel_spmd(_PROG, in_maps, core_ids=list(range(nb)))
    return np.stack([np.asarray(r["out"], dtype=np.float32) for r in res.results], axis=0)
```

```python
import math
from contextlib import ExitStack

import numpy as np
import ml_dtypes
import concourse.bass as bass
import concourse.mybir as mybir
from concourse.bass_utils import run_bass_kernel_spmd

F32 = mybir.dt.float32
BF16 = mybir.dt.bfloat16
AF = mybir.ActivationFunctionType
ALU = mybir.AluOpType
AX = mybir.AxisListType

T = 4096
D = 1024
NT = 32
DEPTH = 2
IN_COLS = 8520
DFF = 2816
NEG = -1.0e30
CH = 64
NCH = T // CH
EDEC = math.exp(-0.5)

COMPUTE = ("pe", "act", "dve", "pool")
QUEUES = ("sp", "act", "pool")
NDSEM = 12


class Sched:
    def __init__(self, nc, es):
        self.nc = nc
        self.es = es
        self.ops = []
        self.last_w = {}
        self.readers = {}
        self.nbuf = 0
        self.gen = 0
        self.pending = []

    def sb(self, shape, dt, es=None, name="sb"):
        self.nbuf += 1
        return (es or self.es).enter_context(self.nc.sbuf_tensor(f"{name}_{self.nbuf}", list(shape), dt))

    def ps(self, shape, dt, es=None, name="ps"):
        self.nbuf += 1
        return (es or self.es).enter_context(self.nc.psum_tensor(f"{name}_{self.nbuf}", list(shape), dt))

    def barrier(self):
        for p in list(self.pending):
            self._flush(p)
        self.gen += 1
        self.last_w = {}
        self.readers = {}

    def add(self, eng, fn, reads=(), writes=(), dma=False):
        if self.pending:
            ws = set(writes)
            rs = set(reads)
            for p in list(self.pending):
                _, (_q, _fn, prd, pwr) = p
                if ws.intersection(prd) or ws.intersection(pwr) or rs.intersection(pwr):
                    self._flush(p)
        idx = len(self.ops)
        deps = set()
        for k in reads:
            j = self.last_w.get(k)
            if j is not None:
                deps.add(j)
        for k in writes:
            j = self.last_w.get(k)
            if j is not None:
                deps.add(j)
            for r in self.readers.get(k, ()):
                deps.add(r)
        for k in writes:
            self.last_w[k] = idx
            self.readers[k] = []
        for k in reads:
            lst = self.readers.setdefault(k, [])
            if not dma:
                lst[:] = [r for r in lst if not (self.ops[r]["eng"] == eng and not self.ops[r]["dma"])]
            lst.append(idx)
        deps.discard(idx)
        self.ops.append(dict(eng=eng, fn=fn, deps=deps, dma=dma, gen=self.gen))
        return idx

    def dma(self, q, out, in_, reads=(), writes=(), slow=False, defer=0):
        kw = {"allow_slow_non_contiguous": True} if slow else {}
        fn = lambda e: e.dma_start(out=out, in_=in_, **kw)
        if defer > 0:
            self.pending.append([defer, (q, fn, tuple(reads), tuple(writes))])
            return None
        idx = self.add(q, fn, reads, writes, dma=True)
        for p in list(self.pending):
            p[0] -= 1
            if p[0] <= 0:
                self._flush(p)
        return idx

    def _flush(self, p):
        if p in self.pending:
            self.pending.remove(p)
            q, fn, reads, writes = p[1]
            self.add(q, fn, reads, writes, dma=True)

    def emit(self):
        nc = self.nc
        es = self.es
        csem = {e: es.enter_context(nc.semaphore(f"c_{e}")) for e in COMPUTE}
        dsem = {q: [es.enter_context(nc.semaphore(f"d_{q}{i}")) for i in range(NDSEM)] for q in QUEUES}
        needed = [False] * len(self.ops)
        last_in_gen = {}
        for i, op in enumerate(self.ops):
            if op["dma"]:
                needed[i] = True
            else:
                last_in_gen[(op["eng"], op["gen"])] = i
            for j in op["deps"]:
                oj = self.ops[j]
                if (not oj["dma"]) and (not op["dma"]) and oj["eng"] == "pe" and op["eng"] == "pe":
                    continue
                needed[j] = True
        for i in last_in_gen.values():
            needed[i] = True
        self.needed = needed
        ccount = {e: 0 for e in COMPUTE}
        dcount = {q: 0 for q in QUEUES}
        dval = {q: [0] * NDSEM for q in QUEUES}
        done = []
        snaps = {}
        cur_gen = 0
        for i, op in enumerate(self.ops):
            if op["gen"] != cur_gen:
                cur_gen = op["gen"]
                snaps[cur_gen] = (dict(ccount), {q: list(v) for q, v in dval.items()})
            if op["dma"]:
                q = op["eng"]
                n = dcount[q]
                dcount[q] += 1
                k = n % NDSEM
                s = dsem[q][k]
                op["prev"] = (s, dval[q][k]) if dval[q][k] > 0 else None
                dval[q][k] += 16
                done.append((s, dval[q][k]))
            else:
                e = op["eng"]
                if needed[i]:
                    ccount[e] += 1
                done.append((csem[e], ccount[e]))
        streams = {e: [] for e in ("pe", "act", "dve", "pool", "sp")}
        for i, op in enumerate(self.ops):
            streams[op["eng"]].append(i)
        self.stats = {e: len(v) for e, v in streams.items()}
        self.stats["signaled"] = sum(needed)

        def run_stream(ename, eng):
            waited = {}
            my_gen = 0

            def do_wait(s, v):
                if v <= 0 or waited.get(id(s), 0) >= v:
                    return
                waited[id(s)] = v
                eng.wait_ge(s, v)

            for i in streams[ename]:
                op = self.ops[i]
                if op["gen"] != my_gen:
                    my_gen = op["gen"]
                    cc, dv = snaps[my_gen]
                    for e in COMPUTE:
                        if e != ename or e != "pe":
                            do_wait(csem[e], cc[e])
                    for q in QUEUES:
                        for k in range(NDSEM):
                            do_wait(dsem[q][k], dv[q][k])
                waits = []
                for j in sorted(op["deps"]):
                    oj = self.ops[j]
                    if (not oj["dma"]) and oj["eng"] == ename and ename == "pe" and not op["dma"]:
                        continue
                    waits.append(done[j])
                if op["dma"] and op["prev"] is not None:
                    waits.append(op["prev"])
                for s, v in waits:
                    do_wait(s, v)
                ins = op["fn"](eng)
                s, v = done[i]
                if needed[i]:
                    ins.then_inc(s, 16 if op["dma"] else 1)
            if ename in QUEUES:
                for k in range(NDSEM):
                    if dval[ename][k] > 0:
                        eng.wait_ge(dsem[ename][k], dval[ename][k])

        with nc.Block() as block:
            @block.tensor
            def _(eng):
                run_stream("pe", eng)

            @block.scalar
            def _(eng):
                run_stream("act", eng)

            @block.vector
            def _(eng):
                run_stream("dve", eng)

            @block.gpsimd
            def _(eng):
                run_stream("pool", eng)

            @block.sync
            def _(eng):
                run_stream("sp", eng)


def mm(S, out, lhsT, rhs, start=True, stop=True, r=(), w=()):
    S.add("pe", lambda e: e.matmul(out, lhsT, rhs, start=start, stop=stop), r, w)


def tr(S, out, in_, ident, r=(), w=()):
    S.add("pe", lambda e: e.transpose(out, in_, ident), r, w)


def act(S, out, in_, func, r=(), w=(), bias=None, scale=None, accum=None):
    kw = {}
    if bias is not None:
        kw["bias"] = bias
    if scale is not None:
        kw["scale"] = scale
    if accum is not None:
        kw["accum_out"] = accum
    S.add("act", lambda e: e.activation(out, in_, func, **kw), r, w)


def ts(S, eng, out, in0, s1, op0, s2=None, op1=None, r=(), w=(), accum=None):
    kw = {}
    if op1 is not None:
        kw["op1"] = op1
    if accum is not None:
        kw["accum_out"] = accum
    S.add(eng, lambda e: e.tensor_scalar(out, in0, s1, s2, op0, **kw), r, w)


def tt(S, eng, out, in0, in1, op, r=(), w=()):
    S.add(eng, lambda e: e.tensor_tensor(out, in0, in1, op), r, w)


def stt(S, eng, out, in0, scalar, in1, op0, op1, r=(), w=()):
    S.add(eng, lambda e: e.scalar_tensor_tensor(out, in0, scalar, in1, op0, op1), r, w)


def cp(S, eng, out, in_, r=(), w=()):
    if eng == "act":
        S.add("act", lambda e: e.activation(out, in_, AF.Copy), r, w)
    else:
        S.add(eng, lambda e: e.tensor_copy(out, in_), r, w)


def mset(S, eng, ap, val, w=()):
    S.add(eng, lambda e: e.memset(ap, val), (), w)


ZQ, ZK, ZQI, ZKI, ZRW, ZCV, ZGT, ZROWS = 0, 512, 1024, 1536, 1664, 3456, 4992, 8064
WQ, WK, WV, WQI, WKI, WWI, WRW, WCV, WGT = 0, 512, 1024, 1536, 2048, 2112, 2120, 3912, 5448


def t5_bucket_np(n):
    n = np.maximum(n, 0)
    nf = np.maximum(n, 1).astype(np.float32)
    large = 16 + (np.log(nf / np.float32(16)) / np.float32(math.log(128 / 16)) * np.float32(16)).astype(np.int32)
    large = np.minimum(large, 31)
    return np.where(n < 16, n, large)


def make_consts():
    bf = ml_dtypes.bfloat16
    c = {}
    c["ident_bf"] = np.eye(128, dtype=np.float32).astype(bf)
    c["ident_f"] = np.eye(128, dtype=np.float32)
    q = np.arange(128)[:, None]
    s = np.arange(128)[None, :]
    c["tri"] = (s <= q).astype(np.float32).astype(bf)
    blk = np.zeros((128, 128), np.float32)
    blk[:64, :64] = 1
    blk[64:, 64:] = 1
    c["blk_bf"] = blk.astype(bf)
    c["blkmean_f"] = (blk / 64.0).astype(np.float32)
    rm = np.ones((128, T), np.float32)
    rm[:, ::CH] = 0
    c["rmask"] = rm
    p = np.arange(64)[:, None]
    f = np.arange(64)[None, :]
    su = (p < f).astype(np.float32)
    sl = (f < p).astype(np.float32)
    ui = (p <= f).astype(np.float32)
    i64 = np.eye(64, dtype=np.float32)
    c["m64"] = np.stack([np.tile(m, (1, 8)) for m in (su, sl, ui, i64)], axis=1).astype(np.float32)
    u = np.arange(384)
    bk = t5_bucket_np(u - 127)
    oh = np.zeros((32, 384), np.float32)
    oh[bk, u] = 1.0
    oh[:, :127] = 0.0
    c["ohb"] = oh
    return c


CONST_SPECS = [("ident_bf", [128, 128], BF16), ("ident_f", [128, 128], F32), ("tri", [128, 128], BF16),
               ("blk_bf", [128, 128], BF16), ("blkmean_f", [128, 128], F32), ("rmask", [128, T], F32),
               ("m64", [64, 4, 512], F32), ("ohb", [32, 384], F32)]

INPUT_SPECS = [
    ("x", [T, D]), ("c", [D]), ("rel_bias", [32, 8]), ("final_norm", [D]),
    ("ada_w", [DEPTH, D, 6 * D]), ("ada_b", [DEPTH, 6 * D]), ("norm_mix", [DEPTH, D]),
    ("w_in", [DEPTH, D, IN_COLS]), ("rwkv_mu", [DEPTH, 1792]), ("rwkv_w0", [DEPTH, 512]),
    ("rwkv_w_up", [DEPTH, 64, 512]), ("rwkv_a0", [DEPTH, 512]), ("rwkv_a_up", [DEPTH, 64, 512]),
    ("rwkv_g_up", [DEPTH, 128, 512]), ("rwkv_k_k", [DEPTH, 512]), ("rwkv_k_a", [DEPTH, 512]),
    ("rwkv_r_k", [DEPTH, 512]), ("rwkv_ln_w", [DEPTH, 512]), ("rwkv_ln_b", [DEPTH, 512]),
    ("sc_conv_w", [DEPTH, 512, 3]), ("w_branch", [DEPTH, 3, 512, D]), ("w_o", [DEPTH, D, D]),
    ("norm_ffn", [DEPTH, D]), ("ffn_w_up", [DEPTH, D, 2 * DFF]), ("ffn_conv_w", [DEPTH, DFF, 3]),
    ("ffn_w_down", [DEPTH, DFF, D]),
]


class Ctx:
    pass


def col_ap(vec_ap, n):
    return vec_ap.rearrange("(j p o) -> p j o", p=128, o=1)


def stage_consts(C):
    S = C.S
    K = Ctx()
    C.K = K
    for name, shape, dt in CONST_SPECS:
        if name in ("rmask", "ohb"):
            continue
        tl = S.sb(shape, dt, name="k_" + name)
        S.dma("sp", tl[:], C.cin[name], writes=["k_" + name])
        setattr(K, name, tl)
    K.ones_bf = S.sb([128, 128], BF16, name="k_ones")
    mset(S, "pool", K.ones_bf[:], 1.0, w=["k_ones"])
    K.eps = S.sb([128, 1], F32, name="k_eps")
    mset(S, "pool", K.eps[:], 1e-6, w=["k_eps"])
    K.eps_gn = S.sb([128, 1], F32, name="k_epsgn")
    mset(S, "pool", K.eps_gn[:], 64e-5, w=["k_epsgn"])
    S.barrier()


def stage_mod(C, l):
    S = C.S
    inp = C.inp
    with ExitStack() as st:
        ccol = S.sb([128, 8, 1], F32, st)
        S.dma("sp", ccol[:], col_ap(inp["c"], 8), writes=["ccol"], slow=True)
        adab = S.sb([1, 6 * D], F32, st)
        S.dma("sp", adab[:], inp["ada_b"][l].rearrange("(o n) -> o n", o=1), writes=["adab"])
        row = S.sb([1, 6 * D], F32, st)
        aw = [S.sb([128, 6 * D], F32, st) for _ in range(2)]
        ps = [S.ps([128, 512], F32, st) for _ in range(6)]
        for half in range(2):
            for k in range(8):
                b = aw[k % 2]
                S.dma("sp", b[:], inp["ada_w"][l, k * 128:(k + 1) * 128, :], writes=[f"aw{k % 2}"])
                for jj in range(6):
                    j = half * 6 + jj
                    mm(S, ps[jj][0:1, :], ccol[:, k, :], b[:, j * 512:(j + 1) * 512], start=(k == 0), stop=(k == 7),
                       r=[f"aw{k % 2}", "ccol"], w=[f"psm{jj}"])
            for jj in range(6):
                j = half * 6 + jj
                tt(S, "dve", row[0:1, j * 512:(j + 1) * 512], ps[jj][0:1, :], adab[0:1, j * 512:(j + 1) * 512], ALU.add,
                   r=[f"psm{jj}", "adab"], w=["row"])
        S.dma("sp", C.modrow[l].rearrange("(o n) -> o n", o=1), row[:], reads=["row"], writes=["modrow"])
    S.barrier()


def load_mod(C, l, st):
    S = C.S
    inp = C.inp
    M = Ctx()
    modcol = S.sb([128, 48, 1], F32, st)
    S.dma("sp", modcol[:], col_ap(C.modrow[l], 48), writes=["modcol"], slow=True)
    nm = S.sb([128, 8, 1], F32, st)
    nf = S.sb([128, 8, 1], F32, st)
    S.dma("sp", nm[:], col_ap(inp["norm_mix"][l], 8), writes=["nm"], slow=True)
    S.dma("sp", nf[:], col_ap(inp["norm_ffn"][l], 8), writes=["nf"], slow=True)
    M.A1 = S.sb([128, 8, 1], F32, st)
    M.A2 = S.sb([128, 8, 1], F32, st)
    stt(S, "dve", M.A1[:], modcol[:, 8:16, :], 1.0, nm[:], ALU.add, ALU.mult, r=["modcol", "nm"], w=["A1"])
    stt(S, "dve", M.A2[:], modcol[:, 32:40, :], 1.0, nf[:], ALU.add, ALU.mult, r=["modcol", "nf"], w=["A2"])
    M.modcol = modcol
    M.g1 = S.sb([128, D], F32, st)
    M.g2 = S.sb([128, D], F32, st)
    S.dma("sp", M.g1[:], C.modrow[l][2 * D:3 * D].partition_broadcast(128), writes=["g1bc"])
    S.dma("sp", M.g2[:], C.modrow[l][5 * D:6 * D].partition_broadcast(128), writes=["g2bc"])
    S.barrier()
    return M


def stage_norm(C, x_ap, Acol, Bcol, hT, st_outer):
    S = C.S
    K = C.K
    with ExitStack() as st:
        xt = [S.sb([128, D], F32, st) for _ in range(2)]
        junk = S.sb([128, D], BF16, st)
        xn = [S.sb([128, D], BF16, st) for _ in range(2)]
        ss = [S.sb([128, 1], F32, st) for _ in range(2)]
        sd = [S.sb([128, 1], F32, st) for _ in range(2)]
        rs = [S.sb([128, 1], F32, st) for _ in range(2)]
        pT = [S.ps([128, 8, 128], BF16, st) for _ in range(2)]
        S.dma("sp", xt[0][:], x_ap[0:128, :], writes=["xt0"])
        for i in range(NT):
            b = i % 2
            if i + 1 < NT:
                S.dma("sp", xt[1 - b][:], x_ap[(i + 1) * 128:(i + 2) * 128, :], writes=[f"xt{1 - b}"])
            act(S, junk[:], xt[b][:], AF.Square, r=[f"xt{b}"], w=["junk", f"ss{b}"], accum=ss[b][:])
            act(S, sd[b][:], ss[b][:], AF.Sqrt, r=[f"ss{b}"], w=[f"sd{b}"], bias=K.eps[:], scale=1.0 / D)
            S.add("dve", (lambda o, i_: (lambda e: e.reciprocal(o, i_)))(rs[b][:], sd[b][:]), [f"sd{b}"], [f"rs{b}"])
            ts(S, "dve", xn[b][:], xt[b][:], rs[b][:], ALU.mult, r=[f"xt{b}", f"rs{b}"], w=[f"xn{b}"])
            for k in range(8):
                tr(S, pT[b][:, k, :], xn[b][:, k * 128:(k + 1) * 128], K.ident_bf[:], r=[f"xn{b}"], w=[f"pT{b}"])
            for k in range(8):
                eng = "dve" if k % 2 == 0 else "pool"
                if eng == "pool":
                    act(S, hT[:, k, i * 128:(i + 1) * 128], pT[b][:, k, :], AF.Identity, r=[f"pT{b}"], w=[f"hT{i}"],
                        bias=Bcol[:, k, :], scale=Acol[:, k, :])
                else:
                    ts(S, "dve", hT[:, k, i * 128:(i + 1) * 128], pT[b][:, k, :], Acol[:, k, :], ALU.mult, s2=Bcol[:, k, :],
                       op1=ALU.add, r=[f"pT{b}"], w=[f"hT{i}"])
    S.barrier()


def proj_fm(C, hT, w_ap, blocks, out_ap, st, scale=None):
    S = C.S
    wst = [S.sb([128, 8, 512], F32, st) for _ in range(2)]
    wbf = [S.sb([128, 8, 512], BF16, st) for _ in range(2)]
    zrow = [S.sb([128, T], BF16, st) for _ in range(2)]
    ps = [S.ps([128, 512], F32, st) for _ in range(4)]
    wv = w_ap.rearrange("(k p) c -> p k c", p=128)
    row0 = 0
    nev = 0
    nz = 0
    for bi, segs in enumerate(blocks):
        b = bi % 2
        off = 0
        for (c0, nc_) in segs:
            S.dma("sp", wst[b][:, :, off:off + nc_], wv[:, :, c0:c0 + nc_], writes=[f"wst{b}"])
            off += nc_
        cp(S, "pool", wbf[b][:, :, 0:off], wst[b][:, :, 0:off], r=[f"wst{b}"], w=[f"wbf{b}"])
        for cc in range(off // 128):
            zb = nz % 2
            nz += 1
            for tb in range(8):
                p = ps[nev % 4]
                pk = f"psp{nev % 4}"
                for k in range(8):
                    mm(S, p[:], wbf[b][:, k, cc * 128:(cc + 1) * 128], hT[:, k, tb * 512:(tb + 1) * 512],
                       start=(k == 0), stop=(k == 7), r=[f"wbf{b}", "hT"], w=[pk])
                if nev % 2 == 0:
                    act(S, zrow[zb][:, tb * 512:(tb + 1) * 512], p[:], AF.Copy, r=[pk], w=[f"zrow{zb}"])
                else:
                    cp(S, "dve", zrow[zb][:, tb * 512:(tb + 1) * 512], p[:], r=[pk], w=[f"zrow{zb}"])
                nev += 1
            S.dma("sp", out_ap[row0:row0 + 128, :], zrow[zb][:], reads=[f"zrow{zb}"], writes=["zout"], defer=2)
            row0 += 128


def stage_inproj(C, l, hT):
    S = C.S
    w = C.inp["w_in"][l]
    with ExitStack() as st:
        blocks = [[(WQ, 512)], [(WK, 512)], [(WQI, 512)], [(WKI, 64), (WKI, 64)]]
        blocks += [[(WRW + i * 512, 512)] for i in range(3)] + [[(WRW + 1536, 256)]]
        blocks += [[(WCV + i * 512, 512)] for i in range(3)]
        blocks += [[(WGT + i * 512, 512)] for i in range(6)]
        proj_fm(C, hT, w, blocks, C.zT, st)
    S.barrier()
    with ExitStack() as st:
        wv = w.rearrange("(k p) c -> p k c", p=128)
        wst = S.sb([128, 8, 520], F32, st)
        wbf = S.sb([128, 8, 520], BF16, st)
        S.dma("sp", wst[:, :, 0:512], wv[:, :, WV:WV + 512], writes=["wvst"])
        S.dma("sp", wst[:, :, 512:520], wv[:, :, WWI:WWI + 8], writes=["wvst"])
        cp(S, "pool", wbf[:], wst[:], r=["wvst"], w=["wvbf"])
        vt = [S.sb([128, 512], BF16, st) for _ in range(2)]
        wit = S.sb([128, NT, 8], F32, st)
        ps = [S.ps([128, 512], F32, st) for _ in range(2)]
        ps2 = [S.ps([128, 512], F32, st) for _ in range(2)]
        for i in range(NT):
            b = i % 2
            for k in range(8):
                mm(S, ps[b][:], hT[:, k, i * 128:(i + 1) * 128], wbf[:, k, 0:512], start=(k == 0), stop=(k == 7),
                   r=["wvbf"], w=[f"psv{b}"])
            for k in range(8):
                mm(S, ps2[b][:, 0:8], hT[:, k, i * 128:(i + 1) * 128], wbf[:, k, 512:520], start=(k == 0), stop=(k == 7),
                   r=["wvbf"], w=[f"psw{b}"])
            act(S, vt[b][:], ps[b][:], AF.Copy, r=[f"psv{b}"], w=[f"vt{b}"])
            ts(S, "dve", wit[:, i, :], ps2[b][:, 0:8], 8.0 ** -0.5, ALU.mult, r=[f"psw{b}"], w=["wit"])
            S.dma("sp", C.vtok[i * 128:(i + 1) * 128, :], vt[b][:], reads=[f"vt{b}"], writes=["vtok"])
        S.dma("sp", C.witok.rearrange("(i p) h -> p i h", p=128), wit[:], reads=["wit"], writes=["witok"])
    S.barrier()


def make_ctx(nc, es, debug_out=()):
    C = Ctx()
    C.nc = nc
    C.S = Sched(nc, es)
    C.inp = {}
    for name, shape in INPUT_SPECS:
        C.inp[name] = nc.dram_tensor(name, shape, F32, kind="ExternalInput").ap()
    C.cin = {}
    for name, shape, dt in CONST_SPECS:
        C.cin[name] = nc.dram_tensor("k_" + name, shape, dt, kind="ExternalInput").ap()
    C.debug_out = set(debug_out)

    def dram(name, shape, dt):
        kind = "ExternalOutput" if name in C.debug_out else "Internal"
        return nc.dram_tensor(name, list(shape), dt, kind=kind).ap()

    C.dram = dram
    C.modrow = [dram(f"modrow{l}", [6 * D], F32) for l in range(DEPTH)]
    C.zT = dram("zT", [ZROWS, T], BF16)
    C.vtok = dram("vtok", [T, 512], BF16)
    C.witok = dram("witok", [T, 8], F32)
    C.oT = dram("oT", [1536, T], BF16)
    C.uT = dram("uT", [DFF, T], BF16)
    C.xs = [dram(f"xs{i}", [T, D], F32) for i in range(2 * DEPTH)]
    C.maskT = dram("maskT", [128, 528 * 128], BF16)
    C.zdT = dram("zdT", [8, 128, 384], F32)
    NR = 512
    for nm in ("rRT", "rAT", "rBT", "rKT", "rBH", "rKH", "rVT", "rBV", "rG"):
        setattr(C, nm, dram(nm, [NR, T], BF16))
    C.rYT = dram("rYT", [NR, T], F32)
    C.rGC = dram("rGC", [128, 4 * NCH], F32)
    return C


def host_inputs(inputs, b):
    m = {}
    for name, shape in INPUT_SPECS:
        a = np.asarray(inputs[name])
        if name in ("x", "c"):
            a = a[b]
        m[name] = np.ascontiguousarray(a, dtype=np.float32)
    for k, v in make_consts().items():
        m["k_" + k] = v
    return m


def conv3(S, eng, acc, src, wcol, keys_r, key_w):
    ts(S, eng, acc[:, :], src[:, 2:T + 2], wcol[:, 2:3], ALU.mult, r=keys_r, w=[key_w])
    stt(S, eng, acc[:, :], src[:, 1:T + 1], wcol[:, 1:2], acc[:, :], ALU.mult, ALU.add, r=keys_r + [key_w], w=[key_w])
    stt(S, eng, acc[:, :], src[:, 0:T], wcol[:, 0:1], acc[:, :], ALU.mult, ALU.add, r=keys_r + [key_w], w=[key_w])


def stage_conv(C, l):
    S = C.S
    with ExitStack() as st:
        wc = S.sb([128, 4, 3], F32, st)
        S.dma("sp", wc[:], C.inp["sc_conv_w"][l].rearrange("(c p) j -> p c j", p=128), writes=["wc"])
        zb = [S.sb([128, T], BF16, st) for _ in range(2)]
        zc = [S.sb([128, T], BF16, st) for _ in range(2)]
        zx = [S.sb([128, T], BF16, st) for _ in range(2)]
        pp = [S.sb([128, T + 2], F32, st) for _ in range(2)]
        acc = [S.sb([128, T], F32, st) for _ in range(2)]
        ob = [S.sb([128, T], BF16, st) for _ in range(2)]
        for b in range(2):
            mset(S, "pool", pp[b][:, 0:2], 0.0, w=[f"pp{b}"])
        for cc in range(4):
            b = cc % 2
            S.dma("sp", zb[b][:], C.zT[ZCV + cc * 128:ZCV + (cc + 1) * 128, :], writes=[f"zb{b}"])
            S.dma("sp", zc[b][:], C.zT[ZCV + 512 + cc * 128:ZCV + 512 + (cc + 1) * 128, :], writes=[f"zc{b}"])
            S.dma("sp", zx[b][:], C.zT[ZCV + 1024 + cc * 128:ZCV + 1024 + (cc + 1) * 128, :], writes=[f"zx{b}"])
            tt(S, "pool", pp[b][:, 2:T + 2], zc[b][:], zx[b][:], ALU.mult, r=[f"zc{b}", f"zx{b}"], w=[f"pp{b}"])
            conv3(S, "dve", acc[b], pp[b], wc[:, cc, :], [f"pp{b}", "wc"], f"acc{b}")
            tt(S, "pool", ob[b][:], acc[b][:], zb[b][:], ALU.mult, r=[f"acc{b}", f"zb{b}"], w=[f"ob{b}"])
            S.dma("sp", C.oT[1024 + cc * 128:1024 + (cc + 1) * 128, :], ob[b][:], reads=[f"ob{b}"], writes=["oT"], defer=3)
    S.barrier()


def stage_merge(C, l, M, x_in, x_out):
    S = C.S
    inp = C.inp
    with ExitStack() as st:
        wb = S.sb([128, 12, D], BF16, st)
        wo = S.sb([128, 8, D], BF16, st)
        wst = [S.sb([128, 2, D], F32, st) for _ in range(2)]
        for i in range(10):
            b = i % 2
            if i < 6:
                src = inp["w_branch"][l, i // 2].rearrange("(k p) d -> p k d", p=128)[:, (i % 2) * 2:(i % 2) * 2 + 2, :]
                dst = wb[:, i * 2:i * 2 + 2, :]
            else:
                src = inp["w_o"][l].rearrange("(k p) d -> p k d", p=128)[:, (i - 6) * 2:(i - 6) * 2 + 2, :]
                dst = wo[:, (i - 6) * 2:(i - 6) * 2 + 2, :]
            S.dma("sp", wst[b][:], src, writes=[f"wst{b}"])
            cp(S, "pool", dst, wst[b][:], r=[f"wst{b}"], w=["wbo"])
        ot = [S.sb([128, 12, 512], BF16, st) for _ in range(2)]
        gt = [S.sb([128, 24, 512], BF16, st) for _ in range(1)]
        mg = [S.sb([128, 8, 512], BF16, st) for _ in range(2)]
        sg = [S.sb([128, 512], BF16, st) for _ in range(3)]
        mt = [S.sb([128, 512], F32, st) for _ in range(3)]
        m01 = S.sb([128, 512], F32, st)
        xt = [S.sb([128, D], F32, st) for _ in range(2)]
        tmp = [S.sb([128, D], F32, st) for _ in range(2)]
        xo = [S.sb([128, D], F32, st) for _ in range(2)]
        ps = [S.ps([128, 512], F32, st) for _ in range(6)]
        pso = [S.ps([128, 512], F32, st) for _ in range(2)]
        oTv = C.oT.rearrange("(c p) t -> p c t", p=128)
        gTv = C.zT[ZGT:ZGT + 3072, :].rearrange("(c p) t -> p c t", p=128)
        cnt = {"ps": 0, "po": 0, "x": 0}

        def branch(tb):
            b = tb % 2
            S.dma("sp", ot[b][:], oTv[:, :, tb * 512:(tb + 1) * 512], writes=[f"ot{b}"])
            S.dma("sp", gt[0][:], gTv[:, :, tb * 512:(tb + 1) * 512], writes=["gt0"])
            for dc in range(8):
                for i in range(3):
                    p = ps[cnt["ps"] % 6]
                    pk = f"psb{cnt['ps'] % 6}"
                    cnt["ps"] += 1
                    for kc in range(4):
                        mm(S, p[:], wb[:, i * 4 + kc, dc * 128:(dc + 1) * 128], ot[b][:, i * 4 + kc, :], start=(kc == 0),
                           stop=(kc == 3), r=["wbo", f"ot{b}"], w=[pk])
                    act(S, sg[i][:], gt[0][:, i * 8 + dc, :], AF.Sigmoid, r=["gt0"], w=[f"sg{i}"])
                    tt(S, "dve", mt[i][:], p[:], sg[i][:], ALU.mult, r=[pk, f"sg{i}"], w=[f"mt{i}"])
                tt(S, "pool", m01[:], mt[0][:], mt[1][:], ALU.add, r=["mt0", "mt1"], w=["m01"])
                tt(S, "pool", mg[b][:, dc, :], m01[:], mt[2][:], ALU.add, r=["m01", "mt2"], w=[f"mg{b}"])

        def wo_part(tb):
            b = tb % 2
            for t4 in range(4):
                xb = cnt["x"] % 2
                cnt["x"] += 1
                tok0 = tb * 512 + t4 * 128
                S.dma("sp", xt[xb][:], x_in[tok0:tok0 + 128, :], writes=[f"xt{xb}"])
                for nb in range(2):
                    p = pso[cnt["po"] % 2]
                    pk = f"pso{cnt['po'] % 2}"
                    cnt["po"] += 1
                    for dc in range(8):
                        mm(S, p[:], mg[b][:, dc, t4 * 128:(t4 + 1) * 128], wo[:, dc, nb * 512:(nb + 1) * 512], start=(dc == 0),
                           stop=(dc == 7), r=["wbo", f"mg{b}"], w=[pk])
                    tt(S, "dve", tmp[xb][:, nb * 512:(nb + 1) * 512], p[:], M.g1[:, nb * 512:(nb + 1) * 512], ALU.mult,
                       r=[pk, "g1bc"], w=[f"tmp{xb}"])
                tt(S, "pool", xo[xb][:], tmp[xb][:], xt[xb][:], ALU.add, r=[f"tmp{xb}", f"xt{xb}"], w=[f"xo{xb}"])
                S.dma("sp", x_out[tok0:tok0 + 128, :], xo[xb][:], reads=[f"xo{xb}"], writes=["xout"], defer=1)

        branch(0)
        for tb in range(8):
            if tb + 1 < 8:
                branch(tb + 1)
            wo_part(tb)
    S.barrier()


def stage_ffn_up(C, l, hT):
    S = C.S
    with ExitStack() as st:
        wca = S.sb([128, 22, 3], F32, st)
        S.dma("sp", wca[:], C.inp["ffn_conv_w"][l].rearrange("(c p) j -> p c j", p=128), writes=["wca"])
        wv = C.inp["ffn_w_up"][l].rearrange("(k p) c -> p k c", p=128)
        wst = [S.sb([128, 8, 256], F32, st) for _ in range(2)]
        wbf = [S.sb([128, 8, 256], BF16, st) for _ in range(2)]
        arow = [S.sb([128, T + 2], F32, st) for _ in range(2)]
        grow = [S.sb([128, T], BF16, st) for _ in range(2)]
        acc = [S.sb([128, T], F32, st)] * 2
        sl = [S.sb([128, T], BF16, st)] * 2
        ub = [S.sb([128, T], BF16, st) for _ in range(2)]
        ps = [S.ps([128, 512], F32, st) for _ in range(4)]
        for b in range(2):
            mset(S, "pool", arow[b][:, 0:2], 0.0, w=[f"arow{b}"])
        nps = 0
        for kc in range(22):
            b = kc % 2
            S.dma("sp", wst[b][:, :, 0:128], wv[:, :, kc * 128:(kc + 1) * 128], writes=[f"wst{b}"])
            S.dma("sp", wst[b][:, :, 128:256], wv[:, :, DFF + kc * 128:DFF + (kc + 1) * 128], writes=[f"wst{b}"])
            cp(S, "pool", wbf[b][:], wst[b][:], r=[f"wst{b}"], w=[f"wbf{b}"])
            for tb in range(8):
                for half in range(2):
                    p = ps[nps % 4]
                    pk = f"psu{nps % 4}"
                    nps += 1
                    for k in range(8):
                        mm(S, p[:], wbf[b][:, k, half * 128:(half + 1) * 128], hT[:, k, tb * 512:(tb + 1) * 512], start=(k == 0),
                           stop=(k == 7), r=[f"wbf{b}"], w=[pk])
                    if half == 0:
                        act(S, arow[b][:, 2 + tb * 512:2 + (tb + 1) * 512], p[:], AF.Copy, r=[pk], w=[f"arow{b}"])
                    else:
                        cp(S, "act", grow[b][:, tb * 512:(tb + 1) * 512], p[:], r=[pk], w=[f"grow{b}"])
            conv3(S, "dve", acc[b], arow[b], wca[:, kc, :], [f"arow{b}", "wca"], "acc0")
            act(S, sl[b][:], acc[b][:], AF.Silu, r=["acc0"], w=["sl0"])
            tt(S, "pool" if kc % 2 == 0 else "dve", ub[b][:], sl[b][:], grow[b][:], ALU.mult, r=["sl0", f"grow{b}"], w=[f"ub{b}"])
            S.dma("sp", C.uT[kc * 128:(kc + 1) * 128, :], ub[b][:], reads=[f"ub{b}"], writes=["uT"], defer=2)
    S.barrier()


def stage_ffn_down(C, l, M, x_in, x_out):
    S = C.S
    with ExitStack() as st:
        wd = S.sb([128, 22, D], BF16, st)
        wst = [S.sb([128, 2, D], F32, st) for _ in range(2)]
        wv = C.inp["ffn_w_down"][l].rearrange("(k p) d -> p k d", p=128)
        for i in range(11):
            b = i % 2
            S.dma("sp", wst[b][:], wv[:, 2 * i:2 * i + 2, :], writes=[f"wst{b}"])
            cp(S, "pool", wd[:, 2 * i:2 * i + 2, :], wst[b][:], r=[f"wst{b}"], w=["wd"])
        ut = [S.sb([128, 22, 512], BF16, st) for _ in range(2)]
        xt = [S.sb([128, D], F32, st) for _ in range(2)]
        tmp = [S.sb([128, D], F32, st) for _ in range(2)]
        xo = [S.sb([128, D], F32, st) for _ in range(2)]
        pso = [S.ps([128, 512], F32, st) for _ in range(4)]
        uTv = C.uT.rearrange("(c p) t -> p c t", p=128)
        npo = 0
        nx = 0
        for tb in range(8):
            b = tb % 2
            S.dma("sp", ut[b][:], uTv[:, :, tb * 512:(tb + 1) * 512], writes=[f"ut{b}"])
            for t4 in range(4):
                xb = nx % 2
                nx += 1
                tok0 = tb * 512 + t4 * 128
                S.dma("sp", xt[xb][:], x_in[tok0:tok0 + 128, :], writes=[f"xt{xb}"])
                for nb in range(2):
                    p = pso[npo % 4]
                    pk = f"pso{npo % 4}"
                    npo += 1
                    for kc in range(22):
                        mm(S, p[:], ut[b][:, kc, t4 * 128:(t4 + 1) * 128], wd[:, kc, nb * 512:(nb + 1) * 512], start=(kc == 0),
                           stop=(kc == 21), r=["wd", f"ut{b}"], w=[pk])
                    tt(S, "dve", tmp[xb][:, nb * 512:(nb + 1) * 512], p[:], M.g2[:, nb * 512:(nb + 1) * 512], ALU.mult,
                       r=[pk, "g2bc"], w=[f"tmp{xb}"])
                tt(S, "pool", xo[xb][:], tmp[xb][:], xt[xb][:], ALU.add, r=[f"tmp{xb}", f"xt{xb}"], w=[f"xo{xb}"])
                S.dma("sp", x_out[tok0:tok0 + 128, :], xo[xb][:], reads=[f"xo{xb}"], writes=["xout"], defer=1)
    S.barrier()


def stage_final(C, x_in, out_ap):
    S = C.S
    K = C.K
    with ExitStack() as st:
        fn = S.sb([128, D], F32, st)
        S.dma("sp", fn[:], C.inp["final_norm"].partition_broadcast(128), writes=["fn"])
        xt = [S.sb([128, D], F32, st) for _ in range(2)]
        junk = S.sb([128, D], BF16, st)
        y1 = [S.sb([128, D], F32, st) for _ in range(2)]
        y2 = [S.sb([128, D], F32, st) for _ in range(2)]
        ss = [S.sb([128, 1], F32, st) for _ in range(2)]
        sd = [S.sb([128, 1], F32, st) for _ in range(2)]
        rs = [S.sb([128, 1], F32, st) for _ in range(2)]
        for i in range(NT):
            b = i % 2
            S.dma("sp", xt[b][:], x_in[i * 128:(i + 1) * 128, :], writes=[f"xt{b}"])
            act(S, junk[:], xt[b][:], AF.Square, r=[f"xt{b}"], w=["junk", f"ss{b}"], accum=ss[b][:])
            act(S, sd[b][:], ss[b][:], AF.Sqrt, r=[f"ss{b}"], w=[f"sd{b}"], bias=K.eps[:], scale=1.0 / D)
            S.add("dve", (lambda o, i_: (lambda e: e.reciprocal(o, i_)))(rs[b][:], sd[b][:]), [f"sd{b}"], [f"rs{b}"])
            ts(S, "dve", y1[b][:], xt[b][:], rs[b][:], ALU.mult, r=[f"xt{b}", f"rs{b}"], w=[f"y1{b}"])
            tt(S, "pool", y2[b][:], y1[b][:], fn[:], ALU.mult, r=[f"y1{b}", "fn"], w=[f"y2{b}"])
            S.dma("sp", out_ap[i * 128:(i + 1) * 128, :], y2[b][:], reads=[f"y2{b}"], writes=["yout"], defer=1)
    S.barrier()


def stage_bias(C):
    S = C.S
    K = C.K
    K.corrD = S.sb([128, 8, 128], BF16, name="corrD")
    K.corrO = S.sb([128, 8, 128], BF16, name="corrO")
    K.rb31 = S.sb([128, 8], F32, name="rb31")
    with ExitStack() as st:
        rb = S.sb([32, 8], F32, st)
        ohb = S.sb([32, 384], F32, st)
        ones32 = S.sb([32, 128], F32, st)
        nrb = S.sb([128, 8], F32, st)
        S.dma("sp", rb[:], C.inp["rel_bias"], writes=["rb"])
        S.dma("sp", ohb[:], C.cin["ohb"], writes=["ohb"])
        S.dma("sp", K.rb31[:], C.inp["rel_bias"][31].partition_broadcast(128), writes=["rb31"])
        mset(S, "pool", ones32[:], 1.0, w=["ones32"])
        ts(S, "dve", nrb[:], K.rb31[:], -1.0, ALU.mult, r=["rb31"], w=["nrb"])
        lh = [S.sb([32, 128], F32, st) for _ in range(2)]
        zs = [S.sb([128, 384], F32, st) for _ in range(2)]
        ps = [S.ps([128, 512], F32, st) for _ in range(2)]
        for h in range(8):
            b = h % 2
            ts(S, "dve", lh[b][:], ones32[:], rb[:, h:h + 1], ALU.mult, r=["ones32", "rb"], w=[f"lh{b}"])
            mm(S, ps[b][:, 0:384], lh[b][:], ohb[:], r=[f"lh{b}", "ohb"], w=[f"psz{b}"])
            cp(S, "dve", zs[b][:], ps[b][:, 0:384], r=[f"psz{b}"], w=[f"zs{b}"])
            S.dma("sp", C.zdT[h], zs[b][:], reads=[f"zs{b}"], writes=["zdT"])
        S.barrier()
        td = [S.sb([128, 128], F32, st) for _ in range(2)]
        n = 0
        for h in range(8):
            for which, off0 in (("D", 127), ("O", 255)):
                b = n % 2
                n += 1
                src = bass.AP(tensor=C.zdT.tensor, offset=h * 128 * 384 + off0, ap=[[383, 128], [1, 128]])
                S.dma("sp", td[b][:], src, writes=[f"td{b}"])
                dst = (K.corrD if which == "D" else K.corrO)[:, h, :]
                act(S, dst, td[b][:], AF.Exp, r=[f"td{b}", "nrb"], w=["corr"], bias=nrb[:, h:h + 1])
    S.barrier()


NIT = 12
_FILL = {}


def _fill_reg(e):
    if id(e) not in _FILL:
        _FILL[id(e)] = e.to_reg(NEG)
    return _FILL[id(e)]


def stage_index(C, l):
    S = C.S
    K = C.K
    with ExitStack() as st:
        qiT = S.sb([128, 4, T], BF16, st)
        kiT = S.sb([128, T], BF16, st)
        wi = S.sb([128, NT, 8], F32, st)
        S.dma("sp", qiT[:], C.zT[ZQI:ZQI + 512, :].rearrange("(c p) t -> p c t", p=128), writes=["qiT"])
        S.dma("sp", kiT[:], C.zT[ZKI:ZKI + 128, :], writes=["kiT"])
        S.dma("sp", wi[:], C.witok.rearrange("(i p) h -> p i h", p=128), writes=["wi"])
        Iacc = [S.sb([128, T], F32, st) for _ in range(2)]
        rl = [S.sb([128, 512], F32, st) for _ in range(3)]
        cmpj = S.sb([128, T], BF16, st)
        mask = [S.sb([128, T], BF16, st) for _ in range(2)]
        mT = [S.sb([128, NT, 128], BF16, st) for _ in range(2)]
        sm = {nm: [S.sb([128, 1], F32, st) for _ in range(2)] for nm in ("rmax", "rmin", "lo", "w", "mid", "cnt", "ge", "sgn", "tot")}
        cmpa = S.sb([128, T], BF16, st)
        pw2 = S.sb([128, NIT], F32, st)
        for k_ in range(NIT):
            mset(S, "pool", pw2[:, k_:k_ + 1], 2.0 ** -(k_ + 1), w=["pw2"])
        wtab = [S.sb([128, NIT], F32, st) for _ in range(2)]
        psI = [S.ps([128, 512], F32, st) for _ in range(3)]
        psT = [S.ps([128, 4, 128], BF16, st) for _ in range(2)]
        off = 0
        n = 0
        ng = 0
        for i in range(NT):
            L = (i + 1) * 128
            b = i % 2
            mk = f"mask{b}"
            if i >= 2:
                nchunk = (L + 511) // 512
                for h in range(8):
                    hp, hc = h % 2, h // 2
                    r0 = hp * 64
                    for ch in range(nchunk):
                        w_ = min(512, L - ch * 512)
                        p = psI[n % 3]
                        pk = f"psI{n % 3}"
                        rt = rl[n % 3]
                        rk = f"rl{n % 3}"
                        n += 1
                        mm(S, p[:, 0:w_], qiT[r0:r0 + 64, hc, i * 128:(i + 1) * 128], kiT[r0:r0 + 64, ch * 512:ch * 512 + w_],
                           r=["qiT", "kiT"], w=[pk])
                        act(S, rt[:, 0:w_], p[:, 0:w_], AF.Relu, r=[pk], w=[rk], scale=0.125)
                        ik = f"I{b}_{ch}"
                        dst = Iacc[b][:, ch * 512:ch * 512 + w_]
                        if h == 0:
                            ts(S, "dve", dst, rt[:, 0:w_], wi[:, i, 0:1], ALU.mult, r=[rk, "wi"], w=[ik])
                        else:
                            stt(S, "dve", dst, rt[:, 0:w_], wi[:, i, h:h + 1], dst, ALU.mult, ALU.add, r=[rk, "wi", ik], w=[ik])
                allI = [f"I{b}_{ch}" for ch in range(nchunk)]
                dg = Iacc[b][:, i * 128:L]
                S.add("pool", (lambda o: (lambda e: e.affine_select(out=o, in_=o, pattern=[[-1, 128]], compare_op=ALU.is_ge,
                                                                    fill=_fill_reg(e), base=0, channel_multiplier=1)))(dg),
                      allI, allI)
                rmax, rmin, lo, wd_, mid, cnt, ge = (sm[nm][b] for nm in ("rmax", "rmin", "lo", "w", "mid", "cnt", "ge"))
                sk = f"sm{b}"
                S.add("dve", (lambda o, a: (lambda e: e.tensor_reduce(out=o, in_=a, axis=AX.X, op=ALU.max)))(rmax[:], Iacc[b][:, 0:L]),
                      allI, [sk + "rmax"])
                S.add("dve", (lambda o, a: (lambda e: e.tensor_reduce(out=o, in_=a, axis=AX.X, op=ALU.min)))(rmin[:], Iacc[b][:, 0:i * 128]),
                      allI, [sk + "rmin"])
                ts(S, "dve", lo[:], rmin[:], -1.0, ALU.add, r=[sk + "rmin"], w=[sk + "lo"])
                tt(S, "dve", wd_[:], rmax[:], lo[:], ALU.subtract, r=[sk + "rmax", sk + "lo"], w=[sk + "w"])
                ts(S, "dve", wtab[b][:], pw2[:], wd_[:], ALU.mult, r=["pw2", sk + "w"], w=[sk + "wtab"])
                stt(S, "dve", mid[:], wd_[:], 0.5, lo[:], ALU.mult, ALU.add, r=[sk + "w", sk + "lo"], w=[sk + "mid"])
                La = ((L // 128) // 2) * 128
                sgn, tot = sm["sgn"][b], sm["tot"][b]
                for it in range(NIT):
                    ts(S, "dve", cmpj[:, 0:La], Iacc[b][:, 0:La], mid[:], ALU.is_gt, op1=ALU.add, r=allI + [sk + "mid"],
                       w=["cmpj", sk + "cnt"], accum=cnt[:])
                    act(S, cmpa[:, La:L], Iacc[b][:, La:L], AF.Sign, r=allI + [sk + "mid"], w=["cmpa", sk + "sgn"], bias=mid[:],
                        scale=-1.0, accum=sgn[:])
                    stt(S, "dve", tot[:], sgn[:], -0.5, cnt[:], ALU.mult, ALU.add, r=[sk + "sgn", sk + "cnt"], w=[sk + "tot"])
                    ts(S, "dve", ge[:], tot[:], 255.5 - 0.5 * (L - La), ALU.is_gt, s2=0.5, op1=ALU.subtract, r=[sk + "tot"],
                       w=[sk + "ge"])
                    stt(S, "dve", mid[:], ge[:], wtab[b][:, it:it + 1], mid[:], ALU.mult, ALU.add,
                        r=[sk + "ge", sk + "wtab", sk + "mid"], w=[sk + "mid"])
                ts(S, "dve", mask[b][:, 0:L], Iacc[b][:, 0:L], mid[:], ALU.is_gt, r=allI + [sk + "mid"], w=[mk])
            elif i == 0:
                cp(S, "dve", mask[b][:, 0:128], K.tri[:], r=["k_tri"], w=[mk])
            else:
                cp(S, "dve", mask[b][:, 0:128], K.ones_bf[:], r=["k_ones"], w=[mk])
                cp(S, "dve", mask[b][:, 128:256], K.tri[:], r=["k_tri"], w=[mk])
            for g in range((i + 4) // 4):
                nb_ = min(4, i + 1 - 4 * g)
                pt = psT[ng % 2]
                ptk = f"psT{ng % 2}"
                ng += 1
                for jj in range(nb_):
                    tr(S, pt[:, jj, :], mask[b][:, (4 * g + jj) * 128:(4 * g + jj + 1) * 128], K.ident_bf[:], r=[mk], w=[ptk])
                cp(S, "act", mT[b][:, 4 * g:4 * g + nb_, :], pt[:, 0:nb_, :], r=[ptk], w=[f"mT{b}"])
            S.dma("sp", C.maskT[:, off * 128:(off + i + 1) * 128].rearrange("p (j q) -> p j q", q=128), mT[b][:, 0:i + 1, :],
                  reads=[f"mT{b}"], writes=["maskT"])
            off += i + 1
    S.barrier()


def stage_attn(C, l):
    S = C.S
    K = C.K
    with ExitStack() as st:
        qT = S.sb([128, 4, T], BF16, st)
        kT = S.sb([128, 4, T], BF16, st)
        V = S.sb([128, NT, 512], BF16, st)
        S.dma("sp", qT[:], C.zT[ZQ:ZQ + 512, :].rearrange("(c p) t -> p c t", p=128), writes=["qT"])
        S.dma("sp", kT[:], C.zT[ZK:ZK + 512, :].rearrange("(c p) t -> p c t", p=128), writes=["kT"])
        S.dma("sp", V[:], C.vtok.rearrange("(j p) c -> p j c", p=128), writes=["V"])
        mk = [S.sb([128, NT, 128], BF16, st) for _ in range(2)]
        E = [S.sb([128, 4, 128], BF16, st) for _ in range(3)]
        P = [S.sb([128, 4, 128], BF16, st) for _ in range(3)]
        rec = [S.sb([128, 128], F32, st) for _ in range(2)]
        ob = [S.sb([128, 4, 128], BF16, st) for _ in range(2)]
        psS = [S.ps([128, 4, 128], F32, st) for _ in range(3)]
        psN = [S.ps([128, 512], F32, st) for _ in range(2)]
        psD = [S.ps([128, 512], F32, st) for _ in range(2)]
        oTv = C.oT[0:512, :].rearrange("(c p) t -> p c t", p=128)
        offs = []
        off = 0
        for i in range(NT):
            offs.append(off)
            off += i + 1

        def load_mask(i):
            b = i % 2
            nblk = i + 1
            S.dma("sp", mk[b][:, 0:nblk, :],
                  C.maskT[:, offs[i] * 128:(offs[i] + nblk) * 128].rearrange("p (j q) -> p j q", q=128), writes=[f"mk{b}"])

        items = []
        for i in range(NT):
            for h in range(8):
                ng_ = (i + 4) // 4
                for g in range(ng_):
                    items.append((i, h, g, g == ng_ - 1))

        def emit_st(n, it):
            i, h, g, _ = it
            hp, hc = h % 2, h // 2
            r0 = hp * 64
            nb_ = min(4, i + 1 - 4 * g)
            ps_ = psS[n % 3]
            for jj in range(nb_):
                j = 4 * g + jj
                mm(S, ps_[:, jj, :], kT[r0:r0 + 64, hc, j * 128:(j + 1) * 128], qT[r0:r0 + 64, hc, i * 128:(i + 1) * 128],
                   r=["qT", "kT"], w=[f"psS{n % 3}"])

        def emit_rest(n, it):
            i, h, g, last = it
            b = i % 2
            hp, hc = h % 2, h // 2
            r0 = hp * 64
            nb_ = min(4, i + 1 - 4 * g)
            ps_ = psS[n % 3]
            psk = f"psS{n % 3}"
            e_, ek = E[n % 3], f"E{n % 3}"
            p_, pk = P[n % 3], f"P{n % 3}"
            pn, pd = psN[hc % 2], psD[hc % 2]
            pnk, pdk = f"psN{hc % 2}", f"psD{hc % 2}"
            act(S, e_[:, 0:nb_, :], ps_[:, 0:nb_, :], AF.Exp, r=[psk, "rb31"], w=[ek], bias=K.rb31[:, h:h + 1], scale=0.125)
            tt(S, "dve", p_[:, 0:nb_, :], e_[:, 0:nb_, :], mk[b][:, 4 * g:4 * g + nb_, :], ALU.mult, r=[ek, f"mk{b}"], w=[pk])
            for jj in range(nb_):
                j = 4 * g + jj
                if j == i:
                    tt(S, "pool", p_[:, jj, :], p_[:, jj, :], K.corrD[:, h, :], ALU.mult, r=[pk, "corr"], w=[pk])
                elif j == i - 1:
                    tt(S, "pool", p_[:, jj, :], p_[:, jj, :], K.corrO[:, h, :], ALU.mult, r=[pk, "corr"], w=[pk])
            for jj in range(nb_):
                j = 4 * g + jj
                mm(S, pn[r0:r0 + 64, 0:128], V[:, j, h * 64:(h + 1) * 64], p_[:, jj, :], start=(j == 0), stop=(j == i),
                   r=["V", pk], w=[pnk])
            mm(S, pd[r0:r0 + 64, 0:nb_ * 128], K.ones_bf[:, 0:64], p_[:].rearrange("p j q -> p (j q)")[:, 0:nb_ * 128],
               start=(g == 0), stop=last, r=["k_ones", pk], w=[pdk])
            if last and hp == 1:
                rc = rec[hc % 2]
                rck = f"rec{hc % 2}"
                nj = min(4, i + 1)
                S.add("dve", (lambda o, a: (lambda e: e.tensor_reduce(out=o, in_=a, axis=AX.X, op=ALU.add)))(
                    rc[:], pd[:, 0:nj * 128].rearrange("p (j q) -> p q j", j=nj)), [pdk], [rck])
                S.add("dve", (lambda o, a: (lambda e: e.reciprocal(o, a)))(rc[:], rc[:]), [rck], [rck])
                tt(S, "dve", ob[b][:, hc, :], pn[:, 0:128], rc[:], ALU.mult, r=[pnk, rck], w=[f"ob{b}"])
            if last and h == 7:
                S.dma("sp", oTv[:, :, i * 128:(i + 1) * 128], ob[b][:], reads=[f"ob{b}"], writes=["oT"])

        load_mask(0)
        load_mask(1)
        emit_st(0, items[0])
        emit_st(1, items[1])
        for n, it in enumerate(items):
            if n + 2 < len(items):
                emit_st(n + 2, items[n + 2])
            emit_rest(n, it)
            i, h, g, last = it
            if last and h == 7 and i + 2 < NT:
                load_mask(i + 2)
    S.barrier()


TB = 1024
NLEV = 4


def stage_rwkv_prep(C, l):
    S = C.S
    K = C.K
    inp = C.inp
    with ExitStack() as st:
        def colp(name, n):
            t_ = S.sb([128, n, 1], F32, st)
            S.dma("sp", t_[:], col_ap(inp[name][l], n), writes=["c_" + name], slow=True)
            return t_
        mu = colp("rwkv_mu", 14)
        w0 = colp("rwkv_w0", 4)
        a0 = colp("rwkv_a0", 4)
        kkc = colp("rwkv_k_k", 4)
        kac = colp("rwkv_k_a", 4)
        rkc = colp("rwkv_r_k", 4)
        omka = S.sb([128, 4, 1], F32, st)
        ts(S, "dve", omka[:], kac[:], -1.0, ALU.mult, s2=1.0, op1=ALU.add, r=["c_rwkv_k_a"], w=["omka"])
        wa_st = S.sb([128, 512], F32, st)
        gu_st = S.sb([128, 512], F32, st)
        wa = S.sb([128, 512], BF16, st)
        gu = S.sb([128, 512], BF16, st)
        S.dma("sp", wa_st[0:64, :], inp["rwkv_w_up"][l], writes=["wa_st"])
        S.dma("sp", wa_st[64:128, :], inp["rwkv_a_up"][l], writes=["wa_st"])
        S.dma("sp", gu_st[:], inp["rwkv_g_up"][l], writes=["gu_st"])
        cp(S, "dve", wa[:], wa_st[:], r=["wa_st"], w=["wa"])
        cp(S, "dve", gu[:], gu_st[:], r=["gu_st"], w=["gu"])
        rmask = S.sb([128, TB], F32, st)
        S.dma("sp", rmask[:], C.cin["rmask"][:, 0:TB], writes=["rmask"])
        gC = S.sb([128, 4, NCH], F32, st)
        xwa = S.sb([128, T], BF16, st)
        sg = S.sb([128, T], BF16, st)
        raw = [S.sb([128, TB + 1], BF16, st) for _ in range(3)]
        F = {nm: S.sb([128, TB], F32, st, name=nm) for nm in
             ("zr", "zk", "zv", "d", "sgw", "af", "lw", "cum", "ex", "epos", "eneg", "eex", "eh", "kx", "nrm", "kk", "t1",
              "kmod", "bq")}
        H = {nm: S.sb([128, TB], BF16, st, name=nm) for nm in
             ("sq", "prod", "g", "RT", "AT", "BT", "KT", "BH", "KH", "VT", "BV")}
        ps = [S.ps([128, 512], F32, st) for _ in range(4)]
        nps = [0]

        def newps():
            i_ = nps[0] % 4
            nps[0] += 1
            return ps[i_], f"psr{i_}"

        def lerp(ci, t0, rawt, rk, out, ok):
            row0 = ZRW + ci * 128
            if t0 == 0:
                mset(S, "pool", rawt[:, 0:1], 0.0, w=[rk])
                S.dma("sp", rawt[:, 1:TB + 1], C.zT[row0:row0 + 128, 0:TB], writes=[rk])
            else:
                S.dma("sp", rawt[:, 0:TB + 1], C.zT[row0:row0 + 128, t0 - 1:t0 + TB], writes=[rk])
            tt(S, "dve", F["d"][:], rawt[:, 0:TB], rawt[:, 1:TB + 1], ALU.subtract, r=[rk], w=["d"])
            stt(S, "dve", out, F["d"][:], mu[:, ci, :], rawt[:, 1:TB + 1], ALU.mult, ALU.add, r=["d", rk, "c_rwkv_mu"], w=[ok])

        for tb in range(T // TB):
            t0 = tb * TB
            lerp(12, t0, raw[0], "raw0", F["zr"][:], "zr")
            act(S, xwa[0:64, t0:t0 + TB], F["zr"][0:64, :], AF.Tanh, r=["zr"], w=["xwa"])
            cp(S, "dve", xwa[64:128, t0:t0 + TB], F["zr"][64:128, :], r=["zr"], w=["xwa"])
            lerp(13, t0, raw[1], "raw1", F["zk"][:], "zk")
            act(S, sg[:, t0:t0 + TB], F["zk"][:], AF.Sigmoid, r=["zk"], w=["sg"])

        for pc in range(4):
            pcs = slice(pc * 128, (pc + 1) * 128)
            for tb in range(T // TB):
                t0 = tb * TB
                lerp(pc, t0, raw[0], "raw0", F["zr"][:], "zr")
                lerp(4 + pc, t0, raw[1], "raw1", F["zk"][:], "zk")
                lerp(8 + pc, t0, raw[2], "raw2", F["zv"][:], "zv")
                for sb in range(TB // 512):
                    c0 = sb * 512
                    tcs = slice(t0 + c0, t0 + c0 + 512)
                    p, pk = newps()
                    mm(S, p[:], wa[0:64, pcs], xwa[0:64, tcs], r=["wa", "xwa"], w=[pk])
                    act(S, F["sgw"][:, c0:c0 + 512], p[:], AF.Sigmoid, r=[pk, "c_rwkv_w0"], w=["sgw"], bias=w0[:, pc, :])
                    p, pk = newps()
                    mm(S, p[:], wa[64:128, pcs], xwa[64:128, tcs], r=["wa", "xwa"], w=[pk])
                    act(S, F["af"][:, c0:c0 + 512], p[:], AF.Sigmoid, r=[pk, "c_rwkv_a0"], w=["af"], bias=a0[:, pc, :])
                    p, pk = newps()
                    mm(S, p[:], gu[:, pcs], sg[:, tcs], r=["gu", "sg"], w=[pk])
                    cp(S, "dve", H["g"][:, c0:c0 + 512], p[:], r=[pk], w=["g"])
                ts(S, "pool", F["lw"][:], F["sgw"][:], -EDEC, ALU.mult, r=["sgw"], w=["lw"])
                S.add("dve", (lambda o, a, b_: (lambda e: e.tensor_tensor_scan(o, a, b_, 0.0, ALU.mult, ALU.add)))(
                    F["cum"][:], rmask[:], F["lw"][:]), ["rmask", "lw"], ["cum"])
                cumv = F["cum"][:].rearrange("p (c t) -> p c t", t=CH)
                act(S, gC[:, pc, tb * (TB // CH):(tb + 1) * (TB // CH)], F["cum"][:, CH - 1:TB:CH], AF.Exp, r=["cum"], w=["gC"])
                act(S, F["epos"][:], F["cum"][:], AF.Exp, r=["cum"], w=["epos"])
                act(S, F["eneg"][:], F["cum"][:], AF.Exp, r=["cum"], w=["eneg"], scale=-1.0)
                tt(S, "pool", F["ex"][:], F["cum"][:], F["lw"][:], ALU.subtract, r=["cum", "lw"], w=["ex"])
                act(S, F["eex"][:], F["ex"][:], AF.Exp, r=["ex"], w=["eex"])
                tt(S, "dve", F["eh"][:].rearrange("p (c t) -> p c t", t=CH), cumv[:, :, CH - 1:CH].broadcast_to([128, TB // CH, CH]),
                   cumv, ALU.subtract, r=["cum"], w=["ehx"])
                act(S, F["eh"][:], F["eh"][:], AF.Exp, r=["ehx"], w=["eh"])
                ts(S, "dve", F["kx"][:], F["zk"][:], kkc[:, pc, :], ALU.mult, r=["zk", "c_rwkv_k_k"], w=["kx"])
                tt(S, "pool", H["sq"][:], F["kx"][:], F["kx"][:], ALU.mult, r=["kx"], w=["sq"])
                for sb in range(TB // 512):
                    c0 = sb * 512
                    p, pk = newps()
                    mm(S, p[:], K.blk_bf[:], H["sq"][:, c0:c0 + 512], r=["k_blk_bf", "sq"], w=[pk])
                    act(S, F["nrm"][:, c0:c0 + 512], p[:], AF.Sqrt, r=[pk], w=["nrm"])
                ts(S, "dve", F["nrm"][:], F["nrm"][:], 1e-12, ALU.max, r=["nrm"], w=["nrm"])
                S.add("dve", (lambda o, a: (lambda e: e.reciprocal(o, a)))(F["nrm"][:], F["nrm"][:]), ["nrm"], ["nrm"])
                tt(S, "dve", F["kk"][:], F["kx"][:], F["nrm"][:], ALU.mult, r=["kx", "nrm"], w=["kk"])
                ts(S, "dve", F["t1"][:], F["af"][:], kac[:, pc, :], ALU.mult, s2=omka[:, pc, :], op1=ALU.add,
                   r=["af", "c_rwkv_k_a", "omka"], w=["t1"])
                tt(S, "pool", F["kmod"][:], F["zk"][:], F["t1"][:], ALU.mult, r=["zk", "t1"], w=["kmod"])
                tt(S, "pool", F["bq"][:], F["kk"][:], F["af"][:], ALU.mult, r=["kk", "af"], w=["bq"])
                tt(S, "pool", H["RT"][:], F["zr"][:], F["epos"][:], ALU.mult, r=["zr", "epos"], w=["RT"])
                stt(S, "dve", H["AT"][:], F["kk"][:], -1.0, F["eex"][:], ALU.mult, ALU.mult, r=["kk", "eex"], w=["AT"])
                tt(S, "pool", H["BT"][:], F["bq"][:], F["eneg"][:], ALU.mult, r=["bq", "eneg"], w=["BT"])
                tt(S, "pool", H["KT"][:], F["kmod"][:], F["eneg"][:], ALU.mult, r=["kmod", "eneg"], w=["KT"])
                tt(S, "pool", H["BH"][:], F["bq"][:], F["eh"][:], ALU.mult, r=["bq", "eh"], w=["BH"])
                tt(S, "pool", H["KH"][:], F["kmod"][:], F["eh"][:], ALU.mult, r=["kmod", "eh"], w=["KH"])
                cp(S, "act", H["VT"][:], F["zv"][:], r=["zv"], w=["VT"])
                stt(S, "dve", H["prod"][:], F["zr"][:], rkc[:, pc, :], F["kmod"][:], ALU.mult, ALU.mult,
                    r=["zr", "kmod", "c_rwkv_r_k"], w=["prod"])
                for sb in range(TB // 512):
                    c0 = sb * 512
                    p, pk = newps()
                    mm(S, p[:], K.blk_bf[:], H["prod"][:, c0:c0 + 512], r=["k_blk_bf", "prod"], w=[pk])
                    tt(S, "dve", H["BV"][:, c0:c0 + 512], p[:], F["zv"][:, c0:c0 + 512], ALU.mult, r=[pk, "zv"], w=["BV"])
                for nm, dst in (("RT", C.rRT), ("AT", C.rAT), ("BT", C.rBT), ("KT", C.rKT), ("BH", C.rBH), ("KH", C.rKH),
                                ("VT", C.rVT), ("BV", C.rBV), ("g", C.rG)):
                    S.dma("sp", dst[pcs, t0:t0 + TB], H[nm][:], reads=[nm], writes=["d_" + nm], defer=3)
        S.dma("sp", C.rGC.rearrange("p (a c) -> p a c", a=4), gC[:], reads=["gC"], writes=["rGC"])
    S.barrier()


def stage_rwkv_scan(C, l, limit=None):
    S = C.S
    K = C.K
    with ExitStack() as st:
        gC = S.sb([128, 4, NCH], F32, st)
        S.dma("sp", gC[:], C.rGC.rearrange("p (a c) -> p a c", a=4), writes=["gC"])
        Sf = S.sb([128, 4, CH], F32, st)
        Sb = S.sb([128, 4, CH], BF16, st)
        mset(S, "pool", Sf[:], 0.0, w=["Sf"])
        mset(S, "pool", Sb[:], 0.0, w=["Sb"])
        names = ("BT", "KT", "BH", "KH", "VT")
        srcs = dict(RT=C.rRT, AT=C.rAT, BT=C.rBT, KT=C.rKT, BH=C.rBH, KH=C.rKH, VT=C.rVT)
        G = {nm: [S.sb([128, 4, 512], BF16, st) for _ in range(2)] for nm in names}
        GM = {nm: [[S.sb([128, 4, 512], BF16, st) for _ in range(2)] for _hp in range(2)] for nm in ("AT", "RT")}
        for nm in ("AT", "RT"):
            for hp_ in range(2):
                for gb_ in range(2):
                    mset(S, "pool", GM[nm][hp_][gb_][:], 0.0, w=[f"GM{nm}{hp_}{gb_}"])
        yg = [S.sb([128, 4, 512], F32, st) for _ in range(2)]
        tokt = {nm: [S.sb([64, 512], BF16, st) for _ in range(2)] for nm in ("BH", "KH", "VT")}
        Xt = [S.sb([64, 512], BF16, st) for _ in range(2)]
        Yt = [S.sb([64, 512], BF16, st) for _ in range(2)]
        Pm = [[S.sb([64, 512], BF16, st) for _ in range(2)] for _par in range(2)]
        Lak = [S.sb([64, 512], BF16, st) for _ in range(2)]
        Mrb = [S.sb([64, 512], BF16, st) for _ in range(2)]
        Mrk = [S.sb([64, 512], BF16, st) for _ in range(2)]
        Wt = S.sb([64, 512], BF16, st)
        Ut = S.sb([64, 512], BF16, st)
        psp = [S.ps([128, 512], F32, st) for _ in range(5)]
        pss = [S.ps([128, 512], F32, st) for _ in range(3)]
        npp = [0]
        nss = [0]

        def newps():
            i_ = npp[0] % 5
            npp[0] += 1
            return psp[i_], f"psp{i_}"

        def newss():
            i_ = nss[0] % 3
            nss[0] += 1
            return pss[i_], f"pss{i_}"

        def load_group(g):
            gb = g % 2
            for nm in names:
                S.dma("sp", G[nm][gb][:], srcs[nm].rearrange("(c p) t -> p c t", p=128)[:, :, g * 512:(g + 1) * 512],
                      writes=[f"G{nm}{gb}"])
            for nm in ("AT", "RT"):
                for hp_ in range(2):
                    rs_ = slice(hp_ * 64, hp_ * 64 + 64)
                    S.dma("sp", GM[nm][hp_][gb][rs_, :, :],
                          srcs[nm].rearrange("(c p) t -> p c t", p=128)[rs_, :, g * 512:(g + 1) * 512],
                          writes=[f"GM{nm}{hp_}{gb}"])

        SU = K.m64[:, 0, :]
        SL = K.m64[:, 1, :]
        UI = K.m64[:, 2, :]
        I64 = K.m64[:, 3, :]
        nchunks = NCH if limit is None else limit
        fin = {}

        def prep(c):
            g, ci = c // 8, c % 8
            gb = g % 2
            par = c % 2
            tsl = slice(ci * CH, (ci + 1) * CH)
            gk = lambda nm: f"G{nm}{gb}"
            for nm in ("BH", "KH", "VT"):
                ptr_, pk = newps()
                pv = ptr_[:].bitcast(BF16)
                for pc in range(4):
                    tr(S, pv[0:64, pc * 128:(pc + 1) * 128], G[nm][gb][:, pc, tsl], K.ident_bf[:], r=[gk(nm)], w=[pk])
                cp(S, "act", tokt[nm][par][:], pv[0:64, 0:512], r=[pk], w=[f"tok{nm}{par}"])
            pX, pXk = newps()
            pY, pYk = newps()
            for h in range(8):
                hp, pc = h % 2, h // 2
                hc = slice(h * 64, (h + 1) * 64)
                A = GM["AT"][hp][gb][:, pc, tsl]
                Bt = G["BT"][gb][:, pc, tsl]
                ak = f"GMAT{hp}{gb}"
                mm(S, pX[0:64, hc], Bt, A, r=[gk("BT"), ak], w=[pXk])
                mm(S, pY[0:64, hc], A, Bt, r=[gk("BT"), ak], w=[pYk])
            X, Xk = Xt[0], "X0"
            Y, Yk = Yt[0], "Y0"
            tt(S, "dve", X[:], pX[0:64, :], SU, ALU.mult, r=[pXk, "k_m64"], w=[Xk])
            tt(S, "dve", Y[:], pY[0:64, :], SL, ALU.mult, r=[pYk, "k_m64"], w=[Yk])
            pL, pLk = newps()
            pRB, pRBk = newps()
            pRK, pRKk = newps()
            for h in range(8):
                hp, pc = h % 2, h // 2
                hc = slice(h * 64, (h + 1) * 64)
                A = GM["AT"][hp][gb][:, pc, tsl]
                Bt = G["BT"][gb][:, pc, tsl]
                Kt = G["KT"][gb][:, pc, tsl]
                R = GM["RT"][hp][gb][:, pc, tsl]
                ak = f"GMAT{hp}{gb}"
                rk_ = f"GMRT{hp}{gb}"
                mm(S, pL[0:64, hc], Kt, A, r=[gk("KT"), ak], w=[pLk])
                mm(S, pRB[0:64, hc], Bt, R, r=[gk("BT"), rk_], w=[pRBk])
                mm(S, pRK[0:64, hc], Kt, R, r=[gk("KT"), rk_], w=[pRKk])
            tt(S, "dve", Lak[par][:], pL[0:64, :], SU, ALU.mult, r=[pLk, "k_m64"], w=[f"Lak{par}"])
            tt(S, "dve", Mrb[par][:], pRB[0:64, :], UI, ALU.mult, r=[pRBk, "k_m64"], w=[f"Mrb{par}"])
            tt(S, "dve", Mrk[par][:], pRK[0:64, :], UI, ALU.mult, r=[pRKk, "k_m64"], w=[f"Mrk{par}"])
            P_, Pk = Pm[par][0], f"P{par}0"
            tt(S, "pool", P_[:], X[:], I64, ALU.add, r=[Xk, "k_m64"], w=[Pk])
            for k in range(1, NLEV + 1):
                nb = k % 2
                pYn, pYnk = newps()
                for h in range(8):
                    hc = slice(h * 64, (h + 1) * 64)
                    mm(S, pYn[0:64, hc], X[:, hc], Y[:, hc], r=[Xk, Yk], w=[pYnk])
                if k < NLEV:
                    pXn, pXnk = newps()
                    for h in range(8):
                        hc = slice(h * 64, (h + 1) * 64)
                        mm(S, pXn[0:64, hc], Y[:, hc], X[:, hc], r=[Xk, Yk], w=[pXnk])
                Yn, Ynk = Yt[nb], f"Y{nb}"
                cp(S, "act", Yn[:], pYn[0:64, :], r=[pYnk], w=[Ynk])
                if k < NLEV:
                    Xn, Xnk = Xt[nb], f"X{nb}"
                    cp(S, "dve", Xn[:], pXn[0:64, :], r=[pXnk], w=[Xnk])
                pP, pPk = newps()
                for h in range(8):
                    hc = slice(h * 64, (h + 1) * 64)
                    mm(S, pP[0:64, hc], Yn[:, hc], P_[:, hc], r=[Ynk, Pk], w=[pPk])
                Pn, Pnk = Pm[par][nb], f"P{par}{nb}"
                tt(S, "dve", Pn[:], pP[0:64, :], P_[:], ALU.add, r=[pPk, Pk], w=[Pnk])
                P_, Pk = Pn, Pnk
                Y, Yk = Yn, Ynk
                if k < NLEV:
                    X, Xk = Xn, Xnk
            fin[c] = (P_, Pk)

        def seq(c):
            g, ci = c // 8, c % 8
            gb = g % 2
            par = c % 2
            tsl = slice(ci * CH, (ci + 1) * CH)
            P_, Pk = fin[c]
            BHt, BHk = tokt["BH"][par], f"tokBH{par}"
            KHt, KHk = tokt["KH"][par], f"tokKH{par}"
            Vt, Vk = tokt["VT"][par], f"tokVT{par}"
            pW, pWk = newss()
            for h in range(8):
                hp, pc = h % 2, h // 2
                hc = slice(h * 64, (h + 1) * 64)
                mm(S, pW[0:64, hc], Lak[par][:, hc], Vt[:, hc], start=True, stop=False, r=[f"Lak{par}", Vk], w=[pWk])
                mm(S, pW[0:64, hc], GM["AT"][hp][gb][:, pc, tsl], Sb[:, pc, :], start=False, stop=True,
                   r=[f"GMAT{hp}{gb}", "Sb"], w=[pWk])
            cp(S, "act", Wt[:], pW[0:64, :], r=[pWk], w=["Wt"])
            pU, pUk = newss()
            for h in range(8):
                hc = slice(h * 64, (h + 1) * 64)
                mm(S, pU[0:64, hc], P_[:, hc], Wt[:, hc], r=[Pk, "Wt"], w=[pUk])
            cp(S, "act", Ut[:], pU[0:64, :], r=[pUk], w=["Ut"])
            pYT, pYTk = newss()
            pSn, pSnk = newss()
            for h in range(8):
                hp, pc = h % 2, h // 2
                r0 = hp * 64
                hc = slice(h * 64, (h + 1) * 64)
                oc = slice(pc * 64, (pc + 1) * 64)
                mm(S, pSn[r0:r0 + 64, oc], BHt[:, hc], Ut[:, hc], start=True, stop=False, r=[BHk, "Ut"], w=[pSnk])
                mm(S, pSn[r0:r0 + 64, oc], KHt[:, hc], Vt[:, hc], start=False, stop=True, r=[KHk, Vk], w=[pSnk])
            for h in range(8):
                hp, pc = h % 2, h // 2
                r0 = hp * 64
                hc = slice(h * 64, (h + 1) * 64)
                oc = slice(pc * 64, (pc + 1) * 64)
                mm(S, pYT[r0:r0 + 64, oc], Sb[:, pc, :], GM["RT"][hp][gb][:, pc, tsl], start=True, stop=False,
                   r=["Sb", f"GMRT{hp}{gb}"], w=[pYTk])
                mm(S, pYT[r0:r0 + 64, oc], Ut[:, hc], Mrb[par][:, hc], start=False, stop=False, r=["Ut", f"Mrb{par}"], w=[pYTk])
                mm(S, pYT[r0:r0 + 64, oc], Vt[:, hc], Mrk[par][:, hc], start=False, stop=True, r=[Vk, f"Mrk{par}"], w=[pYTk])
            for pc in range(4):
                stt(S, "dve", Sf[:, pc, :], Sf[:, pc, :], gC[:, pc, c:c + 1], pSn[:, pc * 64:(pc + 1) * 64], ALU.mult, ALU.add,
                    r=["Sf", "gC", pSnk], w=["Sf"])
            cp(S, "pool", Sb[:], Sf[:], r=["Sf"], w=["Sb"])
            cp(S, "act", yg[gb][:, :, tsl], pYT[:, 0:256].rearrange("p (a t) -> p a t", t=CH), r=[pYTk], w=[f"yg{gb}"])
            if ci == 7:
                S.dma("sp", C.rYT.rearrange("(c p) t -> p c t", p=128)[:, :, g * 512:(g + 1) * 512], yg[gb][:],
                      reads=[f"yg{gb}"], writes=["rYT"])

        def record(fn):
            lst = []
            S.add = lambda *a, **k: lst.append((a, k))
            try:
                fn()
            finally:
                del S.add
            return lst

        def replay(lst):
            for a, k in lst:
                S.add(*a, **k)

        load_group(0)
        replay(record(lambda: prep(0)))
        for c in range(nchunks):
            if c % 8 == 0 and c // 8 + 1 < NCH // 8:
                load_group(c // 8 + 1)
            ls = record(lambda: seq(c))
            lp = record(lambda: prep(c + 1)) if c + 1 < nchunks else []
            merged = []
            ns_, np_ = len(ls), len(lp)
            ip = 0
            for i_s, op_ in enumerate(ls):
                tgt = (i_s * np_) // max(ns_, 1)
                while ip < tgt:
                    merged.append(lp[ip])
                    ip += 1
                merged.append(op_)
            merged.extend(lp[ip:])
            replay(merged)
    S.barrier()


def stage_rwkv_post(C, l):
    S = C.S
    K = C.K
    inp = C.inp
    PB = 2048
    with ExitStack() as st:
        lnw = S.sb([128, 4, 1], F32, st)
        lnb = S.sb([128, 4, 1], F32, st)
        S.dma("sp", lnw[:], col_ap(inp["rwkv_ln_w"][l], 4), writes=["lnw"], slow=True)
        S.dma("sp", lnb[:], col_ap(inp["rwkv_ln_b"][l], 4), writes=["lnb"], slow=True)
        y = [S.sb([128, PB], F32, st) for _ in range(2)]
        bv = [S.sb([128, PB], BF16, st) for _ in range(2)]
        gg = [S.sb([128, PB], BF16, st) for _ in range(2)]
        cent = S.sb([128, PB], F32, st)
        sq = S.sb([128, PB], F32, st)
        sd = S.sb([128, PB], F32, st)
        ob = [S.sb([128, PB], BF16, st) for _ in range(2)]
        ps = [S.ps([128, 512], F32, st) for _ in range(4)]
        n = 0
        it = 0
        for pc in range(4):
            pcs = slice(pc * 128, (pc + 1) * 128)
            for blk in range(T // PB):
                b = it % 2
                it += 1
                t0 = blk * PB
                S.dma("sp", y[b][:], C.rYT[pcs, t0:t0 + PB], writes=[f"y{b}"])
                S.dma("sp", bv[b][:], C.rBV[pcs, t0:t0 + PB], writes=[f"bv{b}"])
                S.dma("sp", gg[b][:], C.rG[pcs, t0:t0 + PB], writes=[f"gg{b}"])
                for sb in range(PB // 512):
                    cs = slice(sb * 512, (sb + 1) * 512)
                    p, pk = ps[n % 4], f"psq{n % 4}"
                    n += 1
                    mm(S, p[:], K.blkmean_f[:], y[b][:, cs], r=["k_blkmean_f", f"y{b}"], w=[pk])
                    tt(S, "dve", cent[:, cs], y[b][:, cs], p[:], ALU.subtract, r=[pk, f"y{b}"], w=["cent"])
                    tt(S, "pool", sq[:, cs], cent[:, cs], cent[:, cs], ALU.mult, r=["cent"], w=["sq"])
                    p, pk = ps[n % 4], f"psq{n % 4}"
                    n += 1
                    mm(S, p[:], K.blkmean_f[:], sq[:, cs], r=["k_blkmean_f", "sq"], w=[pk])
                    act(S, sd[:, cs], p[:], AF.Sqrt, r=[pk], w=["sd"], bias=K.eps_gn[:])
                S.add("dve", (lambda o, a: (lambda e: e.reciprocal(o, a)))(sd[:], sd[:]), ["sd"], ["sd"])
                tt(S, "dve", cent[:], cent[:], sd[:], ALU.mult, r=["cent", "sd"], w=["cent"])
                ts(S, "dve", cent[:], cent[:], lnw[:, pc, :], ALU.mult, s2=lnb[:, pc, :], op1=ALU.add, r=["cent", "lnw", "lnb"],
                   w=["cent"])
                tt(S, "pool", cent[:], cent[:], bv[b][:], ALU.add, r=["cent", f"bv{b}"], w=["cent"])
                tt(S, "pool", ob[b][:], cent[:], gg[b][:], ALU.mult, r=["cent", f"gg{b}"], w=[f"ob{b}"])
                S.dma("sp", C.oT[512 + pc * 128:512 + (pc + 1) * 128, t0:t0 + PB], ob[b][:], reads=[f"ob{b}"], writes=["oT"], defer=3)
    S.barrier()


def build_layer(C, l, x_in, x_mid, x_out, stages=None):
    S = C.S
    stage_mod(C, l)
    with ExitStack() as lst:
        M = load_mod(C, l, lst)
        with ExitStack() as hst:
            hT = S.sb([128, 8, T], BF16, hst)
            stage_norm(C, x_in, M.A1, M.modcol[:, 0:8, :], hT, hst)
            stage_inproj(C, l, hT)
        stage_index(C, l)
        stage_attn(C, l)
        stage_rwkv_prep(C, l)
        stage_rwkv_scan(C, l)
        stage_rwkv_post(C, l)
        stage_conv(C, l)
        stage_merge(C, l, M, x_in, x_mid)
        with ExitStack() as hst:
            hT = S.sb([128, 8, T], BF16, hst)
            stage_norm(C, x_mid, M.A2, M.modcol[:, 24:32, :], hT, hst)
            stage_ffn_up(C, l, hT)
        stage_ffn_down(C, l, M, x_mid, x_out)


def build_program(debug_out=()):
    nc = bass.Bass("TRN2", target_bir_lowering=False)
    out = nc.dram_tensor("out", [T, D], F32, kind="ExternalOutput").ap()
    with ExitStack() as es:
        C = make_ctx(nc, es, debug_out)
        stage_consts(C)
        stage_bias(C)
        x = C.inp["x"]
        for l in range(DEPTH):
            build_layer(C, l, x, C.xs[2 * l], C.xs[2 * l + 1])
            x = C.xs[2 * l + 1]
        stage_final(C, x, out)
        C.S.emit()
        stats = C.S.stats
    return nc, stats


_PROG = None


def kernel(**inputs):
    global _PROG
    pos = np.asarray(inputs["positions"])
    if not np.array_equal(pos, np.broadcast_to(np.arange(T, dtype=pos.dtype), pos.shape)):
        raise NotImplementedError("kernel is specialised (at build time) for positions == arange(SEQ)")
    if _PROG is None:
        _PROG = build_program()[0]
    nb = np.asarray(inputs["x"]).shape[0]
    in_maps = [host_inputs(inputs, b) for b in range(nb)]
    res = run_bass_kernel_spmd(_PROG, in_maps, core_ids=list(range(nb)))
    return np.stack([np.asarray(r["out"], dtype=np.float32) for r in res.results], axis=0)
```
